# Optimizing a Trainium2 kernel written in Bass

```python
import math
import jax
import jax.numpy as jnp
from jax import lax
import numpy as np

D_MODEL = 1024
BATCH = 8
SEQ = 2048
DEPTH = 4

GLA_HEADS = 4
GLA_DK = 128
GLA_DV = 256
GLA_RANK = 16
GLA_TAU = 16.0
GLA_CHUNK = 64
MOBA_HEADS = 8
MOBA_DH = 64
MOBA_BLOCK = 256
MOBA_TOPK = 3
MOBA_Q_CHUNK = 16
REL_BUCKETS = 32
REL_MAX_DIST = 128
D_FF = 2816
CONV_W = 3
EPS = 1e-6
N_BRANCHES = 2
NEG_INF = -1e30

GLA_QK_W = GLA_HEADS * GLA_DK
GLA_V_W = GLA_HEADS * GLA_DV
MOBA_W = MOBA_HEADS * MOBA_DH
IN_SPLITS = (GLA_QK_W, GLA_QK_W, GLA_V_W, GLA_V_W, GLA_RANK, MOBA_W, MOBA_W, MOBA_W, N_BRANCHES * D_MODEL)
D_IN = 2 * GLA_QK_W + 2 * GLA_V_W + GLA_RANK + 3 * MOBA_W + N_BRANCHES * D_MODEL

kernel_name = 'gla_moba_gated_hybrid'


def rmsnorm(x, g):
    xf = x.astype(jnp.float32)
    y = xf * lax.rsqrt(jnp.mean(xf * xf, axis=-1, keepdims=True) + EPS)
    return (y * g.astype(jnp.float32)).astype(x.dtype)


def gla_chunked(q, k, v, log_a):
    B, T, H, DK = q.shape
    DV = v.shape[-1]
    C = GLA_CHUNK
    NC = T // C

    def to_chunks(t):
        return t.astype(jnp.float32).reshape(B, NC, C, H, t.shape[-1]).transpose(0, 3, 1, 2, 4)

    q, k, v, log_a = map(to_chunks, (q, k, v, log_a))
    q = q * (DK ** -0.5)
    b = jnp.cumsum(log_a, axis=3)
    b_last = b[:, :, :, -1:, :]
    q_dec = q * jnp.exp(b)
    k_dec = k * jnp.exp(-b)
    k_to_end = k * jnp.exp(b_last - b)
    causal = jnp.tril(jnp.ones((C, C), dtype=bool))
    attn = jnp.where(causal, jnp.einsum('bhnid,bhnjd->bhnij', q_dec, k_dec), 0.0)
    o_intra = jnp.einsum('bhnij,bhnjv->bhniv', attn, v)

    def step(S, inp):
        qc, kc, vc, dec = inp
        o = jnp.einsum('bhid,bhdv->bhiv', qc, S)
        S = S * dec[..., None] + jnp.einsum('bhjd,bhjv->bhdv', kc, vc)
        return S, o

    xs = (jnp.moveaxis(q_dec, 2, 0), jnp.moveaxis(k_to_end, 2, 0), jnp.moveaxis(v, 2, 0),
          jnp.moveaxis(jnp.exp(b_last[:, :, :, 0, :]), 2, 0))
    S0 = jnp.zeros((B, H, DK, DV), jnp.float32)
    _, o_inter = lax.scan(step, S0, xs)
    o = o_intra + jnp.moveaxis(o_inter, 0, 2)
    return o.transpose(0, 2, 3, 1, 4).reshape(B, T, H, DV)


def t5_bucket(rel):
    n = jnp.maximum(rel, 0)
    max_exact = REL_BUCKETS // 2
    nf = jnp.maximum(n, 1).astype(jnp.float32)
    large = max_exact + (jnp.log(nf / max_exact) / math.log(REL_MAX_DIST / max_exact)
                         * (REL_BUCKETS - max_exact)).astype(jnp.int32)
    large = jnp.minimum(large, REL_BUCKETS - 1)
    return jnp.where(n < max_exact, n, large)


def moba_attention(q, k, v, rel_bias):
    B, T, H, DH = q.shape
    NB = -(-T // MOBA_BLOCK)
    T_pad = NB * MOBA_BLOCK
    pad = ((0, 0), (0, T_pad - T), (0, 0), (0, 0))
    q, k, v = (jnp.pad(t, pad) for t in (q, k, v))
    QC = MOBA_Q_CHUNK
    NQ = T_pad // QC
    KSEL = min(MOBA_TOPK, NB)
    KB = KSEL * MOBA_BLOCK
    scale = DH ** -0.5
    k_blocks = k.transpose(0, 2, 1, 3).reshape(B, H, NB, MOBA_BLOCK, DH)
    v_blocks = v.transpose(0, 2, 1, 3).reshape(B, H, NB, MOBA_BLOCK, DH)
    k_mean = jnp.mean(k_blocks.astype(jnp.float32), axis=3)
    bias_tab = rel_bias.T.astype(jnp.float32)
    b_idx = jnp.arange(B)[:, None, None, None]
    h_idx = jnp.arange(H)[None, :, None, None]
    k_in_blk = jnp.arange(MOBA_BLOCK)
    q_chunks = q.transpose(0, 2, 1, 3).reshape(B, H, NQ, QC, DH).transpose(2, 0, 1, 3, 4)

    def one_chunk(args):
        qc, c = args
        q_pos = c * QC + jnp.arange(QC)
        q_blk = (c * QC) // MOBA_BLOCK
        gate = jnp.einsum('bhqd,bhnd->bhqn', qc.astype(jnp.float32), k_mean)
        gate = jnp.where(jnp.arange(NB) < q_blk, gate, -jnp.inf)
        _, sel = lax.top_k(gate, KSEL)
        sel_valid = jnp.repeat(sel < q_blk, MOBA_BLOCK, axis=-1)
        k_sel = k_blocks[b_idx, h_idx, sel].reshape(B, H, QC, KB, DH)
        v_sel = v_blocks[b_idx, h_idx, sel].reshape(B, H, QC, KB, DH)
        kpos_sel = (sel[..., None] * MOBA_BLOCK + k_in_blk).reshape(B, H, QC, KB)
        logit_sel = jnp.einsum('bhqd,bhqkd->bhqk', qc, k_sel).astype(jnp.float32) * scale
        logit_sel = logit_sel + bias_tab[h_idx, t5_bucket(q_pos[:, None] - kpos_sel)]
        logit_sel = jnp.where(sel_valid, logit_sel, NEG_INF)
        k_own = lax.dynamic_index_in_dim(k_blocks, q_blk, axis=2, keepdims=False)
        v_own = lax.dynamic_index_in_dim(v_blocks, q_blk, axis=2, keepdims=False)
        kpos_own = q_blk * MOBA_BLOCK + k_in_blk
        logit_own = jnp.einsum('bhqd,bhkd->bhqk', qc, k_own).astype(jnp.float32) * scale
        logit_own = logit_own + bias_tab[:, t5_bucket(q_pos[:, None] - kpos_own[None, :])]
        logit_own = jnp.where(kpos_own[None, :] <= q_pos[:, None], logit_own, NEG_INF)
        p = jax.nn.softmax(jnp.concatenate([logit_sel, logit_own], axis=-1), axis=-1).astype(v.dtype)
        return (jnp.einsum('bhqk,bhqkd->bhqd', p[..., :KB], v_sel)
                + jnp.einsum('bhqk,bhkd->bhqd', p[..., KB:], v_own))

    o = lax.map(one_chunk, (q_chunks, jnp.arange(NQ)))
    o = o.transpose(1, 0, 3, 2, 4).reshape(B, T_pad, H * DH)
    return o[:, :T]


def causal_dwconv(u, w, b):
    out = lax.conv_general_dilated(u, w[:, None, :].astype(u.dtype), window_strides=(1,),
                                   padding=[(CONV_W - 1, 0)], dimension_numbers=('NWC', 'WIO', 'NWC'),
                                   feature_group_count=u.shape[-1])
    return out + b.astype(u.dtype)


def hybrid_layer(x, rel_bias, norm_mix, w_in, w_lr_up, b_forget, gla_out_norm, w_branch_gla,
                 w_branch_moba, w_out, norm_ffn, w_up, conv_w, conv_b, w_down):
    B, T, _ = x.shape
    h = rmsnorm(x, norm_mix)
    proj = h @ w_in
    points = [int(p) for p in np.cumsum(IN_SPLITS)[:-1]]
    qa, ka, va, ra, a_lr, qb, kb, vb, gates = jnp.split(proj, points, axis=-1)
    log_a = jax.nn.log_sigmoid((a_lr @ w_lr_up + b_forget).astype(jnp.float32)) / GLA_TAU
    oa = gla_chunked(qa.reshape(B, T, GLA_HEADS, GLA_DK), ka.reshape(B, T, GLA_HEADS, GLA_DK),
                     va.reshape(B, T, GLA_HEADS, GLA_DV), log_a.reshape(B, T, GLA_HEADS, GLA_DK))
    oa = oa * lax.rsqrt(jnp.mean(oa * oa, axis=-1, keepdims=True) + EPS)
    oa = (oa.reshape(B, T, GLA_V_W) * gla_out_norm.astype(jnp.float32)).astype(x.dtype) * jax.nn.silu(ra)
    ob = moba_attention(qb.reshape(B, T, MOBA_HEADS, MOBA_DH), kb.reshape(B, T, MOBA_HEADS, MOBA_DH),
                        vb.reshape(B, T, MOBA_HEADS, MOBA_DH), rel_bias)
    g_a, g_b = jnp.split(jax.nn.sigmoid(gates), 2, axis=-1)
    mixed = g_a * (oa @ w_branch_gla) + g_b * (ob @ w_branch_moba)
    x = x + mixed @ w_out
    h = rmsnorm(x, norm_ffn)
    u = causal_dwconv(h @ w_up, conv_w, conv_b)
    a, bval = jnp.split(u, 2, axis=-1)
    return x + (jax.nn.silu(a) * bval) @ w_down


def setup_inputs(seed: int = 0) -> dict:
    key = jax.random.key(seed)
    ks = jax.random.split(key, 16)

    def nrm(k, shape, scale):
        return jax.random.normal(k, shape, jnp.float32) * scale

    return {
        'x': nrm(ks[0], (BATCH, SEQ, D_MODEL), 1.0),
        'rel_bias': nrm(ks[1], (REL_BUCKETS, MOBA_HEADS), 0.5),
        'norm_mix': 1.0 + nrm(ks[2], (DEPTH, D_MODEL), 0.05),
        'w_in': nrm(ks[3], (DEPTH, D_MODEL, D_IN), D_MODEL ** -0.5),
        'w_lr_up': nrm(ks[4], (DEPTH, GLA_RANK, GLA_QK_W), GLA_RANK ** -0.5),
        'b_forget': 1.0 + nrm(ks[5], (DEPTH, GLA_QK_W), 0.5),
        'gla_out_norm': 1.0 + nrm(ks[6], (DEPTH, GLA_V_W), 0.05),
        'w_branch_gla': nrm(ks[7], (DEPTH, GLA_V_W, D_MODEL), GLA_V_W ** -0.5),
        'w_branch_moba': nrm(ks[8], (DEPTH, MOBA_W, D_MODEL), MOBA_W ** -0.5),
        'w_out': nrm(ks[9], (DEPTH, D_MODEL, D_MODEL), D_MODEL ** -0.5),
        'norm_ffn': 1.0 + nrm(ks[10], (DEPTH, D_MODEL), 0.05),
        'w_up': nrm(ks[11], (DEPTH, D_MODEL, 2 * D_FF), D_MODEL ** -0.5),
        'conv_w': nrm(ks[12], (DEPTH, CONV_W, 2 * D_FF), CONV_W ** -0.5),
        'conv_b': nrm(ks[13], (DEPTH, 2 * D_FF), 0.02),
        'w_down': nrm(ks[14], (DEPTH, D_FF, D_MODEL), D_FF ** -0.5),
        'norm_final': 1.0 + nrm(ks[15], (D_MODEL,), 0.05),
    }


def reference(x, rel_bias, norm_mix, w_in, w_lr_up, b_forget, gla_out_norm, w_branch_gla,
              w_branch_moba, w_out, norm_ffn, w_up, conv_w, conv_b, w_down, norm_final):
    for l in range(DEPTH):
        x = hybrid_layer(x, rel_bias, norm_mix[l], w_in[l], w_lr_up[l], b_forget[l], gla_out_norm[l],
                         w_branch_gla[l], w_branch_moba[l], w_out[l], norm_ffn[l], w_up[l], conv_w[l],
                         conv_b[l], w_down[l])
    return rmsnorm(x, norm_final)
```

```python
import math
import os
import numpy as np
import concourse.bass as bass
import concourse.mybir as mybir
from concourse.bass_utils import run_bass_kernel_spmd

F32 = mybir.dt.float32
BF16 = mybir.dt.bfloat16
AF = mybir.ActivationFunctionType
ALU = mybir.AluOpType

D = 1024
T = 2048
DEPTH = 4
TG = 256
NG = T // TG
D_FF = 2816
EPS = 1e-6
NEG = -30000.0
NBA = 40
NBF = 22
WA_E = 2048
WSLOT = 3072
NSLOT = 5
NDSLOT = 6
O_QA, O_KA, O_VA, O_RA, O_ALR, O_QB, O_KB, O_VB, O_G = 0, 512, 1024, 2048, 3072, 3088, 3600, 4112, 4624

CV_NMIX = 0
CV_NFFN = DEPTH * 8
CV_GLA = 2 * DEPTH * 8
CV_NFIN = 3 * DEPTH * 8
CV_CW = 3 * DEPTH * 8 + 8
CV_N = CV_CW + DEPTH * 2 * 22 * 4
CM_B31 = 0
CM_NEG = 64
CM_N = 64 + 8 * 64
CMAT_N = 5 * 128


class Buf:
    __slots__ = ("name", "w", "r")

    def __init__(self, name):
        self.name = name
        self.w = {}
        self.r = {}


class Sched:
    ENGS = ("pe", "act", "dve", "pool", "sp")

    def __init__(self, nc, plan):
        self.nc = nc
        self.plan = plan
        self.real = plan is not None
        self.idx = {e: 0 for e in self.ENGS}
        self.incs = {e: 0 for e in self.ENGS}
        self.val = {}
        self.waited = {e: {} for e in self.ENGS}
        self.need = set()
        self.dcount = {}
        self.dsem = {}
        self.n_wait = 0
        if self.real:
            self.eng = {"pe": nc.tensor, "act": nc.scalar, "dve": nc.vector, "pool": nc.gpsimd, "sp": nc.sync}
            self.esem = {e: nc.alloc_semaphore("sem_" + e) for e in self.ENGS}

    def _dsem(self, name):
        if name not in self.dsem:
            self.dsem[name] = self.nc.alloc_semaphore("dsem_" + name) if self.real else None
            self.dcount[name] = 0
        return self.dsem[name]

    def _wait(self, eng, t):
        if t[0] == "e":
            _, f, i = t
            if f == eng:
                if eng == "pe" or self.idx[eng] - i > 6:
                    return
            if self.waited[eng].get(f, -1) >= i:
                return
            self.waited[eng][f] = i
            self.need.add((f, i))
            if self.real:
                self.eng[eng].wait_ge(self.esem[f], self.val[(f, i)])
                self.n_wait += 1
        else:
            _, name, n = t
            key = ("d", name)
            if self.waited[eng].get(key, 0) >= n:
                return
            self.waited[eng][key] = n
            if self.real:
                self.eng[eng].wait_ge(self.dsem[name], 16 * n)
                self.n_wait += 1

    def _deps(self, eng, reads, writes):
        for b in reads:
            for t in b.w.values():
                self._wait(eng, t)
        for b in writes:
            for t in b.w.values():
                self._wait(eng, t)
            for t in b.r.values():
                self._wait(eng, t)

    def op(self, eng, fn, reads=(), writes=()):
        self._deps(eng, reads, writes)
        i = self.idx[eng]
        if self.real:
            ins = fn()
            if (eng, i) in self.plan:
                self.incs[eng] += 1
                ins.then_inc(self.esem[eng], 1)
                self.val[(eng, i)] = self.incs[eng]
        self.idx[eng] = i + 1
        t = ("e", eng, i)
        for b in writes:
            b.w[eng] = t
        for b in reads:
            b.r[eng] = t
        return t

    def dma(self, q, fn, sem, reads=(), writes=()):
        self._deps(q, reads, writes)
        self._dsem(sem)
        self.dcount[sem] += 1
        n = self.dcount[sem]
        if self.real:
            fn().then_inc(self.dsem[sem], 16)
        self.idx[q] += 1
        t = ("d", sem, n)
        for b in writes:
            b.w["d:" + sem] = t
        for b in reads:
            b.r["d:" + sem] = t
        return t

    def wait_all_dma(self, eng, sem):
        self._wait(eng, ("d", sem, self.dcount[sem]))


class StopEmit(Exception):
    pass


class Bank:
    def __init__(self, t, i):
        self.t = t
        self.buf = Buf("bank%d" % i)
        self.fresh = True


class Rot:
    def __init__(self, items):
        self.items = items
        self.i = 0

    def next(self):
        it = self.items[self.i % len(self.items)]
        self.i += 1
        return it


class Prog:
    def __init__(self, n_layers=DEPTH, n_groups=NG):
        self.L = n_layers
        self.NGR = n_groups
        nc = bass.Bass("TRN2", target_bir_lowering=False)
        self.nc = nc
        L = n_layers
        dt = nc.dram_tensor
        self.d_x = dt("xT", [D, T], F32, kind="ExternalInput").ap()
        self.d_wa = dt("wa", [L * NBA, 128, WA_E], F32, kind="ExternalInput").ap()
        self.d_wf = dt("wf", [L * NBF, 128, WSLOT], F32, kind="ExternalInput").ap()
        self.d_walr = dt("walr", [L, 128, 128], F32, kind="ExternalInput").ap()
        self.d_wlr = dt("wlr", [17, DEPTH * 512], F32, kind="ExternalInput").ap()
        self.d_cvec = dt("cvec", [128, CV_N], F32, kind="ExternalInput").ap()
        self.d_cbias = dt("cbias", [128, 8 * 512], F32, kind="ExternalInput").ap()
        self.d_cmisc = dt("cmisc", [128, CM_N], F32, kind="ExternalInput").ap()
        self.d_cmat = dt("cmat", [128, CMAT_N], F32, kind="ExternalInput").ap()
        self.d_out = dt("outT", [D, T], F32, kind="ExternalOutput").ap()
        self.d_wa16 = dt("wa16", [L * NBA, 128, WA_E], BF16).ap()
        self.d_wf16 = dt("wf16", [L * NBF, 128, WSLOT], BF16).ap()
        self.d_walr16 = dt("walr16", [L, 128, 128], BF16).ap()

        A = nc.alloc_sbuf_tensor
        self.X = A("X", [128, 8, T], F32)
        self.KT = A("KT", [128, 4, T], BF16)
        self.Vaug = A("Vaug", [128, 16, 8, 65], BF16)
        self.Sf = A("Sf", [128, 4, 256], F32)
        self.Sbf = A("Sbf", [128, 4, 256], BF16)
        self.wslot = [A("wslot%d" % i, [128, WA_E], BF16) for i in range(NSLOT)]
        self.dslot = [A("dslot%d" % i, [128, 1024], BF16) for i in range(NDSLOT)]
        self.alrw = [A("alrw%d" % i, [128, 128], BF16) for i in range(2)]
        self.h = A("h", [128, 8, TG], BF16)
        self.rstd = A("rstd", [128, TG], F32)
        self.qdec = A("qdec", [128, 4, TG], BF16)
        self.kdec = A("kdec", [128, 4, TG], BF16)
        self.kte = A("kte", [128, 2, 512], BF16)
        self.vtm = A("vtm", [128, 2, 1024], BF16)
        self.sra = A("sra", [128, 8, TG], BF16)
        self.QTz = A("QTz", [128, 8, TG], BF16)
        self.alrT = A("alrT", [17, TG], BF16)
        self.lhi = A("lhi", [128, 2, 512], BF16)
        self.llo = A("llo", [128, 2, 512], BF16)
        self.obtm = self.lhi
        self.mixed = self.sra
        self.obT = self.llo[:, :, :].rearrange("p a (b t) -> p (a b) t", t=TG)
        self.oaT = A("oaT", [128, 8, TG], BF16)
        self.accO = A("accO", [128, 2, 8, 65], F32)
        self.gsb = A("gsb", [128, 128], F32)
        self.sel = A("sel", [128, 128], F32)
        self.selw = A("selw", [128, 128], F32)
        self.top8 = A("top8", [128, 16, 8], F32)
        self.smalls = A("smalls", [128, 64], F32)
        self.ksb = A("ksb", [128, 4, 8], BF16)
        self.carry = A("carry", [128, 2, 22, 2], F32)
        self.bigf = [A("bigf%d" % i, [128, 512], F32) for i in range(3)]
        self.smf = [A("smf%d" % i, [128, 2, 258], F32) for i in range(3)]
        self.bfp = [A("bfp%d" % i, [128, 512], BF16) for i in range(6)]
        self.cvec = A("cvec_s", [128, CV_N], F32)
        self.cbias = A("cbias_s", [128, 8, 512], BF16)
        self.cmisc = A("cmisc_s", [128, CM_N], F32)
        self.cmat = A("cmat_s", [128, CMAT_N], BF16)
        self.wlr = A("wlr_s", [17, 512], BF16)
        self.expb31 = A("expb31", [128, 64], F32)
        self.banks_t = [nc.alloc_psum_tensor("bank%d" % i, [128, 512], F32) for i in range(8)]
        self.sbuf_left = nc.sbuf_bytes_remaining

    def emit(self, S):
        self.S = S
        nc = self.nc
        self.banks = [Bank(t, i) for i, t in enumerate(self.banks_t)]
        self.P = Rot(self.banks)
        self.bigfR = Rot([(t, [Buf("bigf%da" % i), Buf("bigf%db" % i)]) for i, t in enumerate(self.bigf)])
        self.smfR = Rot([(t, Buf("smf%d" % i), Buf("smfh%d" % i)) for i, t in enumerate(self.smf)])
        bfl = [(t, Buf("bfp%d" % i)) for i, t in enumerate(self.bfp)]
        self.bfpR = Rot(bfl[0:4])
        self.glaR = Rot(bfl[4:6])
        B = Buf
        self.bX = [B("X%d" % g) for g in range(NG)]
        self.bKT = [B("KT%d" % g) for g in range(NG)]
        self.bV = [B("V%d" % g) for g in range(NG)]
        self.bS = [B("S%d" % i) for i in range(4)]
        self.bSb = [B("Sb%d" % i) for i in range(4)]
        self.bW = [B("wslot%d" % i) for i in range(NSLOT)]
        self.bD = [B("dslot%d" % i) for i in range(NDSLOT)]
        self.bAlrw = [B("alrw0"), B("alrw1")]
        for n in ("h", "rstd", "qdec", "kdec", "kte", "vtm", "sra", "QTz", "alrT", "lhi", "llo", "eTM", "oaT",
                  "obtm", "obT", "mixed", "accO", "gsb", "sel", "selw", "top8", "dec", "ss", "ss2", "rc", "ksumf",
                  "ksb", "carry", "cvec", "cbias", "cmisc", "cmat", "wlr", "expb31"):
            setattr(self, "b_" + n, B(n))
        self.b_obtm = self.b_lhi
        self.b_mixed = self.b_sra
        self.b_obT = self.b_llo
        self.wlist = []
        for l in range(self.L):
            for g in range(self.NGR):
                self.wlist.append(("alr", l))
                for i in range(NBA):
                    self.wlist.append(("a", l * NBA + i))
                for i in range(NBF):
                    self.wlist.append(("f", l * NBF + i))
                    self.wlist.append(("d", l * NBF + i))
        self.w_issued = 0
        self.w_used = 0
        self.alr_n = 0
        self.big_n = 0
        self.d_used = 0
        self.d_n = 0

        self.stop = int(os.environ.get("KSTOP", "99"))
        self.phases = []
        self.init_phase()
        try:
            for l in range(self.L):
                self.layer_init(l)
                for g in range(self.NGR):
                    self.mixer_group(l, g)
                    self.ffn_group(l, g)
                    if l == self.L - 1:
                        self.final_group(g)
        except StopEmit:
            self.final_group(0)
        for sem in ["w%d" % i for i in range(NSLOT)] + ["d%d" % i for i in range(NDSLOT)] + ["alr0", "alr1", "c_wlr", "c_bias", "c_mat", "c_vec", "c_misc"] + ["cv%d" % i for i in range(DEPTH)] + ["cv0_alr", "cv0_a0", "cv0_a1", "cv0_a2", "cv0_a3", "cv0_f0", "cv0_f1"]:
            if sem in S.dcount:
                S.wait_all_dma("sp", sem)
        for sem in ("out0", "out1"):
            if sem in S.dcount:
                S.wait_all_dma("sp", sem)

    def w_issue(self):
        S = self.S
        while self.w_issued < len(self.wlist):
            kind, idx = self.wlist[self.w_issued]
            if kind == "alr":
                j = self.alr_n % 2
                tl, bf, src = self.alrw[j], self.bAlrw[j], self.d_walr16[idx]
                S.dma("sp", lambda: self.nc.sync.dma_start(out=tl[:], in_=src), "alr%d" % j, reads=[self.cvt[("alr", idx)]], writes=[bf])
                self.alr_n += 1
            elif kind == "d":
                if self.d_n >= self.d_used + NDSLOT:
                    return
                j = self.d_n % NDSLOT
                tl, bf, src = self.dslot[j], self.bD[j], self.d_wf16[idx][:, 2048:3072]
                S.dma("sp", lambda: self.nc.sync.dma_start(out=tl[:], in_=src), "d%d" % j, reads=[self.cvt[("f", idx)]], writes=[bf])
                self.d_n += 1
            else:
                if self.big_n >= self.w_used + NSLOT:
                    return
                j = self.big_n % NSLOT
                tl, bf = self.wslot[j], self.bW[j]
                if kind == "a":
                    src = self.d_wa16[idx]
                    S.dma("sp", lambda: self.nc.sync.dma_start(out=tl[:], in_=src), "w%d" % j,
                          reads=[self.cvt[("a", idx)]], writes=[bf])
                else:
                    src = self.d_wf16[idx][:, 0:2048]
                    S.dma("sp", lambda: self.nc.sync.dma_start(out=tl[:], in_=src), "w%d" % j,
                          reads=[self.cvt[("f", idx)]], writes=[bf])
                self.big_n += 1
            self.w_issued += 1

    def w_get(self, off=0):
        assert self.big_n > self.w_used + off, "weight block not issued (ring too small for this access pattern)"
        j = (self.w_used + off) % NSLOT
        return self.wslot[j], self.bW[j]

    def w_done(self):
        self.w_used += 1
        self.w_issue()

    def d_get(self, off=0):
        assert self.d_n > self.d_used + off, "down block not issued"
        j = (self.d_used + off) % NDSLOT
        return self.dslot[j], self.bD[j]

    def d_done(self):
        self.d_used += 1
        self.w_issue()

    def mm(self, bank, out, lhsT, rhs, first, last, reads):
        st = bool(first and bank.fresh)
        if first:
            bank.fresh = False
        self.S.op("pe", lambda: self.nc.tensor.matmul(out, lhsT, rhs, start=st, stop=bool(last), skip_group_check=True),
                  reads=reads, writes=[bank.buf])

    def chk(self, p):
        if self.S.real:
            self.phases.append((p, self.S.idx["pe"]))
        if p >= self.stop:
            raise StopEmit()

    def bank(self):
        b = self.P.next()
        b.fresh = True
        return b

    def init_phase(self):
        S, nc = self.S, self.nc
        act, dve, sp, pool = nc.scalar, nc.vector, nc.sync, nc.gpsimd
        S.dma("sp", lambda: sp.dma_start(out=self.cvec[:], in_=self.d_cvec), "c_vec", writes=[self.b_cvec])
        S.dma("sp", lambda: sp.dma_start(out=self.cmisc[:], in_=self.d_cmisc), "c_misc", writes=[self.b_cmisc])
        S.dma("pool", lambda: pool.dma_start(out=self.cmat[:], in_=self.d_cmat), "c_mat", writes=[self.b_cmat])
        S.dma("pool", lambda: pool.dma_start(out=self.cbias[:], in_=self.d_cbias.rearrange("p (h c) -> p h c", c=512)),
              "c_bias", writes=[self.b_cbias])
        xv = self.d_x.rearrange("(k p) t -> p k t", p=128)
        for g in range(self.NGR):
            S.dma("sp", lambda: sp.dma_start(out=self.X[:, :, g * TG:(g + 1) * TG], in_=xv[:, :, g * TG:(g + 1) * TG]),
                  "x%d" % g, writes=[self.bX[g]])
        self.cvt = {}
        b0 = Buf("cvt0_alr")
        S.dma("pool", lambda: pool.dma_start(out=self.d_walr16[0], in_=self.d_walr[0]), "cv0_alr", writes=[b0])
        self.cvt[("alr", 0)] = b0
        for c in range(4):
            bc = Buf("cvt0_a%d" % c)
            S.dma("pool", lambda: pool.dma_start(out=self.d_wa16[c * 10:(c + 1) * 10], in_=self.d_wa[c * 10:(c + 1) * 10]),
                  "cv0_a%d" % c, writes=[bc])
            for i in range(c * 10, (c + 1) * 10):
                self.cvt[("a", i)] = bc
        for c in range(2):
            bc = Buf("cvt0_f%d" % c)
            S.dma("pool", lambda: pool.dma_start(out=self.d_wf16[c * 11:(c + 1) * 11], in_=self.d_wf[c * 11:(c + 1) * 11]),
                  "cv0_f%d" % c, writes=[bc])
            for i in range(c * 11, (c + 1) * 11):
                self.cvt[("f", i)] = bc
        self.w_issue()
        S.op("dve", lambda: dve.memset(self.Vaug[:], 1.0), writes=self.bV)
        S.op("dve", lambda: dve.memset(self.alrT[:], 1.0), writes=[self.b_alrT])
        S.op("dve", lambda: dve.memset(self.QTz[:], 0.0), writes=[self.b_QTz])
        S.op("dve", lambda: dve.memset(self.ksb[:], 0.0), writes=[self.b_ksb])
        S.op("act", lambda: act.activation(out=self.expb31[:], in_=self.cmisc[:, CM_B31:CM_B31 + 64], func=AF.Exp),
             reads=[self.b_cmisc], writes=[self.b_expb31])
        self.ident = self.cmat[:, 0:128]
        self.ones_m = self.cmat[:, 128:256]
        self.maskU = self.cmat[:, 256:384]
        self.Mle = self.cmat[:, 384:512]
        self.Mgt = self.cmat[:, 512:640]

    def layer_init(self, l):
        S, dve = self.S, self.nc.vector
        S.dma("pool", lambda: self.nc.gpsimd.dma_start(out=self.wlr[:], in_=self.d_wlr[:, l * 512:(l + 1) * 512]), "c_wlr",
              writes=[self.b_wlr])
        S.op("dve", lambda: dve.memset(self.Sf[:], 0.0), writes=self.bS)
        S.op("dve", lambda: dve.memset(self.Sbf[:], 0.0), writes=self.bSb)
        S.op("dve", lambda: dve.memset(self.carry[:], 0.0), writes=[self.b_carry])

    def rmsnorm(self, g, col0, inplace=False):
        S, nc = self.S, self.nc
        act, dve = nc.scalar, nc.vector
        t0 = g * TG
        Xg = self.X[:, :, t0:t0 + TG]
        S.op("act", lambda: act.activation(out=self.h[:], in_=Xg, func=AF.Square), reads=[self.bX[g]], writes=[self.b_h])
        bk = self.bank()
        for k in range(8):
            self.mm(bk, bk.t[:, 0:TG], self.ones_m, self.h[:, k, :], k == 0, k == 7, [self.b_h, self.b_cmat])
        S.op("act", lambda: act.activation(out=self.rstd[:], in_=bk.t[:, 0:TG], func=AF.Ln, bias=EPS),
             writes=[bk.buf, self.b_rstd])
        S.op("act", lambda: act.activation(out=self.rstd[:], in_=self.rstd[:], func=AF.Exp, scale=-0.5),
             reads=[self.b_rstd], writes=[self.b_rstd])
        for k in range(8):
            if inplace:
                S.op("dve", lambda: dve.scalar_tensor_tensor(out=self.X[:, k, t0:t0 + TG], in0=self.X[:, k, t0:t0 + TG],
                                                             scalar=self.cvec[:, col0 + k:col0 + k + 1], in1=self.rstd[:],
                                                             op0=ALU.mult, op1=ALU.mult),
                     reads=[self.b_rstd, self.b_cvec], writes=[self.bX[g]])
            else:
                S.op("dve", lambda: dve.scalar_tensor_tensor(out=self.h[:, k, :], in0=self.X[:, k, t0:t0 + TG],
                                                             scalar=self.cvec[:, col0 + k:col0 + k + 1], in1=self.rstd[:],
                                                             op0=ALU.mult, op1=ALU.mult),
                     reads=[self.bX[g], self.b_rstd, self.b_cvec], writes=[self.b_h])

    def convert_layer(self, l):
        S, pool = self.S, self.nc.gpsimd
        bc = Buf("cvt%d" % l)
        gate = [self.bX[0]]
        S.dma("pool", lambda: pool.dma_start(out=self.d_walr16[l], in_=self.d_walr[l]), "cv%d" % l, reads=gate, writes=[bc])
        S.dma("pool", lambda: pool.dma_start(out=self.d_wa16[l * NBA:(l + 1) * NBA], in_=self.d_wa[l * NBA:(l + 1) * NBA]),
              "cv%d" % l, reads=gate, writes=[bc])
        S.dma("pool", lambda: pool.dma_start(out=self.d_wf16[l * NBF:(l + 1) * NBF], in_=self.d_wf[l * NBF:(l + 1) * NBF]),
              "cv%d" % l, reads=gate, writes=[bc])
        self.cvt[("alr", l)] = bc
        for i in range(l * NBA, (l + 1) * NBA):
            self.cvt[("a", i)] = bc
        for i in range(l * NBF, (l + 1) * NBF):
            self.cvt[("f", i)] = bc

    def mixer_group(self, l, g):
        S, nc = self.S, self.nc
        act, dve = nc.scalar, nc.vector
        t0 = g * TG
        if g == min(1, self.NGR - 1) and l + 1 < self.L:
            self.convert_layer(l + 1)
        h, hB = self.h, self.b_h
        self.rmsnorm(g, CV_NMIX + l * 8)
        self.chk(0)

        ja = (l * self.NGR + g) % 2
        alrw, alrwB = self.alrw[ja], self.bAlrw[ja]
        bk = self.bank()
        for k in range(8):
            self.mm(bk, bk.t[0:16, 0:TG], alrw[:, k * 16:(k + 1) * 16], h[:, k, :], k == 0, k == 7, [alrwB, hB])
        S.op("act", lambda: act.activation(out=self.alrT[0:16, :], in_=bk.t[0:16, 0:TG], func=AF.Copy),
             writes=[bk.buf, self.b_alrT])
        eTMs = []
        for a in range(2):
            bk = self.bank()
            self.mm(bk, bk.t[:, :], self.alrT[:, a * 128:(a + 1) * 128], self.wlr[:, :], True, True,
                    [self.b_alrT, self.b_wlr])
            e, eB = self.bigfR.next()
            S.op("act", lambda: act.activation(out=e[:], in_=bk.t[:, :], func=AF.Exp, scale=-1.0), writes=[bk.buf] + eB)
            S.op("act", lambda: act.activation(out=e[:], in_=e[:], func=AF.Ln, bias=1.0), reads=eB, writes=eB)
            S.op("dve", lambda: dve.tensor_copy(out=self.lhi[:, a, :], in_=e[:]), reads=eB, writes=[self.b_lhi])
            S.op("dve", lambda: dve.tensor_tensor(out=self.llo[:, a, :], in0=e[:], in1=self.lhi[:, a, :], op=ALU.subtract),
                 reads=eB + [self.b_lhi], writes=[self.b_llo])
            bk2 = self.bank()
            self.mm(bk2, bk2.t[:, :], self.Mgt, self.lhi[:, a, :], True, False, [self.b_cmat, self.b_lhi])
            self.mm(bk2, bk2.t[:, :], self.Mgt, self.llo[:, a, :], False, True, [self.b_cmat, self.b_llo])
            et, etB = self.bigfR.next()
            S.op("act", lambda: act.activation(out=et[:], in_=bk2.t[:, :], func=AF.Exp), writes=[bk2.buf] + etB)
            eTMs.append((et, etB))

        self.chk(1)
        def tm_pair(nblk):
            bks = [self.bank(), self.bank()]
            for q in range(nblk):
                w, wB = self.w_get()
                for a in range(2):
                    for k in range(8):
                        self.mm(bks[a], bks[a].t[:, q * 256:(q + 1) * 256], h[:, k, a * 128:(a + 1) * 128], w[:, k * 256:(k + 1) * 256],
                                k == 0, k == 7, [wB, hB])
                self.w_done()
            return bks

        bks = tm_pair(2)
        for a in range(2):
            bk = bks[a]
            et, etB = eTMs[a]
            S.op("dve", lambda: dve.tensor_tensor(out=self.kte[:, a, :], in0=bk.t[:, :], in1=et[:], op=ALU.mult),
                 reads=etB, writes=[bk.buf, self.b_kte])
        for hp in range(2):
            wq, wqB = self.w_get(0)
            wk, wkB = self.w_get(1)
            for hh in (2 * hp, 2 * hp + 1):
                c = hh % 2
                bkb = self.bank()
                for a in range(2):
                    o = bkb.t[:, a * 128:(a + 1) * 128]
                    self.mm(bkb, o, self.lhi[:, a, hh * 128:(hh + 1) * 128], self.Mle, True, False, [self.b_lhi, self.b_cmat])
                    self.mm(bkb, o, self.llo[:, a, hh * 128:(hh + 1) * 128], self.Mle, False, True, [self.b_llo, self.b_cmat])
                ebt, ebBs = self.bigfR.next()
                eb = ebt[:, 0:TG]
                enb = ebt[:, TG:2 * TG]
                S.op("act", lambda: act.activation(out=eb, in_=bkb.t[:, 0:TG], func=AF.Exp), writes=[bkb.buf] + ebBs)
                S.op("act", lambda: act.activation(out=enb, in_=bkb.t[:, 0:TG], func=AF.Exp, scale=-1.0), writes=[bkb.buf] + ebBs)
                for a in range(2):
                    S.op("dve", lambda: dve.tensor_copy(out=self.smalls[:, hh * 2 + a:hh * 2 + a + 1],
                                                        in_=ebt[:, a * 128 + 127:a * 128 + 128]),
                         reads=ebBs, writes=[self.b_dec])
                bq = self.bank()
                for k in range(8):
                    self.mm(bq, bq.t[:, 0:TG], wq[:, k * 256 + c * 128:k * 256 + (c + 1) * 128], h[:, k, :], k == 0, k == 7, [wqB, hB])
                for k in range(8):
                    self.mm(bq, bq.t[:, TG:2 * TG], wk[:, k * 256 + c * 128:k * 256 + (c + 1) * 128], h[:, k, :], k == 0, k == 7, [wkB, hB])
                S.op("dve", lambda: dve.scalar_tensor_tensor(out=self.qdec[:, hh, :], in0=bq.t[:, 0:TG], scalar=128.0 ** -0.5, in1=eb,
                                                             op0=ALU.mult, op1=ALU.mult),
                     reads=ebBs, writes=[bq.buf, self.b_qdec])
                S.op("dve", lambda: dve.tensor_tensor(out=self.kdec[:, hh, :], in0=bq.t[:, TG:2 * TG], in1=enb, op=ALU.mult),
                     reads=ebBs, writes=[bq.buf, self.b_kdec])
            self.w_done()
            self.w_done()

        self.chk(2)

        for half in range(2):
            bks = tm_pair(2)
            for a in range(2):
                bk = bks[a]
                S.op("act", lambda: act.activation(out=self.vtm[:, a, half * 512:(half + 1) * 512], in_=bk.t[:, :], func=AF.Copy),
                     writes=[bk.buf, self.b_vtm])
        self.chk(3)
        for pr2 in range(2):
            w, wB = self.w_get()
            bk = self.bank()
            for j in range(2):
                for k in range(8):
                    self.mm(bk, bk.t[:, j * TG:(j + 1) * TG], w[:, k * 256 + j * 128:k * 256 + (j + 1) * 128], h[:, k, :],
                            k == 0, k == 7, [wB, hB])
            for j in range(2):
                p = 2 * pr2 + j
                S.op("act", lambda: act.activation(out=self.QTz[0:64, 2 * p, :], in_=bk.t[0:64, j * TG:(j + 1) * TG],
                                                   func=AF.Copy, scale=0.125), writes=[bk.buf, self.b_QTz])
                S.op("act", lambda: act.activation(out=self.QTz[64:128, 2 * p + 1, :], in_=bk.t[64:128, j * TG:(j + 1) * TG],
                                                   func=AF.Copy, scale=0.125), writes=[bk.buf, self.b_QTz])
            self.w_done()
        S.op("dve", lambda: dve.memset(self.smalls[:, 32:64], 0.0), writes=[self.b_ksumf])
        for pr2 in range(2):
            w, wB = self.w_get()
            bk = self.bank()
            for j in range(2):
                for k in range(8):
                    self.mm(bk, bk.t[:, j * TG:(j + 1) * TG], w[:, k * 256 + j * 128:k * 256 + (j + 1) * 128], h[:, k, :],
                            k == 0, k == 7, [wB, hB])
            for j in range(2):
                p = 2 * pr2 + j
                S.op("act", lambda: act.activation(out=self.KT[:, p, t0:t0 + TG], in_=bk.t[:, j * TG:(j + 1) * TG], func=AF.Copy,
                                                   accum_out=self.smalls[:, 32 + p:33 + p]),
                     writes=[bk.buf, self.bKT[g], self.b_ksumf])
            self.w_done()
        S.op("dve", lambda: dve.tensor_copy(out=self.ksb[:, :, g], in_=self.smalls[:, 32:36]),
             reads=[self.b_ksumf], writes=[self.b_ksb])
        bks = tm_pair(2)
        for a in range(2):
            bk = bks[a]
            S.op("act", lambda: act.activation(out=self.Vaug[:, 2 * g + a, :, 0:64],
                                               in_=bk.t[:, :].rearrange("p (h d) -> p h d", d=64), func=AF.Copy),
                 writes=[bk.buf, self.bV[g]])
        self.chk(4)
        for i in range(4):
            w, wB = self.w_get()
            bk = self.bank()
            for j in range(2):
                for k in range(8):
                    self.mm(bk, bk.t[:, j * TG:(j + 1) * TG], w[:, k * 256 + j * 128:k * 256 + (j + 1) * 128], h[:, k, :],
                            k == 0, k == 7, [wB, hB])
            S.op("act", lambda: act.activation(out=self.sra[:, 2 * i:2 * i + 2, :],
                                               in_=bk.t[:, :].rearrange("p (j t) -> p j t", t=TG), func=AF.Silu),
                 writes=[bk.buf, self.b_sra])
            self.w_done()

        self.chk(5)
        self.gla_moba_group(l, g)
        self.chk(7)

        for dtile in range(8):
            wA, wAB = self.w_get(0)
            wG, wGB = self.w_get(1)
            b1 = self.bank()
            for c in range(8):
                self.mm(b1, b1.t[:, 0:TG], wA[:, c * 128:(c + 1) * 128], self.oaT[:, c, :], c == 0, c == 7, [wAB, self.b_oaT])
            for c in range(4):
                self.mm(b1, b1.t[:, TG:2 * TG], wA[:, 1024 + c * 128:1024 + (c + 1) * 128], self.obT[:, c, :], c == 0, c == 3,
                        [wAB, self.b_obT])
            b2 = self.bank()
            for k in range(8):
                self.mm(b2, b2.t[:, 0:TG], wG[:, k * 128:(k + 1) * 128], h[:, k, :], k == 0, k == 7, [wGB, hB])
            for k in range(8):
                self.mm(b2, b2.t[:, TG:2 * TG], wG[:, 1024 + k * 128:1024 + (k + 1) * 128], h[:, k, :], k == 0, k == 7, [wGB, hB])
            sg, sgB = self.bigfR.next()
            S.op("act", lambda: act.activation(out=sg[:], in_=b2.t[:, :], func=AF.Sigmoid), writes=[b2.buf] + sgB)
            S.op("dve", lambda: dve.tensor_tensor(out=sg[:], in0=b1.t[:, :], in1=sg[:], op=ALU.mult), writes=[b1.buf] + sgB)
            S.op("dve", lambda: dve.tensor_tensor(out=self.mixed[:, dtile, :], in0=sg[:, 0:TG], in1=sg[:, TG:2 * TG], op=ALU.add),
                 reads=sgB, writes=[self.b_mixed])
            self.w_done()
            self.w_done()
        self.chk(8)
        for i in range(4):
            w, wB = self.w_get()
            bk = self.bank()
            for j in range(2):
                for k in range(8):
                    self.mm(bk, bk.t[:, j * TG:(j + 1) * TG], w[:, k * 256 + j * 128:k * 256 + (j + 1) * 128], self.mixed[:, k, :],
                            k == 0, k == 7, [wB, self.b_mixed])
            d0 = 2 * i
            S.op("dve", lambda: dve.tensor_tensor(out=self.X[:, d0:d0 + 2, t0:t0 + TG], in0=self.X[:, d0:d0 + 2, t0:t0 + TG],
                                                  in1=bk.t[:, :].rearrange("p (j t) -> p j t", t=TG), op=ALU.add),
                 writes=[bk.buf, self.bX[g]])
            self.w_done()

    def gla_unit(self, l, g, a, hh):
        S, nc = self.S, self.nc
        act, dve, pe = nc.scalar, nc.vector, nc.tensor
        if a == 0 and hh == 0:
            S.op("dve", lambda: dve.memset(self.smalls[:, 8:16], 0.0), writes=[self.b_ss])
        if True:
            sl = slice(a * 128, (a + 1) * 128)
            if True:
                vs = self.vtm[:, a, hh * 256:(hh + 1) * 256]
                bA = self.bank()
                self.mm(bA, bA.t[:, 0:128], self.kdec[:, hh, sl], self.qdec[:, hh, sl], True, True, [self.b_kdec, self.b_qdec])
                at, atB = self.glaR.next()
                S.op("dve", lambda: dve.tensor_tensor(out=at[:, 0:128], in0=bA.t[:, 0:128], in1=self.maskU, op=ALU.mult),
                     reads=[self.b_cmat], writes=[bA.buf, atB])
                bO = self.bank()
                self.mm(bO, bO.t[:, 0:256], self.qdec[:, hh, sl], self.Sbf[:, hh, :], True, False, [self.b_qdec, self.bSb[hh]])
                self.mm(bO, bO.t[:, 0:256], at[:, 0:128], vs, False, True, [atB, self.b_vtm])
                self.mm(bO, bO.t[:, 256:512], self.kte[:, a, hh * 128:(hh + 1) * 128], vs, True, True, [self.b_kte, self.b_vtm])
                col = hh * 2 + a
                S.op("dve", lambda: dve.scalar_tensor_tensor(out=self.Sf[:, hh, :], in0=self.Sf[:, hh, :],
                                                             scalar=self.smalls[:, col:col + 1], in1=bO.t[:, 256:512],
                                                             op0=ALU.mult, op1=ALU.add),
                     reads=[self.b_dec], writes=[bO.buf, self.bS[hh]])
                S.op("act", lambda: act.activation(out=self.Sbf[:, hh, :], in_=self.Sf[:, hh, :], func=AF.Copy),
                     reads=[self.bS[hh]], writes=[self.bSb[hh]])
                junk, junkBs = self.bigfR.next()
                S.op("act", lambda: act.activation(out=junk[:, 0:256], in_=bO.t[:, 0:256], func=AF.Square, scale=1.0 / 16.0,
                                                   accum_out=self.smalls[:, 8 + col:9 + col]),
                     writes=[bO.buf, self.b_ss] + junkBs)
                S.op("act", lambda: act.activation(out=self.smalls[:, 16 + col:17 + col], in_=self.smalls[:, 8 + col:9 + col],
                                                   func=AF.Ln, bias=EPS), reads=[self.b_ss], writes=[self.b_ss2])
                S.op("act", lambda: act.activation(out=self.smalls[:, 16 + col:17 + col], in_=self.smalls[:, 16 + col:17 + col],
                                                   func=AF.Exp, scale=-0.5), reads=[self.b_ss2], writes=[self.b_ss2])
                oa, oaB = self.glaR.next()
                S.op("dve", lambda: dve.tensor_scalar(out=oa[:, 0:256], in0=bO.t[:, 0:256], scalar1=self.smalls[:, 16 + col:17 + col],
                                                      scalar2=None, op0=ALU.mult),
                     reads=[self.b_ss2], writes=[bO.buf, oaB])
                bT = self.bank()
                bTv = bT.t[:, :].bitcast(BF16)
                for c in range(2):
                    S.op("pe", lambda: pe.transpose(bTv[:, c * 128:(c + 1) * 128], oa[:, c * 128:(c + 1) * 128], self.ident),
                         reads=[oaB, self.b_cmat], writes=[bT.buf])
                for c in range(2):
                    ch = 2 * hh + c
                    gcol = CV_GLA + l * 8 + ch
                    S.op("dve", lambda: dve.scalar_tensor_tensor(out=self.oaT[:, ch, sl], in0=bTv[:, c * 128:(c + 1) * 128],
                                                                 scalar=self.cvec[:, gcol:gcol + 1], in1=self.sra[:, ch, sl],
                                                                 op0=ALU.mult, op1=ALU.mult),
                         reads=[self.b_cvec, self.b_sra], writes=[bT.buf, self.b_oaT])

    def moba_gate(self, l, g):
        S, nc = self.S, self.nc
        act, dve, pe = nc.scalar, nc.vector, nc.tensor
        if g >= 1:
            bG = self.bank()
            for a in range(2):
                for hd in range(8):
                    o = bG.t[:, a * 64 + hd * 8:a * 64 + hd * 8 + 8]
                    self.mm(bG, o, self.QTz[:, hd, a * 128:(a + 1) * 128], self.ksb[:, hd // 2, :], True, True,
                            [self.b_QTz, self.b_ksb])
            for a in range(2):
                S.op("dve", lambda: dve.tensor_tensor(out=self.gsb[:, a * 64:(a + 1) * 64], in0=bG.t[:, a * 64:(a + 1) * 64],
                                                      in1=self.cmisc[:, CM_NEG + g * 64:CM_NEG + (g + 1) * 64], op=ALU.add),
                     reads=[self.b_cmisc], writes=[bG.buf, self.b_gsb])
            for i in range(16):
                S.op("dve", lambda: dve.max(out=self.top8[:, i, :], in_=self.gsb[:, i * 8:(i + 1) * 8]),
                     reads=[self.b_gsb], writes=[self.b_top8])
            for i in range(16):
                S.op("dve", lambda: dve.tensor_scalar(out=self.sel[:, i * 8:(i + 1) * 8], in0=self.gsb[:, i * 8:(i + 1) * 8],
                                                      scalar1=self.top8[:, i, 2:3], scalar2=None, op0=ALU.is_ge),
                     reads=[self.b_gsb, self.b_top8], writes=[self.b_sel])
            for a in range(2):
                S.op("dve", lambda: dve.tensor_tensor(out=self.selw[:, a * 64:(a + 1) * 64], in0=self.sel[:, a * 64:(a + 1) * 64],
                                                      in1=self.expb31[:], op=ALU.mult),
                     reads=[self.b_sel, self.b_expb31], writes=[self.b_selw])

    def gla_moba_group(self, l, g):
        S, nc = self.S, self.nc
        act, dve, pe = nc.scalar, nc.vector, nc.tensor
        self.moba_gate(l, g)
        units = [(a, hh) for a in range(2) for hh in range(4)]
        for u in units:
            self.gla_unit(l, g, *u)
        if self.S.real:
            self.phases.append((6, self.S.idx["pe"]))
        items = []
        for hd in range(8):
            items.append((hd, g))
            for n in range(g - 1, -1, -1):
                items.append((hd, n))
        n_it = len(items)
        LA = 2
        pend = []
        for i in range(n_it + LA):
            if i < n_it:
                pend.append(self.moba_stage1(g, *items[i]))
            if i >= LA:
                j = i - LA
                self.moba_stage2(g, items[j][0], items[j][1], pend[j])
        for a in range(2):
            bT = self.bank()
            bTv = bT.t[:, :].bitcast(BF16)
            for c in range(4):
                S.op("pe", lambda: pe.transpose(bTv[:, c * 128:(c + 1) * 128], self.obtm[:, a, c * 128:(c + 1) * 128], self.ident),
                     reads=[self.b_obtm, self.b_cmat], writes=[bT.buf])
            S.op("act", lambda: act.activation(out=self.obT[:, :, a * 128:(a + 1) * 128],
                                               in_=bTv[:, 0:512].rearrange("p (c t) -> p c t", t=128), func=AF.Copy),
                 writes=[bT.buf, self.b_obT])

    def moba_stage1(self, g, hd, n):
        S, act = self.S, self.nc.scalar
        pr = hd // 2
        bS = self.bank()
        Q = self.QTz[:, hd, :]
        rd = [self.b_QTz, self.bKT[n]]
        rdb = [self.b_cmat, self.b_cbias]
        BT = self.cbias
        if n == g:
            j0, j1 = 2 * g, 2 * g + 1
            self.mm(bS, bS.t[:, 0:256], self.KT[:, pr, j0 * 128:(j0 + 1) * 128], Q[:, 0:256], True, False, rd)
            self.mm(bS, bS.t[:, 0:256], self.ident, BT[:, hd, 0:256], False, True, rdb)
            self.mm(bS, bS.t[:, 256:384], self.KT[:, pr, j1 * 128:(j1 + 1) * 128], Q[:, 128:256], True, False, rd)
            self.mm(bS, bS.t[:, 256:384], self.ident, BT[:, hd, 0:128], False, True, rdb)
            width = 384
        else:
            j0, j1 = 2 * n, 2 * n + 1
            near = (n == g - 1)
            self.mm(bS, bS.t[:, 0:256], self.KT[:, pr, j0 * 128:(j0 + 1) * 128], Q[:, 0:256], True, not near, rd)
            if near:
                self.mm(bS, bS.t[:, 0:256], self.ident, BT[:, hd, 256:512], False, True, rdb)
            self.mm(bS, bS.t[:, 256:512], self.KT[:, pr, j1 * 128:(j1 + 1) * 128], Q[:, 0:256], True, not near, rd)
            if near:
                self.mm(bS, bS.t[:, 256:512], self.ident, BT[:, hd, 128:384], False, True, rdb)
            width = 512
        pt, ptB = self.bfpR.next()
        S.op("act", lambda: act.activation(out=pt[:, 0:width], in_=bS.t[:, 0:width], func=AF.Exp), writes=[bS.buf, ptB])
        return (pt, ptB)

    def moba_stage2(self, g, hd, n, st):
        S, dve = self.S, self.nc.vector
        pt, ptB = st
        bO = self.bank()
        V = self.Vaug
        if n == g:
            j0, j1 = 2 * g, 2 * g + 1
            rd = [ptB, self.bV[g]]
            self.mm(bO, bO.t[:, 0:65], pt[:, 0:128], V[:, j0, hd, :], True, True, rd)
            self.mm(bO, bO.t[:, 65:130], pt[:, 128:256], V[:, j0, hd, :], True, False, rd)
            self.mm(bO, bO.t[:, 65:130], pt[:, 256:384], V[:, j1, hd, :], False, True, rd)
            S.op("dve", lambda: dve.tensor_copy(out=self.accO[:, :, hd, :], in_=bO.t[:, 0:130].rearrange("p (a c) -> p a c", c=65)),
                 writes=[bO.buf, self.b_accO])
        else:
            j0, j1 = 2 * n, 2 * n + 1
            rd = [ptB, self.bV[n]]
            for a in range(2):
                o = bO.t[:, a * 65:(a + 1) * 65]
                self.mm(bO, o, pt[:, a * 128:(a + 1) * 128], V[:, j0, hd, :], True, False, rd)
                self.mm(bO, o, pt[:, 256 + a * 128:256 + (a + 1) * 128], V[:, j1, hd, :], False, True, rd)
            wt, wtB = (self.sel, self.b_sel) if n == g - 1 else (self.selw, self.b_selw)
            for a in range(2):
                cidx = a * 64 + hd * 8 + n
                S.op("dve", lambda: dve.scalar_tensor_tensor(out=self.accO[:, a, hd, :], in0=bO.t[:, a * 65:(a + 1) * 65],
                                                             scalar=wt[:, cidx:cidx + 1], in1=self.accO[:, a, hd, :],
                                                             op0=ALU.mult, op1=ALU.add),
                     reads=[wtB], writes=[bO.buf, self.b_accO])
        if n == 0:
            S.op("dve", lambda: dve.reciprocal(out=self.smalls[:, 24:26], in_=self.accO[:, :, hd, 64]),
                 reads=[self.b_accO], writes=[self.b_rc])
            for a in range(2):
                S.op("dve", lambda: dve.tensor_scalar(out=self.obtm[:, a, hd * 64:(hd + 1) * 64], in0=self.accO[:, a, hd, 0:64],
                                                      scalar1=self.smalls[:, 24 + a:25 + a], scalar2=None, op0=ALU.mult),
                     reads=[self.b_accO, self.b_rc], writes=[self.b_obtm])

    def ffn_group(self, l, g):
        S, nc = self.S, self.nc
        act, dve = nc.scalar, nc.vector
        t0 = g * TG
        h, hB = self.h, self.b_h
        self.chk(9)
        self.rmsnorm(g, CV_NFFN + l * 8)
        accb = self.banks[4:8]
        for b in accb:
            b.fresh = True
        rot = Rot(self.banks[0:4])
        u0 = self.w_used
        slots = {}

        pool = nc.gpsimd
        st = {}

        def s1(ct):
            w, wB = self.w_get(0)
            bk = rot.next()
            bk.fresh = True
            for k in range(8):
                self.mm(bk, bk.t[:, 0:TG], w[:, k * 128:(k + 1) * 128], h[:, k, :], k == 0, k == 7, [wB, hB])
            for k in range(8):
                self.mm(bk, bk.t[:, TG:2 * TG], w[:, 1024 + k * 128:1024 + (k + 1) * 128], h[:, k, :],
                        k == 0, k == 7, [wB, hB])
            ue, ueB, ueH = self.smfR.next()
            S.op("dve", lambda: dve.tensor_copy(out=ue[:, :, 0:2], in_=self.carry[:, :, ct, :]), reads=[self.b_carry], writes=[ueH])
            S.op("act", lambda: act.activation(out=ue[:, :, 2:258], in_=bk.t[:, :].rearrange("p (a t) -> p a t", t=TG), func=AF.Copy),
                 writes=[bk.buf, ueB])
            st[ct] = dict(ue=ue, ueB=ueB, ueH=ueH)
            self.w_done()

        def s2(ct):
            ue, ueB, ueH = st[ct]["ue"], st[ct]["ueB"], st[ct]["ueH"]
            S.op("dve", lambda: dve.tensor_copy(out=self.carry[:, :, ct, :], in_=ue[:, :, 256:258]), reads=[ueB], writes=[self.b_carry])
            cc, ccBs = self.bigfR.next()
            ccB, ccB2 = ccBs
            st[ct].update(cc=cc, ccB=ccB, ccB2=ccB2)
            for ab in range(2):
                cbuf = ccB if ab == 0 else ccB2
                cb = CV_CW + ((l * 2 + ab) * 22 + ct) * 4
                co = cc[:, ab * TG:(ab + 1) * TG]
                wr = [cbuf]
                S.op("pool", lambda: pool.tensor_scalar(out=co, in0=ue[:, ab, 2:258], scalar1=self.cvec[:, cb + 2:cb + 3],
                                                        scalar2=self.cvec[:, cb + 3:cb + 4], op0=ALU.mult, op1=ALU.add),
                     reads=[ueB, self.b_cvec], writes=wr)
            for ab in range(2):
                cbuf = ccB if ab == 0 else ccB2
                cb = CV_CW + ((l * 2 + ab) * 22 + ct) * 4
                co = cc[:, ab * TG:(ab + 1) * TG]
                wr = [cbuf]
                S.op("dve", lambda: dve.scalar_tensor_tensor(out=co, in0=ue[:, ab, 1:257], scalar=self.cvec[:, cb + 1:cb + 2], in1=co,
                                                             op0=ALU.mult, op1=ALU.add),
                     reads=[ueB, ueH, self.b_cvec], writes=wr)
                S.op("dve", lambda: dve.scalar_tensor_tensor(out=co, in0=ue[:, ab, 0:256], scalar=self.cvec[:, cb:cb + 1], in1=co,
                                                             op0=ALU.mult, op1=ALU.add),
                     reads=[ueB, ueH, self.b_cvec], writes=wr)

        def s3(ct):
            cc, ccB = st[ct]["cc"], st[ct]["ccB"]
            S.op("act", lambda: act.activation(out=cc[:, 0:TG], in_=cc[:, 0:TG], func=AF.Silu), reads=[ccB], writes=[ccB])

        def s4(ct):
            cc, ccB = st[ct]["cc"], st[ct]["ccB"]
            at, atB = self.bfpR.next()
            S.op("dve", lambda: dve.tensor_tensor(out=at[:, 0:TG], in0=cc[:, 0:TG], in1=cc[:, TG:2 * TG], op=ALU.mult),
                 reads=[ccB, st[ct]["ccB2"]], writes=[atB])
            st[ct].update(at=at, atB=atB)

        def s5(ct):
            w, wB = self.d_get(0)
            at, atB = st[ct]["at"], st[ct]["atB"]
            for dtile in range(8):
                ab_ = accb[dtile // 2]
                o = ab_.t[:, (dtile % 2) * TG:(dtile % 2 + 1) * TG]
                self.mm(ab_, o, w[:, dtile * 128:(dtile + 1) * 128], at[:, 0:TG],
                        ct == 0, ct == 21, [wB, atB])
            self.d_done()
            del st[ct]

        for i in range(22 + 3):
            if i < 22:
                s1(i)
            if 0 <= i - 1 < 22:
                s3(i - 1)
            if i < 22:
                s2(i)
            if 0 <= i - 1 < 22:
                s4(i - 1)
            if 0 <= i - 3 < 22:
                s5(i - 3)
        self.chk(10)
        for i in range(4):
            S.op("dve", lambda: dve.tensor_tensor(out=self.X[:, 2 * i:2 * i + 2, t0:t0 + TG], in0=self.X[:, 2 * i:2 * i + 2, t0:t0 + TG],
                                                  in1=accb[i].t[:, :].rearrange("p (j t) -> p j t", t=TG), op=ALU.add),
                 writes=[accb[i].buf, self.bX[g]])

    def final_group(self, g):
        S, sp = self.S, self.nc.sync
        t0 = g * TG
        self.rmsnorm(g, CV_NFIN, inplace=True)
        ov = self.d_out.rearrange("(k p) t -> p k t", p=128)
        S.dma("sp", lambda: sp.dma_start(out=ov[:, :, t0:t0 + TG], in_=self.X[:, :, t0:t0 + TG]), "out%d" % (g % 2),
              reads=[self.bX[g]])


def build_program(n_layers=DEPTH, n_groups=NG):
    prog = Prog(n_layers, n_groups)
    s1 = Sched(prog.nc, None)
    prog.emit(s1)
    s2 = Sched(prog.nc, s1.need)
    prog.emit(s2)
    prog.stats = dict(idx=dict(s2.idx), incs=dict(s2.incs), waits=s2.n_wait, sbuf_left=prog.sbuf_left)
    return prog


def _t5_bucket(n):
    n = np.maximum(n, 0)
    nf = np.maximum(n, 1).astype(np.float32)
    large = 16 + (np.log(nf / np.float32(16.0)) / np.float32(math.log(128 / 16)) * np.float32(16)).astype(np.int32)
    large = np.minimum(large, 31)
    return np.where(n < 16, n, large)


def _blk(w, c0, nc_):
    K = w.shape[0] // 128
    return np.ascontiguousarray(w[:, c0:c0 + nc_].reshape(K, 128, nc_).transpose(1, 0, 2)).reshape(128, K * nc_)


def prep_shared(inp, n_layers=DEPTH):
    f32 = np.float32
    L = n_layers
    w_in = np.asarray(inp["w_in"], f32)
    wa = np.zeros((L * NBA, 128, WA_E), f32)
    wf = np.zeros((L * NBF, 128, WSLOT), f32)
    walr = np.zeros((L, 128, 128), f32)
    for l in range(L):
        wi = w_in[l]
        blocks = []
        blocks += [_blk(wi, O_KA + q * 256, 256) for q in range(2)]
        for hp in range(2):
            blocks += [_blk(wi, O_QA + hp * 256, 256), _blk(wi, O_KA + hp * 256, 256)]
        blocks += [_blk(wi, O_VA + q * 256, 256) for q in range(4)]
        blocks += [_blk(wi, O_QB + q * 256, 256) for q in range(2)]
        blocks += [_blk(wi, O_KB + q * 256, 256) for q in range(2)]
        blocks += [_blk(wi, O_VB + q * 256, 256) for q in range(2)]
        blocks += [_blk(wi, O_RA + q * 256, 256) for q in range(4)]
        wbg = np.asarray(inp["w_branch_gla"][l], f32)
        wbm = np.asarray(inp["w_branch_moba"][l], f32)
        for dtile in range(8):
            c0 = dtile * 128
            m = np.zeros((128, WA_E), f32)
            m[:, 0:1024] = _blk(wbg, c0, 128)
            m[:, 1024:1536] = _blk(wbm, c0, 128)
            blocks.append(m)
            m = np.zeros((128, WA_E), f32)
            m[:, 0:1024] = _blk(wi, O_G + c0, 128)
            m[:, 1024:2048] = _blk(wi, O_G + 1024 + c0, 128)
            blocks.append(m)
        wo = np.asarray(inp["w_out"][l], f32)
        blocks += [_blk(wo, q * 256, 256) for q in range(4)]
        assert len(blocks) == NBA
        for i, bb in enumerate(blocks):
            wa[l * NBA + i] = bb
        walr[l] = _blk(wi, O_ALR, 16)
        wu = np.asarray(inp["w_up"][l], f32)
        wd = np.asarray(inp["w_down"][l], f32)
        for ct in range(NBF):
            wf[l * NBF + ct, :, 0:1024] = _blk(wu, ct * 128, 128)
            wf[l * NBF + ct, :, 1024:2048] = _blk(wu, D_FF + ct * 128, 128)
            wf[l * NBF + ct, :, 2048:3072] = wd[ct * 128:(ct + 1) * 128, :]
    wlr = np.zeros((17, DEPTH * 512), f32)
    for l in range(L):
        wlr[0:16, l * 512:(l + 1) * 512] = np.asarray(inp["w_lr_up"][l], f32)
        wlr[16, l * 512:(l + 1) * 512] = np.asarray(inp["b_forget"][l], f32)
    cvec = np.zeros((128, CV_N), f32)
    for l in range(L):
        cvec[:, CV_NMIX + l * 8:CV_NMIX + (l + 1) * 8] = np.asarray(inp["norm_mix"][l], f32).reshape(8, 128).T
        cvec[:, CV_NFFN + l * 8:CV_NFFN + (l + 1) * 8] = np.asarray(inp["norm_ffn"][l], f32).reshape(8, 128).T
        cvec[:, CV_GLA + l * 8:CV_GLA + (l + 1) * 8] = np.asarray(inp["gla_out_norm"][l], f32).reshape(8, 128).T
        cw = np.asarray(inp["conv_w"][l], f32)
        cb = np.asarray(inp["conv_b"][l], f32)
        for ab in range(2):
            full = np.concatenate([cw[:, ab * D_FF:(ab + 1) * D_FF], cb[None, ab * D_FF:(ab + 1) * D_FF]], axis=0)
            arr = full.reshape(4, 22, 128).transpose(2, 1, 0)
            c0 = CV_CW + (l * 2 + ab) * 22 * 4
            cvec[:, c0:c0 + 88] = arr.reshape(128, 88)
    cvec[:, CV_NFIN:CV_NFIN + 8] = np.asarray(inp["norm_final"], f32).reshape(8, 128).T
    rb = np.asarray(inp["rel_bias"], f32)
    kk = np.arange(128)[:, None]
    qq = np.arange(128)[None, :]
    cbias = np.zeros((128, 8, 512), f32)
    bd = _t5_bucket(qq - kk)
    bs = _t5_bucket(128 + qq - kk)
    for hd in range(8):
        diag = rb[bd, hd]
        cbias[:, hd, 0:128] = np.where(qq >= kk, diag, np.float32(NEG))
        cbias[:, hd, 128:256] = rb[bs, hd]
        cbias[:, hd, 256:512] = rb[31, hd]
    cmisc = np.zeros((128, CM_N), f32)
    cmisc[:, CM_B31:CM_B31 + 64] = np.repeat(rb[31, :], 8)[None, :]
    for g in range(8):
        m = np.where(np.arange(8) < g, 0.0, -1e30).astype(f32)
        cmisc[:, CM_NEG + g * 64:CM_NEG + (g + 1) * 64] = np.tile(m, 8)[None, :]
    cmat = np.zeros((128, CMAT_N), f32)
    s = np.arange(128)[:, None]
    t = np.arange(128)[None, :]
    cmat[:, 0:128] = np.eye(128, dtype=f32)
    cmat[:, 128:256] = 1.0 / 1024.0
    cmat[:, 256:384] = (s <= t).astype(f32)
    cmat[:, 384:512] = np.where(s <= t, -1.0 / 16.0, 0.0)
    cmat[:, 512:640] = np.where(s > t, -1.0 / 16.0, 0.0)
    return dict(wa=wa, wf=wf, walr=walr, wlr=wlr, cvec=cvec, cbias=cbias.reshape(128, 8 * 512), cmisc=cmisc, cmat=cmat)


_PROG_CACHE = {}


def kernel(x, rel_bias, norm_mix, w_in, w_lr_up, b_forget, gla_out_norm, w_branch_gla, w_branch_moba, w_out,
           norm_ffn, w_up, conv_w, conv_b, w_down, norm_final):
    inp = dict(rel_bias=rel_bias, norm_mix=norm_mix, w_in=w_in, w_lr_up=w_lr_up, b_forget=b_forget,
               gla_out_norm=gla_out_norm, w_branch_gla=w_branch_gla, w_branch_moba=w_branch_moba, w_out=w_out,
               norm_ffn=norm_ffn, w_up=w_up, conv_w=conv_w, conv_b=conv_b, w_down=w_down, norm_final=norm_final)
    x = np.asarray(x, np.float32)
    Bn = x.shape[0]
    shared = prep_shared(inp)
    prog = build_program()
    in_maps = []
    for b in range(Bn):
        m = dict(shared)
        m["xT"] = np.ascontiguousarray(x[b].T)
        in_maps.append(m)
    res = run_bass_kernel_spmd(prog.nc, in_maps, core_ids=list(range(Bn)))
    out = np.stack([np.ascontiguousarray(r["outT"].T) for r in res.results], axis=0)
    return out.astype(np.float32)
```

```python
import math
import os
import numpy as np
import concourse.bass as bass
import concourse.mybir as mybir
from concourse.bass_utils import run_bass_kernel_spmd

F32 = mybir.dt.float32
BF16 = mybir.dt.bfloat16
AF = mybir.ActivationFunctionType
ALU = mybir.AluOpType

D = 1024
T = 2048
DEPTH = 4
TG = 256
NG = T // TG
D_FF = 2816
EPS = 1e-6
NEG = -30000.0
NBA = 40
NBF = 22
WA_E = 2048
WSLOT = 3072
NSLOT = 5
NDSLOT = 6
O_QA, O_KA, O_VA, O_RA, O_ALR, O_QB, O_KB, O_VB, O_G = 0, 512, 1024, 2048, 3072, 3088, 3600, 4112, 4624

CV_NMIX = 0
CV_NFFN = DEPTH * 8
CV_GLA = 2 * DEPTH * 8
CV_NFIN = 3 * DEPTH * 8
CV_CW = 3 * DEPTH * 8 + 8
CV_N = CV_CW + DEPTH * 2 * 22 * 4
CM_B31 = 0
CM_NEG = 64
CM_N = 64
CMAT_N = 5 * 128


class Buf:
    __slots__ = ("name", "w", "r")

    def __init__(self, name):
        self.name = name
        self.w = {}
        self.r = {}


class Sched:
    ENGS = ("pe", "act", "dve", "pool", "sp")

    def __init__(self, nc, plan):
        self.nc = nc
        self.plan = plan
        self.real = plan is not None
        self.idx = {e: 0 for e in self.ENGS}
        self.incs = {e: 0 for e in self.ENGS}
        self.val = {}
        self.waited = {e: {} for e in self.ENGS}
        self.need = set()
        self.dcount = {}
        self.dsem = {}
        self.n_wait = 0
        if self.real:
            self.eng = {"pe": nc.tensor, "act": nc.scalar, "dve": nc.vector, "pool": nc.gpsimd, "sp": nc.sync}
            self.esem = {e: nc.alloc_semaphore("sem_" + e) for e in self.ENGS}

    def _dsem(self, name):
        if name not in self.dsem:
            self.dsem[name] = self.nc.alloc_semaphore("dsem_" + name) if self.real else None
            self.dcount[name] = 0
        return self.dsem[name]

    def _wait(self, eng, t):
        if t[0] == "e":
            _, f, i = t
            if f == eng:
                if eng == "pe" or self.idx[eng] - i > 6:
                    return
            if self.waited[eng].get(f, -1) >= i:
                return
            self.waited[eng][f] = i
            self.need.add((f, i))
            if self.real:
                self.eng[eng].wait_ge(self.esem[f], self.val[(f, i)])
                self.n_wait += 1
        else:
            _, name, n = t
            key = ("d", name)
            if self.waited[eng].get(key, 0) >= n:
                return
            self.waited[eng][key] = n
            if self.real:
                self.eng[eng].wait_ge(self.dsem[name], 16 * n)
                self.n_wait += 1

    def _deps(self, eng, reads, writes):
        for b in reads:
            for t in b.w.values():
                self._wait(eng, t)
        for b in writes:
            for t in b.w.values():
                self._wait(eng, t)
            for t in b.r.values():
                self._wait(eng, t)

    def op(self, eng, fn, reads=(), writes=()):
        self._deps(eng, reads, writes)
        i = self.idx[eng]
        if self.real:
            ins = fn()
            if (eng, i) in self.plan:
                self.incs[eng] += 1
                ins.then_inc(self.esem[eng], 1)
                self.val[(eng, i)] = self.incs[eng]
        self.idx[eng] = i + 1
        t = ("e", eng, i)
        for b in writes:
            b.w[eng] = t
        for b in reads:
            b.r[eng] = t
        return t

    def dma(self, q, fn, sem, reads=(), writes=()):
        self._deps(q, reads, writes)
        self._dsem(sem)
        self.dcount[sem] += 1
        n = self.dcount[sem]
        if self.real:
            fn().then_inc(self.dsem[sem], 16)
        self.idx[q] += 1
        t = ("d", sem, n)
        for b in writes:
            b.w["d:" + sem] = t
        for b in reads:
            b.r["d:" + sem] = t
        return t

    def wait_all_dma(self, eng, sem):
        self._wait(eng, ("d", sem, self.dcount[sem]))


class StopEmit(Exception):
    pass


class Bank:
    def __init__(self, t, i):
        self.t = t
        self.buf = Buf("bank%d" % i)
        self.fresh = True


class Rot:
    def __init__(self, items):
        self.items = items
        self.i = 0

    def next(self):
        it = self.items[self.i % len(self.items)]
        self.i += 1
        return it


class Prog:
    def __init__(self, n_layers=DEPTH, n_groups=NG):
        self.L = n_layers
        self.NGR = n_groups
        nc = bass.Bass("TRN2", target_bir_lowering=False)
        self.nc = nc
        L = n_layers
        dt = nc.dram_tensor
        self.d_x = dt("xT", [D, T], F32, kind="ExternalInput").ap()
        self.d_wa = dt("wa", [L * NBA, 128, WA_E], F32, kind="ExternalInput").ap()
        self.d_wf = dt("wf", [L * NBF, 128, WSLOT], F32, kind="ExternalInput").ap()
        self.d_walr = dt("walr", [L, 128, 128], F32, kind="ExternalInput").ap()
        self.d_wlr = dt("wlr", [17, DEPTH * 512], F32, kind="ExternalInput").ap()
        self.d_cvec = dt("cvec", [128, CV_N], F32, kind="ExternalInput").ap()
        self.d_cbias = dt("cbias", [128, 8 * 512], F32, kind="ExternalInput").ap()
        self.d_cmisc = dt("cmisc", [128, CM_N], F32, kind="ExternalInput").ap()
        self.d_cmat = dt("cmat", [128, CMAT_N], F32, kind="ExternalInput").ap()
        self.d_out = dt("outT", [D, T], F32, kind="ExternalOutput").ap()
        self.d_wa16 = dt("wa16", [L * NBA, 128, WA_E], BF16).ap()
        self.d_wf16 = dt("wf16", [L * NBF, 128, WSLOT], BF16).ap()
        self.d_walr16 = dt("walr16", [L, 128, 128], BF16).ap()

        A = nc.alloc_sbuf_tensor
        self.X = A("X", [128, 8, T], F32)
        self.KT = A("KT", [128, 4, T], BF16)
        self.Vaug = A("Vaug", [128, 16, 8, 65], BF16)
        self.Sf = A("Sf", [128, 4, 256], F32)
        self.Sbf = A("Sbf", [128, 4, 256], BF16)
        self.wslot = [A("wslot%d" % i, [128, WA_E], BF16) for i in range(NSLOT)]
        self.dslot = [A("dslot%d" % i, [128, 1024], BF16) for i in range(NDSLOT)]
        self.alrw = [A("alrw%d" % i, [128, 128], BF16) for i in range(2)]
        self.h = A("h", [128, 8, TG], BF16)
        self.rstd = A("rstd", [128, TG], F32)
        self.qdec = A("qdec", [128, 4, TG], BF16)
        self.kdec = A("kdec", [128, 4, TG], BF16)
        self.kte = A("kte", [128, 2, 512], BF16)
        self.vtm = A("vtm", [128, 2, 1024], BF16)
        self.sra = A("sra", [128, 8, TG], BF16)
        self.QTz = A("QTz", [128, 8, TG], BF16)
        self.alrT = A("alrT", [17, TG], BF16)
        self.lhi = A("lhi", [128, 2, 512], BF16)
        self.llo = A("llo", [128, 2, 512], BF16)
        self.obtm = self.lhi
        self.mixed = self.sra
        self.obT = self.llo[:, :, :].rearrange("p a (b t) -> p (a b) t", t=TG)
        self.oaT = A("oaT", [128, 8, TG], BF16)
        self.accO = A("accO", [128, 2, 8, 65], F32)
        self.gsb = A("gsb", [128, 128], F32)
        self.sel = A("sel", [128, 128], F32)
        self.selw = A("selw", [128, 128], F32)
        self.top8 = A("top8", [128, 16, 8], F32)
        self.smalls = A("smalls", [128, 64], F32)
        self.ksb = A("ksb", [128, 4, 8], BF16)
        self.carry = A("carry", [128, 2, 22, 2], F32)
        self.bigf = [A("bigf%d" % i, [128, 512], F32) for i in range(3)]
        self.smf = [A("smf%d" % i, [128, 2, 258], F32) for i in range(3)]
        self.bfp = [A("bfp%d" % i, [128, 512], BF16) for i in range(4)]
        self.g_at = A("g_at", [128, 4, 128], BF16)
        self.g_oa = A("g_oa", [128, 4, 256], BF16)
        self.cvec = A("cvec_s", [128, CV_N], F32)
        self.cbias = A("cbias_s", [128, 8, 512], BF16)
        self.cmisc = A("cmisc_s", [128, CM_N], F32)
        self.cmat = A("cmat_s", [128, CMAT_N], BF16)
        self.wlr = A("wlr_s", [17, 512], BF16)
        self.expb31 = A("expb31", [128, 64], F32)
        self.banks_t = [nc.alloc_psum_tensor("bank%d" % i, [128, 512], F32) for i in range(8)]
        self.sbuf_left = nc.sbuf_bytes_remaining

    def emit(self, S):
        self.S = S
        nc = self.nc
        self.banks = [Bank(t, i) for i, t in enumerate(self.banks_t)]
        self.P = Rot(self.banks)
        self.bigfR = Rot([(t, [Buf("bigf%da" % i), Buf("bigf%db" % i)]) for i, t in enumerate(self.bigf)])
        self.smfR = Rot([(t, Buf("smf%d" % i), Buf("smfh%d" % i)) for i, t in enumerate(self.smf)])
        bfl = [(t, Buf("bfp%d" % i)) for i, t in enumerate(self.bfp)]
        self.bfpR = Rot(bfl[0:4])
        self.b_gat = [Buf("gat%d" % i) for i in range(4)]
        self.b_goa = [Buf("goa%d" % i) for i in range(4)]
        B = Buf
        self.bX = [B("X%d" % g) for g in range(NG)]
        self.bKT = [B("KT%d" % g) for g in range(NG)]
        self.bV = [B("V%d" % g) for g in range(NG)]
        self.bS = [B("S%d" % i) for i in range(4)]
        self.bSb = [B("Sb%d" % i) for i in range(4)]
        self.bW = [B("wslot%d" % i) for i in range(NSLOT)]
        self.bD = [B("dslot%d" % i) for i in range(NDSLOT)]
        self.bAlrw = [B("alrw0"), B("alrw1")]
        for n in ("h", "rstd", "qdec", "kdec", "kte", "vtm", "sra", "QTz", "alrT", "lhi", "llo", "eTM", "oaT",
                  "obtm", "obT", "mixed", "accO", "gsb", "sel", "selw", "top8", "dec", "ss", "ss2", "rc", "ksumf",
                  "ksb", "carry", "cvec", "cbias", "cmisc", "cmat", "wlr", "expb31"):
            setattr(self, "b_" + n, B(n))
        self.b_obtm = self.b_lhi
        self.b_mixed = self.b_sra
        self.b_obT = self.b_llo
        self.wlist = []
        for l in range(self.L):
            for g in range(self.NGR):
                self.wlist.append(("alr", l))
                for i in range(NBA):
                    self.wlist.append(("a", l * NBA + i))
                for i in range(NBF):
                    self.wlist.append(("f", l * NBF + i))
                    self.wlist.append(("d", l * NBF + i))
        self.w_issued = 0
        self.w_used = 0
        self.alr_n = 0
        self.big_n = 0
        self.d_used = 0
        self.d_n = 0

        self.stop = int(os.environ.get("KSTOP", "99"))
        self.phases = []
        self.init_phase()
        try:
            for l in range(self.L):
                self.layer_init(l)
                for g in range(self.NGR):
                    self.mixer_group(l, g)
                    self.ffn_group(l, g)
                    if l == self.L - 1:
                        self.final_group(g)
        except StopEmit:
            self.final_group(0)
        for sem in ["w%d" % i for i in range(NSLOT)] + ["d%d" % i for i in range(NDSLOT)] + ["alr0", "alr1", "c_wlr", "c_bias", "c_mat", "c_vec", "c_misc"] + ["cv%d" % i for i in range(DEPTH)] + ["cv0_alr", "cv0_a0", "cv0_a1", "cv0_a2", "cv0_a3", "cv0_f0", "cv0_f1"]:
            if sem in S.dcount:
                S.wait_all_dma("sp", sem)
        for sem in ("out0", "out1"):
            if sem in S.dcount:
                S.wait_all_dma("sp", sem)

    def w_issue(self):
        S = self.S
        while self.w_issued < len(self.wlist):
            kind, idx = self.wlist[self.w_issued]
            if kind == "alr":
                j = self.alr_n % 2
                tl, bf, src = self.alrw[j], self.bAlrw[j], self.d_walr16[idx]
                S.dma("sp", lambda: self.nc.sync.dma_start(out=tl[:], in_=src), "alr%d" % j, reads=[self.cvt[("alr", idx)]], writes=[bf])
                self.alr_n += 1
            elif kind == "d":
                if self.d_n >= self.d_used + NDSLOT:
                    return
                j = self.d_n % NDSLOT
                tl, bf, src = self.dslot[j], self.bD[j], self.d_wf16[idx][:, 2048:3072]
                S.dma("sp", lambda: self.nc.sync.dma_start(out=tl[:], in_=src), "d%d" % j, reads=[self.cvt[("f", idx)]], writes=[bf])
                self.d_n += 1
            else:
                if self.big_n >= self.w_used + NSLOT:
                    return
                j = self.big_n % NSLOT
                tl, bf = self.wslot[j], self.bW[j]
                if kind == "a":
                    src = self.d_wa16[idx]
                    S.dma("sp", lambda: self.nc.sync.dma_start(out=tl[:], in_=src), "w%d" % j,
                          reads=[self.cvt[("a", idx)]], writes=[bf])
                else:
                    src = self.d_wf16[idx][:, 0:2048]
                    S.dma("sp", lambda: self.nc.sync.dma_start(out=tl[:], in_=src), "w%d" % j,
                          reads=[self.cvt[("f", idx)]], writes=[bf])
                self.big_n += 1
            self.w_issued += 1

    def w_get(self, off=0):
        assert self.big_n > self.w_used + off, "weight block not issued (ring too small for this access pattern)"
        j = (self.w_used + off) % NSLOT
        return self.wslot[j], self.bW[j]

    def w_done(self):
        self.w_used += 1
        self.w_issue()

    def d_get(self, off=0):
        assert self.d_n > self.d_used + off, "down block not issued"
        j = (self.d_used + off) % NDSLOT
        return self.dslot[j], self.bD[j]

    def d_done(self):
        self.d_used += 1
        self.w_issue()

    def mm(self, bank, out, lhsT, rhs, first, last, reads):
        st = bool(first and bank.fresh)
        if first:
            bank.fresh = False
        self.S.op("pe", lambda: self.nc.tensor.matmul(out, lhsT, rhs, start=st, stop=bool(last), skip_group_check=True),
                  reads=reads, writes=[bank.buf])

    def chk(self, p):
        if self.S.real:
            self.phases.append((p, self.S.idx["pe"]))
        if p >= self.stop:
            raise StopEmit()

    def bank(self):
        b = self.P.next()
        b.fresh = True
        return b

    def init_phase(self):
        S, nc = self.S, self.nc
        act, dve, sp, pool = nc.scalar, nc.vector, nc.sync, nc.gpsimd
        S.dma("sp", lambda: sp.dma_start(out=self.cvec[:], in_=self.d_cvec), "c_vec", writes=[self.b_cvec])
        S.dma("sp", lambda: sp.dma_start(out=self.cmisc[:], in_=self.d_cmisc), "c_misc", writes=[self.b_cmisc])
        S.dma("pool", lambda: pool.dma_start(out=self.cmat[:], in_=self.d_cmat), "c_mat", writes=[self.b_cmat])
        S.dma("pool", lambda: pool.dma_start(out=self.cbias[:], in_=self.d_cbias.rearrange("p (h c) -> p h c", c=512)),
              "c_bias", writes=[self.b_cbias])
        xv = self.d_x.rearrange("(k p) t -> p k t", p=128)
        for g in range(self.NGR):
            S.dma("sp", lambda: sp.dma_start(out=self.X[:, :, g * TG:(g + 1) * TG], in_=xv[:, :, g * TG:(g + 1) * TG]),
                  "x%d" % g, writes=[self.bX[g]])
        self.cvt = {}
        b0 = Buf("cvt0_alr")
        S.dma("pool", lambda: pool.dma_start(out=self.d_walr16[0], in_=self.d_walr[0]), "cv0_alr", writes=[b0])
        self.cvt[("alr", 0)] = b0
        for c in range(4):
            bc = Buf("cvt0_a%d" % c)
            S.dma("pool", lambda: pool.dma_start(out=self.d_wa16[c * 10:(c + 1) * 10], in_=self.d_wa[c * 10:(c + 1) * 10]),
                  "cv0_a%d" % c, writes=[bc])
            for i in range(c * 10, (c + 1) * 10):
                self.cvt[("a", i)] = bc
        for c in range(2):
            bc = Buf("cvt0_f%d" % c)
            S.dma("pool", lambda: pool.dma_start(out=self.d_wf16[c * 11:(c + 1) * 11], in_=self.d_wf[c * 11:(c + 1) * 11]),
                  "cv0_f%d" % c, writes=[bc])
            for i in range(c * 11, (c + 1) * 11):
                self.cvt[("f", i)] = bc
        self.w_issue()
        S.op("dve", lambda: dve.memset(self.Vaug[:], 1.0), writes=self.bV)
        S.op("dve", lambda: dve.memset(self.alrT[:], 1.0), writes=[self.b_alrT])
        S.op("dve", lambda: dve.memset(self.QTz[:], 0.0), writes=[self.b_QTz])
        S.op("dve", lambda: dve.memset(self.ksb[:], 0.0), writes=[self.b_ksb])
        S.op("act", lambda: act.activation(out=self.expb31[:], in_=self.cmisc[:, CM_B31:CM_B31 + 64], func=AF.Exp),
             reads=[self.b_cmisc], writes=[self.b_expb31])
        self.ident = self.cmat[:, 0:128]
        self.ones_m = self.cmat[:, 128:256]
        self.maskU = self.cmat[:, 256:384]
        self.Mle = self.cmat[:, 384:512]
        self.Mgt = self.cmat[:, 512:640]

    def layer_init(self, l):
        S, dve = self.S, self.nc.vector
        S.dma("pool", lambda: self.nc.gpsimd.dma_start(out=self.wlr[:], in_=self.d_wlr[:, l * 512:(l + 1) * 512]), "c_wlr",
              writes=[self.b_wlr])
        S.op("dve", lambda: dve.memset(self.Sf[:], 0.0), writes=self.bS)
        S.op("dve", lambda: dve.memset(self.Sbf[:], 0.0), writes=self.bSb)
        S.op("dve", lambda: dve.memset(self.carry[:], 0.0), writes=[self.b_carry])

    def rmsnorm(self, g, col0, inplace=False):
        S, nc = self.S, self.nc
        act, dve = nc.scalar, nc.vector
        t0 = g * TG
        Xg = self.X[:, :, t0:t0 + TG]
        S.op("act", lambda: act.activation(out=self.h[:], in_=Xg, func=AF.Square), reads=[self.bX[g]], writes=[self.b_h])
        bk = self.bank()
        for k in range(8):
            self.mm(bk, bk.t[:, 0:TG], self.ones_m, self.h[:, k, :], k == 0, k == 7, [self.b_h, self.b_cmat])
        S.op("act", lambda: act.activation(out=self.rstd[:], in_=bk.t[:, 0:TG], func=AF.Ln, bias=EPS),
             writes=[bk.buf, self.b_rstd])
        S.op("act", lambda: act.activation(out=self.rstd[:], in_=self.rstd[:], func=AF.Exp, scale=-0.5),
             reads=[self.b_rstd], writes=[self.b_rstd])
        for k in range(8):
            if inplace:
                S.op("dve", lambda: dve.scalar_tensor_tensor(out=self.X[:, k, t0:t0 + TG], in0=self.X[:, k, t0:t0 + TG],
                                                             scalar=self.cvec[:, col0 + k:col0 + k + 1], in1=self.rstd[:],
                                                             op0=ALU.mult, op1=ALU.mult),
                     reads=[self.b_rstd, self.b_cvec], writes=[self.bX[g]])
            else:
                S.op("dve", lambda: dve.scalar_tensor_tensor(out=self.h[:, k, :], in0=self.X[:, k, t0:t0 + TG],
                                                             scalar=self.cvec[:, col0 + k:col0 + k + 1], in1=self.rstd[:],
                                                             op0=ALU.mult, op1=ALU.mult),
                     reads=[self.bX[g], self.b_rstd, self.b_cvec], writes=[self.b_h])

    def convert_layer(self, l):
        S, pool = self.S, self.nc.gpsimd
        bc = Buf("cvt%d" % l)
        gate = [self.bX[0]]
        S.dma("pool", lambda: pool.dma_start(out=self.d_walr16[l], in_=self.d_walr[l]), "cv%d" % l, reads=gate, writes=[bc])
        S.dma("pool", lambda: pool.dma_start(out=self.d_wa16[l * NBA:(l + 1) * NBA], in_=self.d_wa[l * NBA:(l + 1) * NBA]),
              "cv%d" % l, reads=gate, writes=[bc])
        S.dma("pool", lambda: pool.dma_start(out=self.d_wf16[l * NBF:(l + 1) * NBF], in_=self.d_wf[l * NBF:(l + 1) * NBF]),
              "cv%d" % l, reads=gate, writes=[bc])
        self.cvt[("alr", l)] = bc
        for i in range(l * NBA, (l + 1) * NBA):
            self.cvt[("a", i)] = bc
        for i in range(l * NBF, (l + 1) * NBF):
            self.cvt[("f", i)] = bc

    def mixer_group(self, l, g):
        S, nc = self.S, self.nc
        act, dve = nc.scalar, nc.vector
        t0 = g * TG
        if g == min(1, self.NGR - 1) and l + 1 < self.L:
            self.convert_layer(l + 1)
        h, hB = self.h, self.b_h
        self.rmsnorm(g, CV_NMIX + l * 8)
        self.chk(0)

        ja = (l * self.NGR + g) % 2
        alrw, alrwB = self.alrw[ja], self.bAlrw[ja]
        bk = self.bank()
        for k in range(8):
            self.mm(bk, bk.t[0:16, 0:TG], alrw[:, k * 16:(k + 1) * 16], h[:, k, :], k == 0, k == 7, [alrwB, hB])
        S.op("act", lambda: act.activation(out=self.alrT[0:16, :], in_=bk.t[0:16, 0:TG], func=AF.Copy),
             writes=[bk.buf, self.b_alrT])
        eTMs = []
        for a in range(2):
            bk = self.bank()
            self.mm(bk, bk.t[:, :], self.alrT[:, a * 128:(a + 1) * 128], self.wlr[:, :], True, True,
                    [self.b_alrT, self.b_wlr])
            e, eB = self.bigfR.next()
            S.op("act", lambda: act.activation(out=e[:], in_=bk.t[:, :], func=AF.Exp, scale=-1.0), writes=[bk.buf] + eB)
            S.op("act", lambda: act.activation(out=e[:], in_=e[:], func=AF.Ln, bias=1.0), reads=eB, writes=eB)
            S.op("dve", lambda: dve.tensor_copy(out=self.lhi[:, a, :], in_=e[:]), reads=eB, writes=[self.b_lhi])
            S.op("dve", lambda: dve.tensor_tensor(out=self.llo[:, a, :], in0=e[:], in1=self.lhi[:, a, :], op=ALU.subtract),
                 reads=eB + [self.b_lhi], writes=[self.b_llo])
            bk2 = self.bank()
            self.mm(bk2, bk2.t[:, :], self.Mgt, self.lhi[:, a, :], True, False, [self.b_cmat, self.b_lhi])
            self.mm(bk2, bk2.t[:, :], self.Mgt, self.llo[:, a, :], False, True, [self.b_cmat, self.b_llo])
            et, etB = self.bigfR.next()
            S.op("act", lambda: act.activation(out=et[:], in_=bk2.t[:, :], func=AF.Exp), writes=[bk2.buf] + etB)
            eTMs.append((et, etB))

        self.chk(1)
        def tm_pair(nblk):
            bks = [self.bank(), self.bank()]
            for q in range(nblk):
                w, wB = self.w_get()
                for a in range(2):
                    for k in range(8):
                        self.mm(bks[a], bks[a].t[:, q * 256:(q + 1) * 256], h[:, k, a * 128:(a + 1) * 128], w[:, k * 256:(k + 1) * 256],
                                k == 0, k == 7, [wB, hB])
                self.w_done()
            return bks

        bks = tm_pair(2)
        for a in range(2):
            bk = bks[a]
            et, etB = eTMs[a]
            S.op("dve", lambda: dve.tensor_tensor(out=self.kte[:, a, :], in0=bk.t[:, :], in1=et[:], op=ALU.mult),
                 reads=etB, writes=[bk.buf, self.b_kte])
        for hp in range(2):
            wq, wqB = self.w_get(0)
            wk, wkB = self.w_get(1)
            for hh in (2 * hp, 2 * hp + 1):
                c = hh % 2
                bkb = self.bank()
                for a in range(2):
                    o = bkb.t[:, a * 128:(a + 1) * 128]
                    self.mm(bkb, o, self.lhi[:, a, hh * 128:(hh + 1) * 128], self.Mle, True, False, [self.b_lhi, self.b_cmat])
                    self.mm(bkb, o, self.llo[:, a, hh * 128:(hh + 1) * 128], self.Mle, False, True, [self.b_llo, self.b_cmat])
                ebt, ebBs = self.bigfR.next()
                eb = ebt[:, 0:TG]
                enb = ebt[:, TG:2 * TG]
                S.op("act", lambda: act.activation(out=eb, in_=bkb.t[:, 0:TG], func=AF.Exp), writes=[bkb.buf] + ebBs)
                S.op("act", lambda: act.activation(out=enb, in_=bkb.t[:, 0:TG], func=AF.Exp, scale=-1.0), writes=[bkb.buf] + ebBs)
                for a in range(2):
                    S.op("dve", lambda: dve.tensor_copy(out=self.smalls[:, hh * 2 + a:hh * 2 + a + 1],
                                                        in_=ebt[:, a * 128 + 127:a * 128 + 128]),
                         reads=ebBs, writes=[self.b_dec])
                bq = self.bank()
                for k in range(8):
                    self.mm(bq, bq.t[:, 0:TG], wq[:, k * 256 + c * 128:k * 256 + (c + 1) * 128], h[:, k, :], k == 0, k == 7, [wqB, hB])
                for k in range(8):
                    self.mm(bq, bq.t[:, TG:2 * TG], wk[:, k * 256 + c * 128:k * 256 + (c + 1) * 128], h[:, k, :], k == 0, k == 7, [wkB, hB])
                S.op("dve", lambda: dve.scalar_tensor_tensor(out=self.qdec[:, hh, :], in0=bq.t[:, 0:TG], scalar=128.0 ** -0.5, in1=eb,
                                                             op0=ALU.mult, op1=ALU.mult),
                     reads=ebBs, writes=[bq.buf, self.b_qdec])
                S.op("dve", lambda: dve.tensor_tensor(out=self.kdec[:, hh, :], in0=bq.t[:, TG:2 * TG], in1=enb, op=ALU.mult),
                     reads=ebBs, writes=[bq.buf, self.b_kdec])
            self.w_done()
            self.w_done()

        self.chk(2)

        for half in range(2):
            bks = tm_pair(2)
            for a in range(2):
                bk = bks[a]
                S.op("act", lambda: act.activation(out=self.vtm[:, a, half * 512:(half + 1) * 512], in_=bk.t[:, :], func=AF.Copy),
                     writes=[bk.buf, self.b_vtm])
        self.chk(3)
        for pr2 in range(2):
            w, wB = self.w_get()
            bk = self.bank()
            for j in range(2):
                for k in range(8):
                    self.mm(bk, bk.t[:, j * TG:(j + 1) * TG], w[:, k * 256 + j * 128:k * 256 + (j + 1) * 128], h[:, k, :],
                            k == 0, k == 7, [wB, hB])
            for j in range(2):
                p = 2 * pr2 + j
                S.op("act", lambda: act.activation(out=self.QTz[0:64, 2 * p, :], in_=bk.t[0:64, j * TG:(j + 1) * TG],
                                                   func=AF.Copy, scale=0.125), writes=[bk.buf, self.b_QTz])
                S.op("act", lambda: act.activation(out=self.QTz[64:128, 2 * p + 1, :], in_=bk.t[64:128, j * TG:(j + 1) * TG],
                                                   func=AF.Copy, scale=0.125), writes=[bk.buf, self.b_QTz])
            self.w_done()
        S.op("dve", lambda: dve.memset(self.smalls[:, 32:64], 0.0), writes=[self.b_ksumf])
        for pr2 in range(2):
            w, wB = self.w_get()
            bk = self.bank()
            for j in range(2):
                for k in range(8):
                    self.mm(bk, bk.t[:, j * TG:(j + 1) * TG], w[:, k * 256 + j * 128:k * 256 + (j + 1) * 128], h[:, k, :],
                            k == 0, k == 7, [wB, hB])
            for j in range(2):
                p = 2 * pr2 + j
                S.op("act", lambda: act.activation(out=self.KT[:, p, t0:t0 + TG], in_=bk.t[:, j * TG:(j + 1) * TG], func=AF.Copy,
                                                   accum_out=self.smalls[:, 32 + p:33 + p]),
                     writes=[bk.buf, self.bKT[g], self.b_ksumf])
            self.w_done()
        S.op("dve", lambda: dve.tensor_copy(out=self.ksb[:, :, g], in_=self.smalls[:, 32:36]),
             reads=[self.b_ksumf], writes=[self.b_ksb])
        bks = tm_pair(2)
        for a in range(2):
            bk = bks[a]
            S.op("act", lambda: act.activation(out=self.Vaug[:, 2 * g + a, :, 0:64],
                                               in_=bk.t[:, :].rearrange("p (h d) -> p h d", d=64), func=AF.Copy),
                 writes=[bk.buf, self.bV[g]])
        self.chk(4)
        for i in range(4):
            w, wB = self.w_get()
            bk = self.bank()
            for j in range(2):
                for k in range(8):
                    self.mm(bk, bk.t[:, j * TG:(j + 1) * TG], w[:, k * 256 + j * 128:k * 256 + (j + 1) * 128], h[:, k, :],
                            k == 0, k == 7, [wB, hB])
            S.op("act", lambda: act.activation(out=self.sra[:, 2 * i:2 * i + 2, :],
                                               in_=bk.t[:, :].rearrange("p (j t) -> p j t", t=TG), func=AF.Silu),
                 writes=[bk.buf, self.b_sra])
            self.w_done()

        self.chk(5)
        self.gla_moba_group(l, g)
        self.chk(7)

        for dtile in range(8):
            wA, wAB = self.w_get(0)
            wG, wGB = self.w_get(1)
            b1 = self.bank()
            for c in range(8):
                self.mm(b1, b1.t[:, 0:TG], wA[:, c * 128:(c + 1) * 128], self.oaT[:, c, :], c == 0, c == 7, [wAB, self.b_oaT])
            for c in range(4):
                self.mm(b1, b1.t[:, TG:2 * TG], wA[:, 1024 + c * 128:1024 + (c + 1) * 128], self.obT[:, c, :], c == 0, c == 3,
                        [wAB, self.b_obT])
            b2 = self.bank()
            for k in range(8):
                self.mm(b2, b2.t[:, 0:TG], wG[:, k * 128:(k + 1) * 128], h[:, k, :], k == 0, k == 7, [wGB, hB])
            for k in range(8):
                self.mm(b2, b2.t[:, TG:2 * TG], wG[:, 1024 + k * 128:1024 + (k + 1) * 128], h[:, k, :], k == 0, k == 7, [wGB, hB])
            sg, sgB = self.bigfR.next()
            S.op("act", lambda: act.activation(out=sg[:], in_=b2.t[:, :], func=AF.Sigmoid), writes=[b2.buf] + sgB)
            S.op("dve", lambda: dve.tensor_tensor(out=sg[:], in0=b1.t[:, :], in1=sg[:], op=ALU.mult), writes=[b1.buf] + sgB)
            S.op("dve", lambda: dve.tensor_tensor(out=self.mixed[:, dtile, :], in0=sg[:, 0:TG], in1=sg[:, TG:2 * TG], op=ALU.add),
                 reads=sgB, writes=[self.b_mixed])
            self.w_done()
            self.w_done()
        self.chk(8)
        for i in range(4):
            w, wB = self.w_get()
            bk = self.bank()
            for j in range(2):
                for k in range(8):
                    self.mm(bk, bk.t[:, j * TG:(j + 1) * TG], w[:, k * 256 + j * 128:k * 256 + (j + 1) * 128], self.mixed[:, k, :],
                            k == 0, k == 7, [wB, self.b_mixed])
            d0 = 2 * i
            S.op("dve", lambda: dve.tensor_tensor(out=self.X[:, d0:d0 + 2, t0:t0 + TG], in0=self.X[:, d0:d0 + 2, t0:t0 + TG],
                                                  in1=bk.t[:, :].rearrange("p (j t) -> p j t", t=TG), op=ALU.add),
                 writes=[bk.buf, self.bX[g]])
            self.w_done()

    def gla_group(self, l, g, mid_hook=None):
        S, nc = self.S, self.nc
        act, dve, pe = nc.scalar, nc.vector, nc.tensor
        S.op("dve", lambda: dve.memset(self.smalls[:, 8:16], 0.0), writes=[self.b_ss])
        H4 = range(4)
        for a in range(2):
            sl = slice(a * 128, (a + 1) * 128)
            vs = [self.vtm[:, a, hh * 256:(hh + 1) * 256] for hh in H4]
            bA = []
            for hh in H4:
                bk = self.bank()
                bA.append(bk)
                self.mm(bk, bk.t[:, 0:128], self.kdec[:, hh, sl], self.qdec[:, hh, sl], True, True, [self.b_kdec, self.b_qdec])
            for hh in H4:
                S.op("dve", lambda: dve.tensor_tensor(out=self.g_at[:, hh, :], in0=bA[hh].t[:, 0:128], in1=self.maskU, op=ALU.mult),
                     reads=[self.b_cmat], writes=[bA[hh].buf, self.b_gat[hh]])
            bO = []
            for hh in H4:
                bk = self.bank()
                bO.append(bk)
                self.mm(bk, bk.t[:, 0:256], self.qdec[:, hh, sl], self.Sbf[:, hh, :], True, False, [self.b_qdec, self.bSb[hh]])
                self.mm(bk, bk.t[:, 0:256], self.g_at[:, hh, :], vs[hh], False, True, [self.b_gat[hh], self.b_vtm])
                self.mm(bk, bk.t[:, 256:512], self.kte[:, a, hh * 128:(hh + 1) * 128], vs[hh], True, True, [self.b_kte, self.b_vtm])
            if a == 0 and mid_hook is not None:
                mid_hook()
            for hh in H4:
                col = hh * 2 + a
                S.op("dve", lambda: dve.scalar_tensor_tensor(out=self.Sf[:, hh, :], in0=self.Sf[:, hh, :],
                                                             scalar=self.smalls[:, col:col + 1], in1=bO[hh].t[:, 256:512],
                                                             op0=ALU.mult, op1=ALU.add),
                     reads=[self.b_dec], writes=[bO[hh].buf, self.bS[hh]])
            for hh in H4:
                col = hh * 2 + a
                junk, junkBs = self.bigfR.next()
                S.op("act", lambda: act.activation(out=junk[:, 0:256], in_=bO[hh].t[:, 0:256], func=AF.Square, scale=1.0 / 16.0,
                                                   accum_out=self.smalls[:, 8 + col:9 + col]),
                     writes=[bO[hh].buf, self.b_ss] + junkBs)
            for hh in H4:
                S.op("act", lambda: act.activation(out=self.Sbf[:, hh, :], in_=self.Sf[:, hh, :], func=AF.Copy),
                     reads=[self.bS[hh]], writes=[self.bSb[hh]])
            for hh in H4:
                col = hh * 2 + a
                S.op("act", lambda: act.activation(out=self.smalls[:, 16 + col:17 + col], in_=self.smalls[:, 8 + col:9 + col],
                                                   func=AF.Ln, bias=EPS), reads=[self.b_ss], writes=[self.b_ss2])
            for hh in H4:
                col = hh * 2 + a
                S.op("act", lambda: act.activation(out=self.smalls[:, 16 + col:17 + col], in_=self.smalls[:, 16 + col:17 + col],
                                                   func=AF.Exp, scale=-0.5), reads=[self.b_ss2], writes=[self.b_ss2])
            for hh in H4:
                col = hh * 2 + a
                S.op("dve", lambda: dve.tensor_scalar(out=self.g_oa[:, hh, :], in0=bO[hh].t[:, 0:256],
                                                      scalar1=self.smalls[:, 16 + col:17 + col], scalar2=None, op0=ALU.mult),
                     reads=[self.b_ss2], writes=[bO[hh].buf, self.b_goa[hh]])
            bT = []
            for hh in H4:
                bk = self.bank()
                bT.append(bk)
                bTv = bk.t[:, :].bitcast(BF16)
                for c in range(2):
                    S.op("pe", lambda: pe.transpose(bTv[:, c * 128:(c + 1) * 128], self.g_oa[:, hh, c * 128:(c + 1) * 128], self.ident),
                         reads=[self.b_goa[hh], self.b_cmat], writes=[bk.buf])
            for hh in H4:
                bTv = bT[hh].t[:, :].bitcast(BF16)
                for c in range(2):
                    ch = 2 * hh + c
                    gcol = CV_GLA + l * 8 + ch
                    S.op("dve", lambda: dve.scalar_tensor_tensor(out=self.oaT[:, ch, sl], in0=bTv[:, c * 128:(c + 1) * 128],
                                                                 scalar=self.cvec[:, gcol:gcol + 1], in1=self.sra[:, ch, sl],
                                                                 op0=ALU.mult, op1=ALU.mult),
                         reads=[self.b_cvec, self.b_sra], writes=[bT[hh].buf, self.b_oaT])

    def moba_gate1(self, l, g):
        S, nc = self.S, self.nc
        dve = nc.vector
        if g >= 1:
            bG = self.bank()
            for a in range(2):
                for hd in range(8):
                    o = bG.t[:, a * 64 + hd * 8:a * 64 + hd * 8 + 8]
                    self.mm(bG, o, self.QTz[:, hd, a * 128:(a + 1) * 128], self.ksb[:, hd // 2, :], True, True,
                            [self.b_QTz, self.b_ksb])
            S.op("dve", lambda: dve.tensor_copy(out=self.gsb[:, :], in_=bG.t[:, 0:128]), writes=[bG.buf, self.b_gsb])
            if g < 8:
                S.op("dve", lambda: dve.memset(self.gsb[:, :].rearrange("p (i n) -> p i n", n=8)[:, :, g:8], -1e30),
                     writes=[self.b_gsb])

    def moba_gate2(self, l, g):
        S, nc = self.S, self.nc
        dve = nc.vector
        if g >= 1:
            for i in range(16):
                S.op("dve", lambda: dve.max(out=self.top8[:, i, :], in_=self.gsb[:, i * 8:(i + 1) * 8]),
                     reads=[self.b_gsb], writes=[self.b_top8])
            for i in range(16):
                S.op("dve", lambda: dve.tensor_scalar(out=self.sel[:, i * 8:(i + 1) * 8], in0=self.gsb[:, i * 8:(i + 1) * 8],
                                                      scalar1=self.top8[:, i, 2:3], scalar2=None, op0=ALU.is_ge),
                     reads=[self.b_gsb, self.b_top8], writes=[self.b_sel])
            for a in range(2):
                S.op("dve", lambda: dve.tensor_tensor(out=self.selw[:, a * 64:(a + 1) * 64], in0=self.sel[:, a * 64:(a + 1) * 64],
                                                      in1=self.expb31[:], op=ALU.mult),
                     reads=[self.b_sel, self.b_expb31], writes=[self.b_selw])

    def gla_moba_group(self, l, g):
        S, nc = self.S, self.nc
        act, dve, pe = nc.scalar, nc.vector, nc.tensor
        self.moba_gate1(l, g)
        self.gla_group(l, g, mid_hook=lambda: self.moba_gate2(l, g))
        if self.S.real:
            self.phases.append((6, self.S.idx["pe"]))
        items = []
        for hd in range(8):
            items.append((hd, g))
            for n in range(g - 1, -1, -1):
                items.append((hd, n))
        n_it = len(items)
        LA = 2
        pend = []
        for i in range(n_it + LA):
            if i < n_it:
                pend.append(self.moba_stage1(g, *items[i]))
            if i >= LA:
                j = i - LA
                self.moba_stage2(g, items[j][0], items[j][1], pend[j])
        for a in range(2):
            bT = self.bank()
            bTv = bT.t[:, :].bitcast(BF16)
            for c in range(4):
                S.op("pe", lambda: pe.transpose(bTv[:, c * 128:(c + 1) * 128], self.obtm[:, a, c * 128:(c + 1) * 128], self.ident),
                     reads=[self.b_obtm, self.b_cmat], writes=[bT.buf])
            S.op("act", lambda: act.activation(out=self.obT[:, :, a * 128:(a + 1) * 128],
                                               in_=bTv[:, 0:512].rearrange("p (c t) -> p c t", t=128), func=AF.Copy),
                 writes=[bT.buf, self.b_obT])

    def moba_stage1(self, g, hd, n):
        S, act = self.S, self.nc.scalar
        pr = hd // 2
        bS = self.bank()
        Q = self.QTz[:, hd, :]
        rd = [self.b_QTz, self.bKT[n]]
        rdb = [self.b_cmat, self.b_cbias]
        BT = self.cbias
        if n == g:
            j0, j1 = 2 * g, 2 * g + 1
            self.mm(bS, bS.t[:, 0:256], self.KT[:, pr, j0 * 128:(j0 + 1) * 128], Q[:, 0:256], True, False, rd)
            self.mm(bS, bS.t[:, 0:256], self.ident, BT[:, hd, 0:256], False, True, rdb)
            self.mm(bS, bS.t[:, 256:384], self.KT[:, pr, j1 * 128:(j1 + 1) * 128], Q[:, 128:256], True, False, rd)
            self.mm(bS, bS.t[:, 256:384], self.ident, BT[:, hd, 0:128], False, True, rdb)
            width = 384
        else:
            j0, j1 = 2 * n, 2 * n + 1
            near = (n == g - 1)
            self.mm(bS, bS.t[:, 0:256], self.KT[:, pr, j0 * 128:(j0 + 1) * 128], Q[:, 0:256], True, not near, rd)
            if near:
                self.mm(bS, bS.t[:, 0:256], self.ident, BT[:, hd, 256:512], False, True, rdb)
            self.mm(bS, bS.t[:, 256:512], self.KT[:, pr, j1 * 128:(j1 + 1) * 128], Q[:, 0:256], True, not near, rd)
            if near:
                self.mm(bS, bS.t[:, 256:512], self.ident, BT[:, hd, 128:384], False, True, rdb)
            width = 512
        pt, ptB = self.bfpR.next()
        S.op("act", lambda: act.activation(out=pt[:, 0:width], in_=bS.t[:, 0:width], func=AF.Exp), writes=[bS.buf, ptB])
        return (pt, ptB)

    def moba_stage2(self, g, hd, n, st):
        S, dve = self.S, self.nc.vector
        pt, ptB = st
        bO = self.bank()
        V = self.Vaug
        if n == g:
            j0, j1 = 2 * g, 2 * g + 1
            rd = [ptB, self.bV[g]]
            self.mm(bO, bO.t[:, 0:65], pt[:, 0:128], V[:, j0, hd, :], True, True, rd)
            self.mm(bO, bO.t[:, 65:130], pt[:, 128:256], V[:, j0, hd, :], True, False, rd)
            self.mm(bO, bO.t[:, 65:130], pt[:, 256:384], V[:, j1, hd, :], False, True, rd)
            S.op("dve", lambda: dve.tensor_copy(out=self.accO[:, :, hd, :], in_=bO.t[:, 0:130].rearrange("p (a c) -> p a c", c=65)),
                 writes=[bO.buf, self.b_accO])
        else:
            j0, j1 = 2 * n, 2 * n + 1
            rd = [ptB, self.bV[n]]
            for a in range(2):
                o = bO.t[:, a * 65:(a + 1) * 65]
                self.mm(bO, o, pt[:, a * 128:(a + 1) * 128], V[:, j0, hd, :], True, False, rd)
                self.mm(bO, o, pt[:, 256 + a * 128:256 + (a + 1) * 128], V[:, j1, hd, :], False, True, rd)
            wt, wtB = (self.sel, self.b_sel) if n == g - 1 else (self.selw, self.b_selw)
            for a in range(2):
                cidx = a * 64 + hd * 8 + n
                S.op("dve", lambda: dve.scalar_tensor_tensor(out=self.accO[:, a, hd, :], in0=bO.t[:, a * 65:(a + 1) * 65],
                                                             scalar=wt[:, cidx:cidx + 1], in1=self.accO[:, a, hd, :],
                                                             op0=ALU.mult, op1=ALU.add),
                     reads=[wtB], writes=[bO.buf, self.b_accO])
        if n == 0:
            S.op("dve", lambda: dve.reciprocal(out=self.smalls[:, 24:26], in_=self.accO[:, :, hd, 64]),
                 reads=[self.b_accO], writes=[self.b_rc])
            for a in range(2):
                S.op("dve", lambda: dve.tensor_scalar(out=self.obtm[:, a, hd * 64:(hd + 1) * 64], in0=self.accO[:, a, hd, 0:64],
                                                      scalar1=self.smalls[:, 24 + a:25 + a], scalar2=None, op0=ALU.mult),
                     reads=[self.b_accO, self.b_rc], writes=[self.b_obtm])

    def ffn_group(self, l, g):
        S, nc = self.S, self.nc
        act, dve = nc.scalar, nc.vector
        t0 = g * TG
        h, hB = self.h, self.b_h
        self.chk(9)
        self.rmsnorm(g, CV_NFFN + l * 8)
        accb = self.banks[4:8]
        for b in accb:
            b.fresh = True
        rot = Rot(self.banks[0:4])
        u0 = self.w_used
        slots = {}

        pool = nc.gpsimd
        st = {}

        def s1(ct):
            w, wB = self.w_get(0)
            bk = rot.next()
            bk.fresh = True
            for k in range(8):
                self.mm(bk, bk.t[:, 0:TG], w[:, k * 128:(k + 1) * 128], h[:, k, :], k == 0, k == 7, [wB, hB])
            for k in range(8):
                self.mm(bk, bk.t[:, TG:2 * TG], w[:, 1024 + k * 128:1024 + (k + 1) * 128], h[:, k, :],
                        k == 0, k == 7, [wB, hB])
            ue, ueB, ueH = self.smfR.next()
            S.op("dve", lambda: dve.tensor_copy(out=ue[:, :, 0:2], in_=self.carry[:, :, ct, :]), reads=[self.b_carry], writes=[ueH])
            S.op("act", lambda: act.activation(out=ue[:, :, 2:258], in_=bk.t[:, :].rearrange("p (a t) -> p a t", t=TG), func=AF.Copy),
                 writes=[bk.buf, ueB])
            st[ct] = dict(ue=ue, ueB=ueB, ueH=ueH)
            self.w_done()

        def s2(ct):
            ue, ueB, ueH = st[ct]["ue"], st[ct]["ueB"], st[ct]["ueH"]
            S.op("dve", lambda: dve.tensor_copy(out=self.carry[:, :, ct, :], in_=ue[:, :, 256:258]), reads=[ueB], writes=[self.b_carry])
            cc, ccBs = self.bigfR.next()
            ccB, ccB2 = ccBs
            st[ct].update(cc=cc, ccB=ccB, ccB2=ccB2)
            for ab in range(2):
                cbuf = ccB if ab == 0 else ccB2
                cb = CV_CW + ((l * 2 + ab) * 22 + ct) * 4
                co = cc[:, ab * TG:(ab + 1) * TG]
                wr = [cbuf]
                S.op("pool", lambda: pool.tensor_scalar(out=co, in0=ue[:, ab, 2:258], scalar1=self.cvec[:, cb + 2:cb + 3],
                                                        scalar2=self.cvec[:, cb + 3:cb + 4], op0=ALU.mult, op1=ALU.add),
                     reads=[ueB, self.b_cvec], writes=wr)
            for ab in range(2):
                cbuf = ccB if ab == 0 else ccB2
                cb = CV_CW + ((l * 2 + ab) * 22 + ct) * 4
                co = cc[:, ab * TG:(ab + 1) * TG]
                wr = [cbuf]
                S.op("dve", lambda: dve.scalar_tensor_tensor(out=co, in0=ue[:, ab, 1:257], scalar=self.cvec[:, cb + 1:cb + 2], in1=co,
                                                             op0=ALU.mult, op1=ALU.add),
                     reads=[ueB, ueH, self.b_cvec], writes=wr)
                S.op("dve", lambda: dve.scalar_tensor_tensor(out=co, in0=ue[:, ab, 0:256], scalar=self.cvec[:, cb:cb + 1], in1=co,
                                                             op0=ALU.mult, op1=ALU.add),
                     reads=[ueB, ueH, self.b_cvec], writes=wr)

        def s3(ct):
            cc, ccB = st[ct]["cc"], st[ct]["ccB"]
            S.op("act", lambda: act.activation(out=cc[:, 0:TG], in_=cc[:, 0:TG], func=AF.Silu), reads=[ccB], writes=[ccB])

        def s4(ct):
            cc, ccB = st[ct]["cc"], st[ct]["ccB"]
            at, atB = self.bfpR.next()
            S.op("dve", lambda: dve.tensor_tensor(out=at[:, 0:TG], in0=cc[:, 0:TG], in1=cc[:, TG:2 * TG], op=ALU.mult),
                 reads=[ccB, st[ct]["ccB2"]], writes=[atB])
            st[ct].update(at=at, atB=atB)

        def s5(ct):
            w, wB = self.d_get(0)
            at, atB = st[ct]["at"], st[ct]["atB"]
            for dtile in range(8):
                ab_ = accb[dtile // 2]
                o = ab_.t[:, (dtile % 2) * TG:(dtile % 2 + 1) * TG]
                self.mm(ab_, o, w[:, dtile * 128:(dtile + 1) * 128], at[:, 0:TG],
                        ct == 0, ct == 21, [wB, atB])
            self.d_done()
            del st[ct]

        for i in range(22 + 3):
            if i < 22:
                s1(i)
            if 0 <= i - 1 < 22:
                s3(i - 1)
            if i < 22:
                s2(i)
            if 0 <= i - 1 < 22:
                s4(i - 1)
            if 0 <= i - 3 < 22:
                s5(i - 3)
        self.chk(10)
        for i in range(4):
            S.op("dve", lambda: dve.tensor_tensor(out=self.X[:, 2 * i:2 * i + 2, t0:t0 + TG], in0=self.X[:, 2 * i:2 * i + 2, t0:t0 + TG],
                                                  in1=accb[i].t[:, :].rearrange("p (j t) -> p j t", t=TG), op=ALU.add),
                 writes=[accb[i].buf, self.bX[g]])

    def final_group(self, g):
        S, sp = self.S, self.nc.sync
        t0 = g * TG
        self.rmsnorm(g, CV_NFIN, inplace=True)
        ov = self.d_out.rearrange("(k p) t -> p k t", p=128)
        S.dma("sp", lambda: sp.dma_start(out=ov[:, :, t0:t0 + TG], in_=self.X[:, :, t0:t0 + TG]), "out%d" % (g % 2),
              reads=[self.bX[g]])


def build_program(n_layers=DEPTH, n_groups=NG):
    prog = Prog(n_layers, n_groups)
    s1 = Sched(prog.nc, None)
    prog.emit(s1)
    s2 = Sched(prog.nc, s1.need)
    prog.emit(s2)
    prog.stats = dict(idx=dict(s2.idx), incs=dict(s2.incs), waits=s2.n_wait, sbuf_left=prog.sbuf_left)
    return prog


def _t5_bucket(n):
    n = np.maximum(n, 0)
    nf = np.maximum(n, 1).astype(np.float32)
    large = 16 + (np.log(nf / np.float32(16.0)) / np.float32(math.log(128 / 16)) * np.float32(16)).astype(np.int32)
    large = np.minimum(large, 31)
    return np.where(n < 16, n, large)


def _blk(w, c0, nc_):
    K = w.shape[0] // 128
    return np.ascontiguousarray(w[:, c0:c0 + nc_].reshape(K, 128, nc_).transpose(1, 0, 2)).reshape(128, K * nc_)


def prep_shared(inp, n_layers=DEPTH):
    f32 = np.float32
    L = n_layers
    w_in = np.asarray(inp["w_in"], f32)
    wa = np.zeros((L * NBA, 128, WA_E), f32)
    wf = np.zeros((L * NBF, 128, WSLOT), f32)
    walr = np.zeros((L, 128, 128), f32)
    for l in range(L):
        wi = w_in[l]
        blocks = []
        blocks += [_blk(wi, O_KA + q * 256, 256) for q in range(2)]
        for hp in range(2):
            blocks += [_blk(wi, O_QA + hp * 256, 256), _blk(wi, O_KA + hp * 256, 256)]
        blocks += [_blk(wi, O_VA + q * 256, 256) for q in range(4)]
        blocks += [_blk(wi, O_QB + q * 256, 256) for q in range(2)]
        blocks += [_blk(wi, O_KB + q * 256, 256) for q in range(2)]
        blocks += [_blk(wi, O_VB + q * 256, 256) for q in range(2)]
        blocks += [_blk(wi, O_RA + q * 256, 256) for q in range(4)]
        wbg = np.asarray(inp["w_branch_gla"][l], f32)
        wbm = np.asarray(inp["w_branch_moba"][l], f32)
        for dtile in range(8):
            c0 = dtile * 128
            m = np.zeros((128, WA_E), f32)
            m[:, 0:1024] = _blk(wbg, c0, 128)
            m[:, 1024:1536] = _blk(wbm, c0, 128)
            blocks.append(m)
            m = np.zeros((128, WA_E), f32)
            m[:, 0:1024] = _blk(wi, O_G + c0, 128)
            m[:, 1024:2048] = _blk(wi, O_G + 1024 + c0, 128)
            blocks.append(m)
        wo = np.asarray(inp["w_out"][l], f32)
        blocks += [_blk(wo, q * 256, 256) for q in range(4)]
        assert len(blocks) == NBA
        for i, bb in enumerate(blocks):
            wa[l * NBA + i] = bb
        walr[l] = _blk(wi, O_ALR, 16)
        wu = np.asarray(inp["w_up"][l], f32)
        wd = np.asarray(inp["w_down"][l], f32)
        for ct in range(NBF):
            wf[l * NBF + ct, :, 0:1024] = _blk(wu, ct * 128, 128)
            wf[l * NBF + ct, :, 1024:2048] = _blk(wu, D_FF + ct * 128, 128)
            wf[l * NBF + ct, :, 2048:3072] = wd[ct * 128:(ct + 1) * 128, :]
    wlr = np.zeros((17, DEPTH * 512), f32)
    for l in range(L):
        wlr[0:16, l * 512:(l + 1) * 512] = np.asarray(inp["w_lr_up"][l], f32)
        wlr[16, l * 512:(l + 1) * 512] = np.asarray(inp["b_forget"][l], f32)
    cvec = np.zeros((128, CV_N), f32)
    for l in range(L):
        cvec[:, CV_NMIX + l * 8:CV_NMIX + (l + 1) * 8] = np.asarray(inp["norm_mix"][l], f32).reshape(8, 128).T
        cvec[:, CV_NFFN + l * 8:CV_NFFN + (l + 1) * 8] = np.asarray(inp["norm_ffn"][l], f32).reshape(8, 128).T
        cvec[:, CV_GLA + l * 8:CV_GLA + (l + 1) * 8] = np.asarray(inp["gla_out_norm"][l], f32).reshape(8, 128).T
        cw = np.asarray(inp["conv_w"][l], f32)
        cb = np.asarray(inp["conv_b"][l], f32)
        for ab in range(2):
            full = np.concatenate([cw[:, ab * D_FF:(ab + 1) * D_FF], cb[None, ab * D_FF:(ab + 1) * D_FF]], axis=0)
            arr = full.reshape(4, 22, 128).transpose(2, 1, 0)
            c0 = CV_CW + (l * 2 + ab) * 22 * 4
            cvec[:, c0:c0 + 88] = arr.reshape(128, 88)
    cvec[:, CV_NFIN:CV_NFIN + 8] = np.asarray(inp["norm_final"], f32).reshape(8, 128).T
    rb = np.asarray(inp["rel_bias"], f32)
    kk = np.arange(128)[:, None]
    qq = np.arange(128)[None, :]
    cbias = np.zeros((128, 8, 512), f32)
    bd = _t5_bucket(qq - kk)
    bs = _t5_bucket(128 + qq - kk)
    for hd in range(8):
        diag = rb[bd, hd]
        cbias[:, hd, 0:128] = np.where(qq >= kk, diag, np.float32(NEG))
        cbias[:, hd, 128:256] = rb[bs, hd]
        cbias[:, hd, 256:512] = rb[31, hd]
    cmisc = np.zeros((128, CM_N), f32)
    cmisc[:, CM_B31:CM_B31 + 64] = np.repeat(rb[31, :], 8)[None, :]
    cmat = np.zeros((128, CMAT_N), f32)
    s = np.arange(128)[:, None]
    t = np.arange(128)[None, :]
    cmat[:, 0:128] = np.eye(128, dtype=f32)
    cmat[:, 128:256] = 1.0 / 1024.0
    cmat[:, 256:384] = (s <= t).astype(f32)
    cmat[:, 384:512] = np.where(s <= t, -1.0 / 16.0, 0.0)
    cmat[:, 512:640] = np.where(s > t, -1.0 / 16.0, 0.0)
    return dict(wa=wa, wf=wf, walr=walr, wlr=wlr, cvec=cvec, cbias=cbias.reshape(128, 8 * 512), cmisc=cmisc, cmat=cmat)


_PROG_CACHE = {}


def kernel(x, rel_bias, norm_mix, w_in, w_lr_up, b_forget, gla_out_norm, w_branch_gla, w_branch_moba, w_out,
           norm_ffn, w_up, conv_w, conv_b, w_down, norm_final):
    inp = dict(rel_bias=rel_bias, norm_mix=norm_mix, w_in=w_in, w_lr_up=w_lr_up, b_forget=b_forget,
               gla_out_norm=gla_out_norm, w_branch_gla=w_branch_gla, w_branch_moba=w_branch_moba, w_out=w_out,
               norm_ffn=norm_ffn, w_up=w_up, conv_w=conv_w, conv_b=conv_b, w_down=w_down, norm_final=norm_final)
    x = np.asarray(x, np.float32)
    Bn = x.shape[0]
    shared = prep_shared(inp)
    prog = build_program()
    in_maps = []
    for b in range(Bn):
        m = dict(shared)
        m["xT"] = np.ascontiguousarray(x[b].T)
        in_maps.append(m)
    res = run_bass_kernel_spmd(prog.nc, in_maps, core_ids=list(range(Bn)))
    out = np.stack([np.ascontiguousarray(r["outT"].T) for r in res.results], axis=0)
    return out.astype(np.float32)
```

```python
import math
import os
import numpy as np
import concourse.bass as bass
import concourse.mybir as mybir
from concourse.bass_utils import run_bass_kernel_spmd

F32 = mybir.dt.float32
BF16 = mybir.dt.bfloat16
AF = mybir.ActivationFunctionType
ALU = mybir.AluOpType

D = 1024
T = 2048
DEPTH = 4
TG = 256
NG = T // TG
D_FF = 2816
EPS = 1e-6
NEG = -30000.0
NBA = 40
NBF = 22
WA_E = 2048
WSLOT = 3072
NSLOT = 5
NDSLOT = 6
O_QA, O_KA, O_VA, O_RA, O_ALR, O_QB, O_KB, O_VB, O_G = 0, 512, 1024, 2048, 3072, 3088, 3600, 4112, 4624

CV_NMIX = 0
CV_NFFN = DEPTH * 8
CV_GLA = 2 * DEPTH * 8
CV_NFIN = 3 * DEPTH * 8
CV_CW = 3 * DEPTH * 8 + 8
CV_N = CV_CW + DEPTH * 2 * 22 * 4
CM_B31 = 0
CM_NEG = 64
CM_N = 64
CMAT_N = 5 * 128


class Buf:
    __slots__ = ("name", "w", "r")

    def __init__(self, name):
        self.name = name
        self.w = {}
        self.r = {}


class Sched:
    ENGS = ("pe", "act", "dve", "pool", "sp")

    def __init__(self, nc, plan):
        self.nc = nc
        self.plan = plan
        self.real = plan is not None
        self.idx = {e: 0 for e in self.ENGS}
        self.incs = {e: 0 for e in self.ENGS}
        self.val = {}
        self.waited = {e: {} for e in self.ENGS}
        self.need = set()
        self.dcount = {}
        self.dsem = {}
        self.n_wait = 0
        if self.real:
            self.eng = {"pe": nc.tensor, "act": nc.scalar, "dve": nc.vector, "pool": nc.gpsimd, "sp": nc.sync}
            self.esem = {e: nc.alloc_semaphore("sem_" + e) for e in self.ENGS}

    def _dsem(self, name):
        if name not in self.dsem:
            self.dsem[name] = self.nc.alloc_semaphore("dsem_" + name) if self.real else None
            self.dcount[name] = 0
        return self.dsem[name]

    def _wait(self, eng, t):
        if t[0] == "e":
            _, f, i = t
            if f == eng:
                if eng == "pe" or self.idx[eng] - i > 6:
                    return
            if self.waited[eng].get(f, -1) >= i:
                return
            self.waited[eng][f] = i
            self.need.add((f, i))
            if self.real:
                self.eng[eng].wait_ge(self.esem[f], self.val[(f, i)])
                self.n_wait += 1
        else:
            _, name, n = t
            key = ("d", name)
            if self.waited[eng].get(key, 0) >= n:
                return
            self.waited[eng][key] = n
            if self.real:
                self.eng[eng].wait_ge(self.dsem[name], 16 * n)
                self.n_wait += 1

    def _deps(self, eng, reads, writes):
        for b in reads:
            for t in b.w.values():
                self._wait(eng, t)
        for b in writes:
            for t in b.w.values():
                self._wait(eng, t)
            for t in b.r.values():
                self._wait(eng, t)

    def op(self, eng, fn, reads=(), writes=()):
        self._deps(eng, reads, writes)
        i = self.idx[eng]
        if self.real:
            ins = fn()
            if (eng, i) in self.plan:
                self.incs[eng] += 1
                ins.then_inc(self.esem[eng], 1)
                self.val[(eng, i)] = self.incs[eng]
        self.idx[eng] = i + 1
        t = ("e", eng, i)
        for b in writes:
            b.w[eng] = t
        for b in reads:
            b.r[eng] = t
        return t

    def dma(self, q, fn, sem, reads=(), writes=()):
        self._deps(q, reads, writes)
        self._dsem(sem)
        self.dcount[sem] += 1
        n = self.dcount[sem]
        if self.real:
            fn().then_inc(self.dsem[sem], 16)
        self.idx[q] += 1
        t = ("d", sem, n)
        for b in writes:
            b.w["d:" + sem] = t
        for b in reads:
            b.r["d:" + sem] = t
        return t

    def wait_all_dma(self, eng, sem):
        self._wait(eng, ("d", sem, self.dcount[sem]))


class StopEmit(Exception):
    pass


class Bank:
    def __init__(self, t, i):
        self.t = t
        self.buf = Buf("bank%d" % i)
        self.fresh = True


class Rot:
    def __init__(self, items):
        self.items = items
        self.i = 0

    def next(self):
        it = self.items[self.i % len(self.items)]
        self.i += 1
        return it


class Prog:
    def __init__(self, n_layers=DEPTH, n_groups=NG):
        self.L = n_layers
        self.NGR = n_groups
        nc = bass.Bass("TRN2", target_bir_lowering=False)
        self.nc = nc
        L = n_layers
        dt = nc.dram_tensor
        self.d_x = dt("xT", [D, T], F32, kind="ExternalInput").ap()
        self.d_wa = dt("wa", [L * NBA, 128, WA_E], F32, kind="ExternalInput").ap()
        self.d_wf = dt("wf", [L * NBF, 128, WSLOT], F32, kind="ExternalInput").ap()
        self.d_walr = dt("walr", [L, 128, 128], F32, kind="ExternalInput").ap()
        self.d_wlr = dt("wlr", [17, DEPTH * 512], F32, kind="ExternalInput").ap()
        self.d_cvec = dt("cvec", [128, CV_N], F32, kind="ExternalInput").ap()
        self.d_cbias = dt("cbias", [128, 8 * 512], F32, kind="ExternalInput").ap()
        self.d_cmisc = dt("cmisc", [128, CM_N], F32, kind="ExternalInput").ap()
        self.d_cmat = dt("cmat", [128, CMAT_N], F32, kind="ExternalInput").ap()
        self.d_out = dt("outT", [D, T], F32, kind="ExternalOutput").ap()
        self.d_wa16 = dt("wa16", [L * NBA, 128, WA_E], BF16).ap()
        self.d_wf16 = dt("wf16", [L * NBF, 128, WSLOT], BF16).ap()
        self.d_walr16 = dt("walr16", [L, 128, 128], BF16).ap()

        A = nc.alloc_sbuf_tensor
        self.X = A("X", [128, 8, T], F32)
        self.KT = A("KT", [128, 4, T], BF16)
        self.Vaug = A("Vaug", [128, 16, 8, 65], BF16)
        self.Sf = A("Sf", [128, 4, 256], F32)
        self.Sbf = A("Sbf", [128, 4, 256], BF16)
        self.wslot = [A("wslot%d" % i, [128, WA_E], BF16) for i in range(NSLOT)]
        self.dslot = [A("dslot%d" % i, [128, 1024], BF16) for i in range(NDSLOT)]
        self.alrw = [A("alrw%d" % i, [128, 128], BF16) for i in range(2)]
        self.h = A("h", [128, 8, TG], BF16)
        self.rstd = A("rstd", [128, TG], F32)
        self.qdec = A("qdec", [128, 4, TG], BF16)
        self.kdec = A("kdec", [128, 4, TG], BF16)
        self.kte = A("kte", [128, 2, 512], BF16)
        self.vtm = A("vtm", [128, 2, 1024], BF16)
        self.sra = A("sra", [128, 8, TG], BF16)
        self.QTz = A("QTz", [128, 8, TG], BF16)
        self.alrT = A("alrT", [17, TG], BF16)
        self.lhi = A("lhi", [128, 2, 512], BF16)
        self.llo = A("llo", [128, 2, 512], BF16)
        self.obtm = self.lhi
        self.mixed = self.sra
        self.obT = self.llo[:, :, :].rearrange("p a (b t) -> p (a b) t", t=TG)
        self.oaT = A("oaT", [128, 8, TG], BF16)
        self.accO = A("accO", [128, 2, 8, 65], F32)
        self.gsb = A("gsb", [128, 128], F32)
        self.sel = A("sel", [128, 128], F32)
        self.selw = A("selw", [128, 128], F32)
        self.top8 = A("top8", [128, 16, 8], F32)
        self.smalls = A("smalls", [128, 64], F32)
        self.ksb = A("ksb", [128, 4, 8], BF16)
        self.carry = A("carry", [128, 2, 22, 2], F32)
        self.bigf = [A("bigf%d" % i, [128, 512], F32) for i in range(3)]
        self.smf = [A("smf%d" % i, [128, 2, 258], F32) for i in range(3)]
        self.bfp = [A("bfp%d" % i, [128, 512], BF16) for i in range(4)]
        self.g_at = A("g_at", [128, 4, 128], BF16)
        self.g_oa = A("g_oa", [128, 4, 256], BF16)
        self.cvec = A("cvec_s", [128, CV_N], F32)
        self.cbias = A("cbias_s", [128, 8, 512], BF16)
        self.cmisc = A("cmisc_s", [128, CM_N], F32)
        self.cmat = A("cmat_s", [128, CMAT_N], BF16)
        self.wlr = A("wlr_s", [17, 512], BF16)
        self.expb31 = A("expb31", [128, 64], F32)
        self.banks_t = [nc.alloc_psum_tensor("bank%d" % i, [128, 512], F32) for i in range(8)]
        self.sbuf_left = nc.sbuf_bytes_remaining

    def emit(self, S):
        self.S = S
        nc = self.nc
        self.banks = [Bank(t, i) for i, t in enumerate(self.banks_t)]
        self.P = Rot(self.banks)
        self.bigfR = Rot([(t, [Buf("bigf%da" % i), Buf("bigf%db" % i)]) for i, t in enumerate(self.bigf)])
        self.smfR = Rot([(t, Buf("smf%d" % i), Buf("smfh%d" % i)) for i, t in enumerate(self.smf)])
        bfl = [(t, Buf("bfp%d" % i)) for i, t in enumerate(self.bfp)]
        self.bfpR = Rot(bfl[0:4])
        self.b_gat = [Buf("gat%d" % i) for i in range(4)]
        self.b_goa = [Buf("goa%d" % i) for i in range(4)]
        B = Buf
        self.bX = [B("X%d" % g) for g in range(NG)]
        self.bKT = [B("KT%d" % g) for g in range(NG)]
        self.bV = [B("V%d" % g) for g in range(NG)]
        self.bS = [B("S%d" % i) for i in range(4)]
        self.bSb = [B("Sb%d" % i) for i in range(4)]
        self.bW = [B("wslot%d" % i) for i in range(NSLOT)]
        self.bD = [B("dslot%d" % i) for i in range(NDSLOT)]
        self.bAlrw = [B("alrw0"), B("alrw1")]
        for n in ("h", "rstd", "qdec", "kdec", "kte", "vtm", "sra", "QTz", "alrT", "lhi", "llo", "eTM", "oaT",
                  "obtm", "obT", "mixed", "accO", "gsb", "sel", "selw", "top8", "dec", "ss", "ss2", "rc", "ksumf",
                  "ksb", "carry", "cvec", "cbias", "cmisc", "cmat", "wlr", "expb31"):
            setattr(self, "b_" + n, B(n))
        self.b_obtm = self.b_lhi
        self.b_mixed = self.b_sra
        self.b_obT = self.b_llo
        self.wlist = []
        for l in range(self.L):
            for g in range(self.NGR):
                self.wlist.append(("alr", l))
                for i in range(NBA):
                    self.wlist.append(("a", l * NBA + i))
                for i in range(NBF):
                    self.wlist.append(("f", l * NBF + i))
                    self.wlist.append(("d", l * NBF + i))
        self.w_issued = 0
        self.w_used = 0
        self.alr_n = 0
        self.big_n = 0
        self.d_used = 0
        self.d_n = 0

        self.stop = int(os.environ.get("KSTOP", "99"))
        self.phases = []
        self.init_phase()
        try:
            for l in range(self.L):
                self.layer_init(l)
                for g in range(self.NGR):
                    self.mixer_group(l, g)
                    self.ffn_group(l, g)
                    if l == self.L - 1:
                        self.final_group(g)
        except StopEmit:
            self.final_group(0)
        for sem in ["w%d" % i for i in range(NSLOT)] + ["d%d" % i for i in range(NDSLOT)] + ["alr0", "alr1", "c_wlr", "c_bias", "c_mat", "c_vec", "c_misc"] + ["cv%d" % i for i in range(DEPTH)] + ["cv0_alr", "cv0_a0", "cv0_a1", "cv0_a2", "cv0_a3", "cv0_f0", "cv0_f1"]:
            if sem in S.dcount:
                S.wait_all_dma("sp", sem)
        for sem in ("out0", "out1"):
            if sem in S.dcount:
                S.wait_all_dma("sp", sem)

    def w_issue(self):
        S = self.S
        while self.w_issued < len(self.wlist):
            kind, idx = self.wlist[self.w_issued]
            if kind == "alr":
                j = self.alr_n % 2
                tl, bf, src = self.alrw[j], self.bAlrw[j], self.d_walr16[idx]
                S.dma("sp", lambda: self.nc.sync.dma_start(out=tl[:], in_=src), "alr%d" % j, reads=[self.cvt[("alr", idx)]], writes=[bf])
                self.alr_n += 1
            elif kind == "d":
                if self.d_n >= self.d_used + NDSLOT:
                    return
                j = self.d_n % NDSLOT
                tl, bf, src = self.dslot[j], self.bD[j], self.d_wf16[idx][:, 2048:3072]
                S.dma("sp", lambda: self.nc.sync.dma_start(out=tl[:], in_=src), "d%d" % j, reads=[self.cvt[("f", idx)]], writes=[bf])
                self.d_n += 1
            else:
                if self.big_n >= self.w_used + NSLOT:
                    return
                j = self.big_n % NSLOT
                tl, bf = self.wslot[j], self.bW[j]
                if kind == "a":
                    src = self.d_wa16[idx]
                    S.dma("sp", lambda: self.nc.sync.dma_start(out=tl[:], in_=src), "w%d" % j,
                          reads=[self.cvt[("a", idx)]], writes=[bf])
                else:
                    src = self.d_wf16[idx][:, 0:2048]
                    S.dma("sp", lambda: self.nc.sync.dma_start(out=tl[:], in_=src), "w%d" % j,
                          reads=[self.cvt[("f", idx)]], writes=[bf])
                self.big_n += 1
            self.w_issued += 1

    def w_get(self, off=0):
        assert self.big_n > self.w_used + off, "weight block not issued (ring too small for this access pattern)"
        j = (self.w_used + off) % NSLOT
        return self.wslot[j], self.bW[j]

    def w_done(self):
        self.w_used += 1
        self.w_issue()

    def d_get(self, off=0):
        assert self.d_n > self.d_used + off, "down block not issued"
        j = (self.d_used + off) % NDSLOT
        return self.dslot[j], self.bD[j]

    def d_done(self):
        self.d_used += 1
        self.w_issue()

    def mm(self, bank, out, lhsT, rhs, first, last, reads):
        st = bool(first and bank.fresh)
        if first:
            bank.fresh = False
        self.S.op("pe", lambda: self.nc.tensor.matmul(out, lhsT, rhs, start=st, stop=bool(last), skip_group_check=True),
                  reads=reads, writes=[bank.buf])

    def chk(self, p):
        if self.S.real:
            self.phases.append((p, self.S.idx["pe"]))
        if p >= self.stop:
            raise StopEmit()

    def bank(self):
        b = self.P.next()
        b.fresh = True
        return b

    def init_phase(self):
        S, nc = self.S, self.nc
        act, dve, sp, pool = nc.scalar, nc.vector, nc.sync, nc.gpsimd
        S.dma("sp", lambda: sp.dma_start(out=self.cvec[:], in_=self.d_cvec), "c_vec", writes=[self.b_cvec])
        S.dma("sp", lambda: sp.dma_start(out=self.cmisc[:], in_=self.d_cmisc), "c_misc", writes=[self.b_cmisc])
        S.dma("pool", lambda: pool.dma_start(out=self.cmat[:], in_=self.d_cmat), "c_mat", writes=[self.b_cmat])
        S.dma("pool", lambda: pool.dma_start(out=self.cbias[:], in_=self.d_cbias.rearrange("p (h c) -> p h c", c=512)),
              "c_bias", writes=[self.b_cbias])
        xv = self.d_x.rearrange("(k p) t -> p k t", p=128)
        for g in range(self.NGR):
            S.dma("sp", lambda: sp.dma_start(out=self.X[:, :, g * TG:(g + 1) * TG], in_=xv[:, :, g * TG:(g + 1) * TG]),
                  "x%d" % g, writes=[self.bX[g]])
        self.cvt = {}
        b0 = Buf("cvt0_alr")
        S.dma("pool", lambda: pool.dma_start(out=self.d_walr16[0], in_=self.d_walr[0]), "cv0_alr", writes=[b0])
        self.cvt[("alr", 0)] = b0
        for c in range(4):
            bc = Buf("cvt0_a%d" % c)
            S.dma("pool", lambda: pool.dma_start(out=self.d_wa16[c * 10:(c + 1) * 10], in_=self.d_wa[c * 10:(c + 1) * 10]),
                  "cv0_a%d" % c, writes=[bc])
            for i in range(c * 10, (c + 1) * 10):
                self.cvt[("a", i)] = bc
        for c in range(2):
            bc = Buf("cvt0_f%d" % c)
            S.dma("pool", lambda: pool.dma_start(out=self.d_wf16[c * 11:(c + 1) * 11], in_=self.d_wf[c * 11:(c + 1) * 11]),
                  "cv0_f%d" % c, writes=[bc])
            for i in range(c * 11, (c + 1) * 11):
                self.cvt[("f", i)] = bc
        self.w_issue()
        S.op("dve", lambda: dve.memset(self.Vaug[:], 1.0), writes=self.bV)
        S.op("dve", lambda: dve.memset(self.alrT[:], 1.0), writes=[self.b_alrT])
        S.op("dve", lambda: dve.memset(self.QTz[:], 0.0), writes=[self.b_QTz])
        S.op("dve", lambda: dve.memset(self.ksb[:], 0.0), writes=[self.b_ksb])
        S.op("act", lambda: act.activation(out=self.expb31[:], in_=self.cmisc[:, CM_B31:CM_B31 + 64], func=AF.Exp),
             reads=[self.b_cmisc], writes=[self.b_expb31])
        self.ident = self.cmat[:, 0:128]
        self.ones_m = self.cmat[:, 128:256]
        self.maskU = self.cmat[:, 256:384]
        self.Mle = self.cmat[:, 384:512]
        self.Mgt = self.cmat[:, 512:640]

    def layer_init(self, l):
        S, dve = self.S, self.nc.vector
        S.dma("pool", lambda: self.nc.gpsimd.dma_start(out=self.wlr[:], in_=self.d_wlr[:, l * 512:(l + 1) * 512]), "c_wlr",
              writes=[self.b_wlr])
        S.op("dve", lambda: dve.memset(self.Sf[:], 0.0), writes=self.bS)
        S.op("dve", lambda: dve.memset(self.Sbf[:], 0.0), writes=self.bSb)
        S.op("dve", lambda: dve.memset(self.carry[:], 0.0), writes=[self.b_carry])

    def rmsnorm(self, g, col0, inplace=False):
        S, nc = self.S, self.nc
        act, dve = nc.scalar, nc.vector
        t0 = g * TG
        Xg = self.X[:, :, t0:t0 + TG]
        S.op("act", lambda: act.activation(out=self.h[:], in_=Xg, func=AF.Square), reads=[self.bX[g]], writes=[self.b_h])
        bk = self.bank()
        for k in range(8):
            self.mm(bk, bk.t[:, 0:TG], self.ones_m, self.h[:, k, :], k == 0, k == 7, [self.b_h, self.b_cmat])
        S.op("act", lambda: act.activation(out=self.rstd[:], in_=bk.t[:, 0:TG], func=AF.Ln, bias=EPS),
             writes=[bk.buf, self.b_rstd])
        S.op("act", lambda: act.activation(out=self.rstd[:], in_=self.rstd[:], func=AF.Exp, scale=-0.5),
             reads=[self.b_rstd], writes=[self.b_rstd])
        for k in range(8):
            if inplace:
                S.op("dve", lambda: dve.scalar_tensor_tensor(out=self.X[:, k, t0:t0 + TG], in0=self.X[:, k, t0:t0 + TG],
                                                             scalar=self.cvec[:, col0 + k:col0 + k + 1], in1=self.rstd[:],
                                                             op0=ALU.mult, op1=ALU.mult),
                     reads=[self.b_rstd, self.b_cvec], writes=[self.bX[g]])
            else:
                S.op("dve", lambda: dve.scalar_tensor_tensor(out=self.h[:, k, :], in0=self.X[:, k, t0:t0 + TG],
                                                             scalar=self.cvec[:, col0 + k:col0 + k + 1], in1=self.rstd[:],
                                                             op0=ALU.mult, op1=ALU.mult),
                     reads=[self.bX[g], self.b_rstd, self.b_cvec], writes=[self.b_h])

    def convert_layer(self, l):
        S, pool = self.S, self.nc.gpsimd
        bc = Buf("cvt%d" % l)
        gate = [self.bX[0]]
        S.dma("pool", lambda: pool.dma_start(out=self.d_walr16[l], in_=self.d_walr[l]), "cv%d" % l, reads=gate, writes=[bc])
        S.dma("pool", lambda: pool.dma_start(out=self.d_wa16[l * NBA:(l + 1) * NBA], in_=self.d_wa[l * NBA:(l + 1) * NBA]),
              "cv%d" % l, reads=gate, writes=[bc])
        S.dma("pool", lambda: pool.dma_start(out=self.d_wf16[l * NBF:(l + 1) * NBF], in_=self.d_wf[l * NBF:(l + 1) * NBF]),
              "cv%d" % l, reads=gate, writes=[bc])
        self.cvt[("alr", l)] = bc
        for i in range(l * NBA, (l + 1) * NBA):
            self.cvt[("a", i)] = bc
        for i in range(l * NBF, (l + 1) * NBF):
            self.cvt[("f", i)] = bc

    def mixer_group(self, l, g):
        S, nc = self.S, self.nc
        act, dve = nc.scalar, nc.vector
        t0 = g * TG
        if g == min(1, self.NGR - 1) and l + 1 < self.L:
            self.convert_layer(l + 1)
        h, hB = self.h, self.b_h
        self.rmsnorm(g, CV_NMIX + l * 8)
        self.chk(0)

        ja = (l * self.NGR + g) % 2
        alrw, alrwB = self.alrw[ja], self.bAlrw[ja]
        bk = self.bank()
        for k in range(8):
            self.mm(bk, bk.t[0:16, 0:TG], alrw[:, k * 16:(k + 1) * 16], h[:, k, :], k == 0, k == 7, [alrwB, hB])
        S.op("act", lambda: act.activation(out=self.alrT[0:16, :], in_=bk.t[0:16, 0:TG], func=AF.Copy),
             writes=[bk.buf, self.b_alrT])
        eTMs = []
        for a in range(2):
            bk = self.bank()
            self.mm(bk, bk.t[:, :], self.alrT[:, a * 128:(a + 1) * 128], self.wlr[:, :], True, True,
                    [self.b_alrT, self.b_wlr])
            e, eB = self.bigfR.next()
            S.op("act", lambda: act.activation(out=e[:], in_=bk.t[:, :], func=AF.Exp, scale=-1.0), writes=[bk.buf] + eB)
            S.op("act", lambda: act.activation(out=e[:], in_=e[:], func=AF.Ln, bias=1.0), reads=eB, writes=eB)
            S.op("dve", lambda: dve.tensor_copy(out=self.lhi[:, a, :], in_=e[:]), reads=eB, writes=[self.b_lhi])
            S.op("dve", lambda: dve.tensor_tensor(out=self.llo[:, a, :], in0=e[:], in1=self.lhi[:, a, :], op=ALU.subtract),
                 reads=eB + [self.b_lhi], writes=[self.b_llo])
            bk2 = self.bank()
            self.mm(bk2, bk2.t[:, :], self.Mgt, self.lhi[:, a, :], True, False, [self.b_cmat, self.b_lhi])
            self.mm(bk2, bk2.t[:, :], self.Mgt, self.llo[:, a, :], False, True, [self.b_cmat, self.b_llo])
            et, etB = self.bigfR.next()
            S.op("act", lambda: act.activation(out=et[:], in_=bk2.t[:, :], func=AF.Exp), writes=[bk2.buf] + etB)
            eTMs.append((et, etB))

        self.chk(1)
        def tm_pair(nblk):
            bks = [self.bank(), self.bank()]
            for q in range(nblk):
                w, wB = self.w_get()
                for a in range(2):
                    for k in range(8):
                        self.mm(bks[a], bks[a].t[:, q * 256:(q + 1) * 256], h[:, k, a * 128:(a + 1) * 128], w[:, k * 256:(k + 1) * 256],
                                k == 0, k == 7, [wB, hB])
                self.w_done()
            return bks

        bks = tm_pair(2)
        for a in range(2):
            bk = bks[a]
            et, etB = eTMs[a]
            S.op("dve", lambda: dve.tensor_tensor(out=self.kte[:, a, :], in0=bk.t[:, :], in1=et[:], op=ALU.mult),
                 reads=etB, writes=[bk.buf, self.b_kte])
        for hp in range(2):
            wq, wqB = self.w_get(0)
            wk, wkB = self.w_get(1)
            for hh in (2 * hp, 2 * hp + 1):
                c = hh % 2
                bkb = self.bank()
                for a in range(2):
                    o = bkb.t[:, a * 128:(a + 1) * 128]
                    self.mm(bkb, o, self.lhi[:, a, hh * 128:(hh + 1) * 128], self.Mle, True, False, [self.b_lhi, self.b_cmat])
                    self.mm(bkb, o, self.llo[:, a, hh * 128:(hh + 1) * 128], self.Mle, False, True, [self.b_llo, self.b_cmat])
                ebt, ebBs = self.bigfR.next()
                eb = ebt[:, 0:TG]
                enb = ebt[:, TG:2 * TG]
                S.op("act", lambda: act.activation(out=eb, in_=bkb.t[:, 0:TG], func=AF.Exp), writes=[bkb.buf] + ebBs)
                S.op("act", lambda: act.activation(out=enb, in_=bkb.t[:, 0:TG], func=AF.Exp, scale=-1.0), writes=[bkb.buf] + ebBs)
                for a in range(2):
                    S.op("dve", lambda: dve.tensor_copy(out=self.smalls[:, hh * 2 + a:hh * 2 + a + 1],
                                                        in_=ebt[:, a * 128 + 127:a * 128 + 128]),
                         reads=ebBs, writes=[self.b_dec])
                bq = self.bank()
                for k in range(8):
                    self.mm(bq, bq.t[:, 0:TG], wq[:, k * 256 + c * 128:k * 256 + (c + 1) * 128], h[:, k, :], k == 0, k == 7, [wqB, hB])
                for k in range(8):
                    self.mm(bq, bq.t[:, TG:2 * TG], wk[:, k * 256 + c * 128:k * 256 + (c + 1) * 128], h[:, k, :], k == 0, k == 7, [wkB, hB])
                S.op("dve", lambda: dve.scalar_tensor_tensor(out=self.qdec[:, hh, :], in0=bq.t[:, 0:TG], scalar=128.0 ** -0.5, in1=eb,
                                                             op0=ALU.mult, op1=ALU.mult),
                     reads=ebBs, writes=[bq.buf, self.b_qdec])
                S.op("dve", lambda: dve.tensor_tensor(out=self.kdec[:, hh, :], in0=bq.t[:, TG:2 * TG], in1=enb, op=ALU.mult),
                     reads=ebBs, writes=[bq.buf, self.b_kdec])
            self.w_done()
            self.w_done()

        self.chk(2)

        for half in range(2):
            bks = tm_pair(2)
            for a in range(2):
                bk = bks[a]
                S.op("act", lambda: act.activation(out=self.vtm[:, a, half * 512:(half + 1) * 512], in_=bk.t[:, :], func=AF.Copy),
                     writes=[bk.buf, self.b_vtm])
        self.chk(3)
        for pr2 in range(2):
            w, wB = self.w_get()
            bk = self.bank()
            for j in range(2):
                for k in range(8):
                    self.mm(bk, bk.t[:, j * TG:(j + 1) * TG], w[:, k * 256 + j * 128:k * 256 + (j + 1) * 128], h[:, k, :],
                            k == 0, k == 7, [wB, hB])
            for j in range(2):
                p = 2 * pr2 + j
                S.op("act", lambda: act.activation(out=self.QTz[0:64, 2 * p, :], in_=bk.t[0:64, j * TG:(j + 1) * TG],
                                                   func=AF.Copy, scale=0.125), writes=[bk.buf, self.b_QTz])
                S.op("act", lambda: act.activation(out=self.QTz[64:128, 2 * p + 1, :], in_=bk.t[64:128, j * TG:(j + 1) * TG],
                                                   func=AF.Copy, scale=0.125), writes=[bk.buf, self.b_QTz])
            self.w_done()
        S.op("dve", lambda: dve.memset(self.smalls[:, 32:64], 0.0), writes=[self.b_ksumf])
        for pr2 in range(2):
            w, wB = self.w_get()
            bk = self.bank()
            for j in range(2):
                for k in range(8):
                    self.mm(bk, bk.t[:, j * TG:(j + 1) * TG], w[:, k * 256 + j * 128:k * 256 + (j + 1) * 128], h[:, k, :],
                            k == 0, k == 7, [wB, hB])
            for j in range(2):
                p = 2 * pr2 + j
                S.op("act", lambda: act.activation(out=self.KT[:, p, t0:t0 + TG], in_=bk.t[:, j * TG:(j + 1) * TG], func=AF.Copy,
                                                   accum_out=self.smalls[:, 32 + p:33 + p]),
                     writes=[bk.buf, self.bKT[g], self.b_ksumf])
            self.w_done()
        S.op("dve", lambda: dve.tensor_copy(out=self.ksb[:, :, g], in_=self.smalls[:, 32:36]),
             reads=[self.b_ksumf], writes=[self.b_ksb])
        bks = tm_pair(2)
        for a in range(2):
            bk = bks[a]
            S.op("act", lambda: act.activation(out=self.Vaug[:, 2 * g + a, :, 0:64],
                                               in_=bk.t[:, :].rearrange("p (h d) -> p h d", d=64), func=AF.Copy),
                 writes=[bk.buf, self.bV[g]])
        self.chk(4)
        for i in range(4):
            w, wB = self.w_get()
            bk = self.bank()
            for j in range(2):
                for k in range(8):
                    self.mm(bk, bk.t[:, j * TG:(j + 1) * TG], w[:, k * 256 + j * 128:k * 256 + (j + 1) * 128], h[:, k, :],
                            k == 0, k == 7, [wB, hB])
            S.op("act", lambda: act.activation(out=self.sra[:, 2 * i:2 * i + 2, :],
                                               in_=bk.t[:, :].rearrange("p (j t) -> p j t", t=TG), func=AF.Silu),
                 writes=[bk.buf, self.b_sra])
            self.w_done()

        self.chk(5)
        self.gla_moba_group(l, g)
        self.chk(7)

        for dtile in range(8):
            wA, wAB = self.w_get(0)
            wG, wGB = self.w_get(1)
            b1 = self.bank()
            for c in range(8):
                self.mm(b1, b1.t[:, 0:TG], wA[:, c * 128:(c + 1) * 128], self.oaT[:, c, :], c == 0, c == 7, [wAB, self.b_oaT])
            for c in range(4):
                self.mm(b1, b1.t[:, TG:2 * TG], wA[:, 1024 + c * 128:1024 + (c + 1) * 128], self.obT[:, c, :], c == 0, c == 3,
                        [wAB, self.b_obT])
            b2 = self.bank()
            for k in range(8):
                self.mm(b2, b2.t[:, 0:TG], wG[:, k * 128:(k + 1) * 128], h[:, k, :], k == 0, k == 7, [wGB, hB])
            for k in range(8):
                self.mm(b2, b2.t[:, TG:2 * TG], wG[:, 1024 + k * 128:1024 + (k + 1) * 128], h[:, k, :], k == 0, k == 7, [wGB, hB])
            sg, sgB = self.bigfR.next()
            S.op("act", lambda: act.activation(out=sg[:], in_=b2.t[:, :], func=AF.Sigmoid), writes=[b2.buf] + sgB)
            S.op("dve", lambda: dve.tensor_tensor(out=sg[:], in0=b1.t[:, :], in1=sg[:], op=ALU.mult), writes=[b1.buf] + sgB)
            S.op("dve", lambda: dve.tensor_tensor(out=self.mixed[:, dtile, :], in0=sg[:, 0:TG], in1=sg[:, TG:2 * TG], op=ALU.add),
                 reads=sgB, writes=[self.b_mixed])
            self.w_done()
            self.w_done()
        self.chk(8)
        for i in range(4):
            w, wB = self.w_get()
            bk = self.bank()
            for j in range(2):
                for k in range(8):
                    self.mm(bk, bk.t[:, j * TG:(j + 1) * TG], w[:, k * 256 + j * 128:k * 256 + (j + 1) * 128], self.mixed[:, k, :],
                            k == 0, k == 7, [wB, self.b_mixed])
            d0 = 2 * i
            S.op("dve", lambda: dve.tensor_tensor(out=self.X[:, d0:d0 + 2, t0:t0 + TG], in0=self.X[:, d0:d0 + 2, t0:t0 + TG],
                                                  in1=bk.t[:, :].rearrange("p (j t) -> p j t", t=TG), op=ALU.add),
                 writes=[bk.buf, self.bX[g]])
            self.w_done()

    def gla_group(self, l, g, mid_hook=None):
        S, nc = self.S, self.nc
        act, dve, pe = nc.scalar, nc.vector, nc.tensor
        S.op("dve", lambda: dve.memset(self.smalls[:, 8:16], 0.0), writes=[self.b_ss])
        H4 = range(4)
        for a in range(2):
            sl = slice(a * 128, (a + 1) * 128)
            vs = [self.vtm[:, a, hh * 256:(hh + 1) * 256] for hh in H4]
            bA = []
            for hh in H4:
                bk = self.bank()
                bA.append(bk)
                self.mm(bk, bk.t[:, 0:128], self.kdec[:, hh, sl], self.qdec[:, hh, sl], True, True, [self.b_kdec, self.b_qdec])
            for hh in H4:
                S.op("dve", lambda: dve.tensor_tensor(out=self.g_at[:, hh, :], in0=bA[hh].t[:, 0:128], in1=self.maskU, op=ALU.mult),
                     reads=[self.b_cmat], writes=[bA[hh].buf, self.b_gat[hh]])
            bO = []
            for hh in H4:
                bk = self.bank()
                bO.append(bk)
                self.mm(bk, bk.t[:, 0:256], self.qdec[:, hh, sl], self.Sbf[:, hh, :], True, False, [self.b_qdec, self.bSb[hh]])
                self.mm(bk, bk.t[:, 0:256], self.g_at[:, hh, :], vs[hh], False, True, [self.b_gat[hh], self.b_vtm])
                self.mm(bk, bk.t[:, 256:512], self.kte[:, a, hh * 128:(hh + 1) * 128], vs[hh], True, True, [self.b_kte, self.b_vtm])
            for hh in H4:
                col = hh * 2 + a
                S.op("dve", lambda: dve.scalar_tensor_tensor(out=self.Sf[:, hh, :], in0=self.Sf[:, hh, :],
                                                             scalar=self.smalls[:, col:col + 1], in1=bO[hh].t[:, 256:512],
                                                             op0=ALU.mult, op1=ALU.add),
                     reads=[self.b_dec], writes=[bO[hh].buf, self.bS[hh]])
            if a == 0 and mid_hook is not None:
                mid_hook(0)
            for hh in H4:
                col = hh * 2 + a
                junk, junkBs = self.bigfR.next()
                S.op("act", lambda: act.activation(out=junk[:, 0:256], in_=bO[hh].t[:, 0:256], func=AF.Square, scale=1.0 / 16.0,
                                                   accum_out=self.smalls[:, 8 + col:9 + col]),
                     writes=[bO[hh].buf, self.b_ss] + junkBs)
            for hh in H4:
                S.op("act", lambda: act.activation(out=self.Sbf[:, hh, :], in_=self.Sf[:, hh, :], func=AF.Copy),
                     reads=[self.bS[hh]], writes=[self.bSb[hh]])
            for hh in H4:
                col = hh * 2 + a
                S.op("act", lambda: act.activation(out=self.smalls[:, 16 + col:17 + col], in_=self.smalls[:, 8 + col:9 + col],
                                                   func=AF.Ln, bias=EPS), reads=[self.b_ss], writes=[self.b_ss2])
            for hh in H4:
                col = hh * 2 + a
                S.op("act", lambda: act.activation(out=self.smalls[:, 16 + col:17 + col], in_=self.smalls[:, 16 + col:17 + col],
                                                   func=AF.Exp, scale=-0.5), reads=[self.b_ss2], writes=[self.b_ss2])
            for hh in H4:
                col = hh * 2 + a
                S.op("dve", lambda: dve.tensor_scalar(out=self.g_oa[:, hh, :], in0=bO[hh].t[:, 0:256],
                                                      scalar1=self.smalls[:, 16 + col:17 + col], scalar2=None, op0=ALU.mult),
                     reads=[self.b_ss2], writes=[bO[hh].buf, self.b_goa[hh]])
            if a == 0 and mid_hook is not None:
                mid_hook(1)
            bT = []
            for hh in H4:
                bk = self.bank()
                bT.append(bk)
                bTv = bk.t[:, :].bitcast(BF16)
                for c in range(2):
                    S.op("pe", lambda: pe.transpose(bTv[:, c * 128:(c + 1) * 128], self.g_oa[:, hh, c * 128:(c + 1) * 128], self.ident),
                         reads=[self.b_goa[hh], self.b_cmat], writes=[bk.buf])
            for hh in H4:
                bTv = bT[hh].t[:, :].bitcast(BF16)
                for c in range(2):
                    ch = 2 * hh + c
                    gcol = CV_GLA + l * 8 + ch
                    S.op("dve", lambda: dve.scalar_tensor_tensor(out=self.oaT[:, ch, sl], in0=bTv[:, c * 128:(c + 1) * 128],
                                                                 scalar=self.cvec[:, gcol:gcol + 1], in1=self.sra[:, ch, sl],
                                                                 op0=ALU.mult, op1=ALU.mult),
                         reads=[self.b_cvec, self.b_sra], writes=[bT[hh].buf, self.b_oaT])

    def moba_gate1(self, l, g):
        S, nc = self.S, self.nc
        dve = nc.vector
        if g >= 1:
            bG = self.bank()
            for a in range(2):
                for hd in range(8):
                    o = bG.t[:, a * 64 + hd * 8:a * 64 + hd * 8 + 8]
                    self.mm(bG, o, self.QTz[:, hd, a * 128:(a + 1) * 128], self.ksb[:, hd // 2, :], True, True,
                            [self.b_QTz, self.b_ksb])
            S.op("dve", lambda: dve.tensor_copy(out=self.gsb[:, :], in_=bG.t[:, 0:128]), writes=[bG.buf, self.b_gsb])
            if g < 8:
                S.op("dve", lambda: dve.memset(self.gsb[:, :].rearrange("p (i n) -> p i n", n=8)[:, :, g:8], -1e30),
                     writes=[self.b_gsb])

    def moba_gate2(self, l, g, part):
        S, nc = self.S, self.nc
        dve = nc.vector
        if g >= 1 and part == 0:
            for i in range(16):
                S.op("dve", lambda: dve.max(out=self.top8[:, i, :], in_=self.gsb[:, i * 8:(i + 1) * 8]),
                     reads=[self.b_gsb], writes=[self.b_top8])
        if g >= 1 and part == 1:
            for i in range(16):
                S.op("dve", lambda: dve.tensor_scalar(out=self.sel[:, i * 8:(i + 1) * 8], in0=self.gsb[:, i * 8:(i + 1) * 8],
                                                      scalar1=self.top8[:, i, 2:3], scalar2=None, op0=ALU.is_ge),
                     reads=[self.b_gsb, self.b_top8], writes=[self.b_sel])
            for a in range(2):
                S.op("dve", lambda: dve.tensor_tensor(out=self.selw[:, a * 64:(a + 1) * 64], in0=self.sel[:, a * 64:(a + 1) * 64],
                                                      in1=self.expb31[:], op=ALU.mult),
                     reads=[self.b_sel, self.b_expb31], writes=[self.b_selw])

    def gla_moba_group(self, l, g):
        S, nc = self.S, self.nc
        act, dve, pe = nc.scalar, nc.vector, nc.tensor
        self.moba_gate1(l, g)
        self.gla_group(l, g, mid_hook=lambda part: self.moba_gate2(l, g, part))
        if self.S.real:
            self.phases.append((6, self.S.idx["pe"]))
        items = []
        for hd in range(8):
            items.append((hd, g))
            for n in range(g - 1, -1, -1):
                items.append((hd, n))
        n_it = len(items)
        LA = 3
        pend = []
        for i in range(n_it + LA):
            if i < n_it:
                pend.append(self.moba_stage1(g, *items[i]))
            if i >= LA:
                j = i - LA
                self.moba_stage2(g, items[j][0], items[j][1], pend[j])
        for a in range(2):
            bT = self.bank()
            bTv = bT.t[:, :].bitcast(BF16)
            for c in range(4):
                S.op("pe", lambda: pe.transpose(bTv[:, c * 128:(c + 1) * 128], self.obtm[:, a, c * 128:(c + 1) * 128], self.ident),
                     reads=[self.b_obtm, self.b_cmat], writes=[bT.buf])
            S.op("act", lambda: act.activation(out=self.obT[:, :, a * 128:(a + 1) * 128],
                                               in_=bTv[:, 0:512].rearrange("p (c t) -> p c t", t=128), func=AF.Copy),
                 writes=[bT.buf, self.b_obT])

    def moba_stage1(self, g, hd, n):
        S, act = self.S, self.nc.scalar
        pr = hd // 2
        bS = self.bank()
        Q = self.QTz[:, hd, :]
        rd = [self.b_QTz, self.bKT[n]]
        rdb = [self.b_cmat, self.b_cbias]
        BT = self.cbias
        if n == g:
            j0, j1 = 2 * g, 2 * g + 1
            self.mm(bS, bS.t[:, 0:256], self.KT[:, pr, j0 * 128:(j0 + 1) * 128], Q[:, 0:256], True, False, rd)
            self.mm(bS, bS.t[:, 0:256], self.ident, BT[:, hd, 0:256], False, True, rdb)
            self.mm(bS, bS.t[:, 256:384], self.KT[:, pr, j1 * 128:(j1 + 1) * 128], Q[:, 128:256], True, False, rd)
            self.mm(bS, bS.t[:, 256:384], self.ident, BT[:, hd, 0:128], False, True, rdb)
            width = 384
        else:
            j0, j1 = 2 * n, 2 * n + 1
            near = (n == g - 1)
            self.mm(bS, bS.t[:, 0:256], self.KT[:, pr, j0 * 128:(j0 + 1) * 128], Q[:, 0:256], True, not near, rd)
            if near:
                self.mm(bS, bS.t[:, 0:256], self.ident, BT[:, hd, 256:512], False, True, rdb)
            self.mm(bS, bS.t[:, 256:512], self.KT[:, pr, j1 * 128:(j1 + 1) * 128], Q[:, 0:256], True, not near, rd)
            if near:
                self.mm(bS, bS.t[:, 256:512], self.ident, BT[:, hd, 128:384], False, True, rdb)
            width = 512
        pt, ptB = self.bfpR.next()
        S.op("act", lambda: act.activation(out=pt[:, 0:width], in_=bS.t[:, 0:width], func=AF.Exp), writes=[bS.buf, ptB])
        return (pt, ptB)

    def moba_stage2(self, g, hd, n, st):
        S, dve = self.S, self.nc.vector
        pt, ptB = st
        bO = self.bank()
        V = self.Vaug
        if n == g:
            j0, j1 = 2 * g, 2 * g + 1
            rd = [ptB, self.bV[g]]
            self.mm(bO, bO.t[:, 0:65], pt[:, 0:128], V[:, j0, hd, :], True, True, rd)
            self.mm(bO, bO.t[:, 65:130], pt[:, 128:256], V[:, j0, hd, :], True, False, rd)
            self.mm(bO, bO.t[:, 65:130], pt[:, 256:384], V[:, j1, hd, :], False, True, rd)
            S.op("dve", lambda: dve.tensor_copy(out=self.accO[:, :, hd, :], in_=bO.t[:, 0:130].rearrange("p (a c) -> p a c", c=65)),
                 writes=[bO.buf, self.b_accO])
        else:
            j0, j1 = 2 * n, 2 * n + 1
            rd = [ptB, self.bV[n]]
            for a in range(2):
                o = bO.t[:, a * 65:(a + 1) * 65]
                self.mm(bO, o, pt[:, a * 128:(a + 1) * 128], V[:, j0, hd, :], True, False, rd)
                self.mm(bO, o, pt[:, 256 + a * 128:256 + (a + 1) * 128], V[:, j1, hd, :], False, True, rd)
            wt, wtB = (self.sel, self.b_sel) if n == g - 1 else (self.selw, self.b_selw)
            for a in range(2):
                cidx = a * 64 + hd * 8 + n
                S.op("dve", lambda: dve.scalar_tensor_tensor(out=self.accO[:, a, hd, :], in0=bO.t[:, a * 65:(a + 1) * 65],
                                                             scalar=wt[:, cidx:cidx + 1], in1=self.accO[:, a, hd, :],
                                                             op0=ALU.mult, op1=ALU.add),
                     reads=[wtB], writes=[bO.buf, self.b_accO])
        if n == 0:
            S.op("dve", lambda: dve.reciprocal(out=self.smalls[:, 24:26], in_=self.accO[:, :, hd, 64]),
                 reads=[self.b_accO], writes=[self.b_rc])
            for a in range(2):
                S.op("dve", lambda: dve.tensor_scalar(out=self.obtm[:, a, hd * 64:(hd + 1) * 64], in0=self.accO[:, a, hd, 0:64],
                                                      scalar1=self.smalls[:, 24 + a:25 + a], scalar2=None, op0=ALU.mult),
                     reads=[self.b_accO, self.b_rc], writes=[self.b_obtm])

    def ffn_group(self, l, g):
        S, nc = self.S, self.nc
        act, dve = nc.scalar, nc.vector
        t0 = g * TG
        h, hB = self.h, self.b_h
        self.chk(9)
        self.rmsnorm(g, CV_NFFN + l * 8)
        accb = self.banks[4:8]
        for b in accb:
            b.fresh = True
        rot = Rot(self.banks[0:4])
        u0 = self.w_used
        slots = {}

        pool = nc.gpsimd
        st = {}

        def s1(ct):
            w, wB = self.w_get(0)
            bk = rot.next()
            bk.fresh = True
            for k in range(8):
                self.mm(bk, bk.t[:, 0:TG], w[:, k * 128:(k + 1) * 128], h[:, k, :], k == 0, k == 7, [wB, hB])
            for k in range(8):
                self.mm(bk, bk.t[:, TG:2 * TG], w[:, 1024 + k * 128:1024 + (k + 1) * 128], h[:, k, :],
                        k == 0, k == 7, [wB, hB])
            ue, ueB, ueH = self.smfR.next()
            S.op("dve", lambda: dve.tensor_copy(out=ue[:, :, 0:2], in_=self.carry[:, :, ct, :]), reads=[self.b_carry], writes=[ueH])
            S.op("act", lambda: act.activation(out=ue[:, :, 2:258], in_=bk.t[:, :].rearrange("p (a t) -> p a t", t=TG), func=AF.Copy),
                 writes=[bk.buf, ueB])
            st[ct] = dict(ue=ue, ueB=ueB, ueH=ueH)
            self.w_done()

        def s2(ct):
            ue, ueB, ueH = st[ct]["ue"], st[ct]["ueB"], st[ct]["ueH"]
            S.op("dve", lambda: dve.tensor_copy(out=self.carry[:, :, ct, :], in_=ue[:, :, 256:258]), reads=[ueB], writes=[self.b_carry])
            cc, ccBs = self.bigfR.next()
            ccB, ccB2 = ccBs
            st[ct].update(cc=cc, ccB=ccB, ccB2=ccB2)
            for ab in range(2):
                cbuf = ccB if ab == 0 else ccB2
                cb = CV_CW + ((l * 2 + ab) * 22 + ct) * 4
                co = cc[:, ab * TG:(ab + 1) * TG]
                wr = [cbuf]
                S.op("pool", lambda: pool.tensor_scalar(out=co, in0=ue[:, ab, 2:258], scalar1=self.cvec[:, cb + 2:cb + 3],
                                                        scalar2=self.cvec[:, cb + 3:cb + 4], op0=ALU.mult, op1=ALU.add),
                     reads=[ueB, self.b_cvec], writes=wr)
            for ab in range(2):
                cbuf = ccB if ab == 0 else ccB2
                cb = CV_CW + ((l * 2 + ab) * 22 + ct) * 4
                co = cc[:, ab * TG:(ab + 1) * TG]
                wr = [cbuf]
                S.op("dve", lambda: dve.scalar_tensor_tensor(out=co, in0=ue[:, ab, 1:257], scalar=self.cvec[:, cb + 1:cb + 2], in1=co,
                                                             op0=ALU.mult, op1=ALU.add),
                     reads=[ueB, ueH, self.b_cvec], writes=wr)
                S.op("dve", lambda: dve.scalar_tensor_tensor(out=co, in0=ue[:, ab, 0:256], scalar=self.cvec[:, cb:cb + 1], in1=co,
                                                             op0=ALU.mult, op1=ALU.add),
                     reads=[ueB, ueH, self.b_cvec], writes=wr)

        def s3(ct):
            cc, ccB = st[ct]["cc"], st[ct]["ccB"]
            S.op("act", lambda: act.activation(out=cc[:, 0:TG], in_=cc[:, 0:TG], func=AF.Silu), reads=[ccB], writes=[ccB])

        def s4(ct):
            cc, ccB = st[ct]["cc"], st[ct]["ccB"]
            at, atB = self.bfpR.next()
            S.op("dve", lambda: dve.tensor_tensor(out=at[:, 0:TG], in0=cc[:, 0:TG], in1=cc[:, TG:2 * TG], op=ALU.mult),
                 reads=[ccB, st[ct]["ccB2"]], writes=[atB])
            st[ct].update(at=at, atB=atB)

        def s5(ct):
            w, wB = self.d_get(0)
            at, atB = st[ct]["at"], st[ct]["atB"]
            for dtile in range(8):
                ab_ = accb[dtile // 2]
                o = ab_.t[:, (dtile % 2) * TG:(dtile % 2 + 1) * TG]
                self.mm(ab_, o, w[:, dtile * 128:(dtile + 1) * 128], at[:, 0:TG],
                        ct == 0, ct == 21, [wB, atB])
            self.d_done()
            del st[ct]

        for i in range(22 + 3):
            if i < 22:
                s1(i)
            if 0 <= i - 1 < 22:
                s3(i - 1)
            if i < 22:
                s2(i)
            if 0 <= i - 1 < 22:
                s4(i - 1)
            if 0 <= i - 3 < 22:
                s5(i - 3)
        self.chk(10)
        for i in range(4):
            S.op("dve", lambda: dve.tensor_tensor(out=self.X[:, 2 * i:2 * i + 2, t0:t0 + TG], in0=self.X[:, 2 * i:2 * i + 2, t0:t0 + TG],
                                                  in1=accb[i].t[:, :].rearrange("p (j t) -> p j t", t=TG), op=ALU.add),
                 writes=[accb[i].buf, self.bX[g]])

    def final_group(self, g):
        S, sp = self.S, self.nc.sync
        t0 = g * TG
        self.rmsnorm(g, CV_NFIN, inplace=True)
        ov = self.d_out.rearrange("(k p) t -> p k t", p=128)
        S.dma("sp", lambda: sp.dma_start(out=ov[:, :, t0:t0 + TG], in_=self.X[:, :, t0:t0 + TG]), "out%d" % (g % 2),
              reads=[self.bX[g]])


def build_program(n_layers=DEPTH, n_groups=NG):
    prog = Prog(n_layers, n_groups)
    s1 = Sched(prog.nc, None)
    prog.emit(s1)
    s2 = Sched(prog.nc, s1.need)
    prog.emit(s2)
    prog.stats = dict(idx=dict(s2.idx), incs=dict(s2.incs), waits=s2.n_wait, sbuf_left=prog.sbuf_left)
    return prog


def _t5_bucket(n):
    n = np.maximum(n, 0)
    nf = np.maximum(n, 1).astype(np.float32)
    large = 16 + (np.log(nf / np.float32(16.0)) / np.float32(math.log(128 / 16)) * np.float32(16)).astype(np.int32)
    large = np.minimum(large, 31)
    return np.where(n < 16, n, large)


def _blk(w, c0, nc_):
    K = w.shape[0] // 128
    return np.ascontiguousarray(w[:, c0:c0 + nc_].reshape(K, 128, nc_).transpose(1, 0, 2)).reshape(128, K * nc_)


def prep_shared(inp, n_layers=DEPTH):
    f32 = np.float32
    L = n_layers
    w_in = np.asarray(inp["w_in"], f32)
    wa = np.zeros((L * NBA, 128, WA_E), f32)
    wf = np.zeros((L * NBF, 128, WSLOT), f32)
    walr = np.zeros((L, 128, 128), f32)
    for l in range(L):
        wi = w_in[l]
        blocks = []
        blocks += [_blk(wi, O_KA + q * 256, 256) for q in range(2)]
        for hp in range(2):
            blocks += [_blk(wi, O_QA + hp * 256, 256), _blk(wi, O_KA + hp * 256, 256)]
        blocks += [_blk(wi, O_VA + q * 256, 256) for q in range(4)]
        blocks += [_blk(wi, O_QB + q * 256, 256) for q in range(2)]
        blocks += [_blk(wi, O_KB + q * 256, 256) for q in range(2)]
        blocks += [_blk(wi, O_VB + q * 256, 256) for q in range(2)]
        blocks += [_blk(wi, O_RA + q * 256, 256) for q in range(4)]
        wbg = np.asarray(inp["w_branch_gla"][l], f32)
        wbm = np.asarray(inp["w_branch_moba"][l], f32)
        for dtile in range(8):
            c0 = dtile * 128
            m = np.zeros((128, WA_E), f32)
            m[:, 0:1024] = _blk(wbg, c0, 128)
            m[:, 1024:1536] = _blk(wbm, c0, 128)
            blocks.append(m)
            m = np.zeros((128, WA_E), f32)
            m[:, 0:1024] = _blk(wi, O_G + c0, 128)
            m[:, 1024:2048] = _blk(wi, O_G + 1024 + c0, 128)
            blocks.append(m)
        wo = np.asarray(inp["w_out"][l], f32)
        blocks += [_blk(wo, q * 256, 256) for q in range(4)]
        assert len(blocks) == NBA
        for i, bb in enumerate(blocks):
            wa[l * NBA + i] = bb
        walr[l] = _blk(wi, O_ALR, 16)
        wu = np.asarray(inp["w_up"][l], f32)
        wd = np.asarray(inp["w_down"][l], f32)
        for ct in range(NBF):
            wf[l * NBF + ct, :, 0:1024] = _blk(wu, ct * 128, 128)
            wf[l * NBF + ct, :, 1024:2048] = _blk(wu, D_FF + ct * 128, 128)
            wf[l * NBF + ct, :, 2048:3072] = wd[ct * 128:(ct + 1) * 128, :]
    wlr = np.zeros((17, DEPTH * 512), f32)
    for l in range(L):
        wlr[0:16, l * 512:(l + 1) * 512] = np.asarray(inp["w_lr_up"][l], f32)
        wlr[16, l * 512:(l + 1) * 512] = np.asarray(inp["b_forget"][l], f32)
    cvec = np.zeros((128, CV_N), f32)
    for l in range(L):
        cvec[:, CV_NMIX + l * 8:CV_NMIX + (l + 1) * 8] = np.asarray(inp["norm_mix"][l], f32).reshape(8, 128).T
        cvec[:, CV_NFFN + l * 8:CV_NFFN + (l + 1) * 8] = np.asarray(inp["norm_ffn"][l], f32).reshape(8, 128).T
        cvec[:, CV_GLA + l * 8:CV_GLA + (l + 1) * 8] = np.asarray(inp["gla_out_norm"][l], f32).reshape(8, 128).T
        cw = np.asarray(inp["conv_w"][l], f32)
        cb = np.asarray(inp["conv_b"][l], f32)
        for ab in range(2):
            full = np.concatenate([cw[:, ab * D_FF:(ab + 1) * D_FF], cb[None, ab * D_FF:(ab + 1) * D_FF]], axis=0)
            arr = full.reshape(4, 22, 128).transpose(2, 1, 0)
            c0 = CV_CW + (l * 2 + ab) * 22 * 4
            cvec[:, c0:c0 + 88] = arr.reshape(128, 88)
    cvec[:, CV_NFIN:CV_NFIN + 8] = np.asarray(inp["norm_final"], f32).reshape(8, 128).T
    rb = np.asarray(inp["rel_bias"], f32)
    kk = np.arange(128)[:, None]
    qq = np.arange(128)[None, :]
    cbias = np.zeros((128, 8, 512), f32)
    bd = _t5_bucket(qq - kk)
    bs = _t5_bucket(128 + qq - kk)
    for hd in range(8):
        diag = rb[bd, hd]
        cbias[:, hd, 0:128] = np.where(qq >= kk, diag, np.float32(NEG))
        cbias[:, hd, 128:256] = rb[bs, hd]
        cbias[:, hd, 256:512] = rb[31, hd]
    cmisc = np.zeros((128, CM_N), f32)
    cmisc[:, CM_B31:CM_B31 + 64] = np.repeat(rb[31, :], 8)[None, :]
    cmat = np.zeros((128, CMAT_N), f32)
    s = np.arange(128)[:, None]
    t = np.arange(128)[None, :]
    cmat[:, 0:128] = np.eye(128, dtype=f32)
    cmat[:, 128:256] = 1.0 / 1024.0
    cmat[:, 256:384] = (s <= t).astype(f32)
    cmat[:, 384:512] = np.where(s <= t, -1.0 / 16.0, 0.0)
    cmat[:, 512:640] = np.where(s > t, -1.0 / 16.0, 0.0)
    return dict(wa=wa, wf=wf, walr=walr, wlr=wlr, cvec=cvec, cbias=cbias.reshape(128, 8 * 512), cmisc=cmisc, cmat=cmat)


_PROG_CACHE = {}


def kernel(x, rel_bias, norm_mix, w_in, w_lr_up, b_forget, gla_out_norm, w_branch_gla, w_branch_moba, w_out,
           norm_ffn, w_up, conv_w, conv_b, w_down, norm_final):
    inp = dict(rel_bias=rel_bias, norm_mix=norm_mix, w_in=w_in, w_lr_up=w_lr_up, b_forget=b_forget,
               gla_out_norm=gla_out_norm, w_branch_gla=w_branch_gla, w_branch_moba=w_branch_moba, w_out=w_out,
               norm_ffn=norm_ffn, w_up=w_up, conv_w=conv_w, conv_b=conv_b, w_down=w_down, norm_final=norm_final)
    x = np.asarray(x, np.float32)
    Bn = x.shape[0]
    shared = prep_shared(inp)
    prog = build_program()
    in_maps = []
    for b in range(Bn):
        m = dict(shared)
        m["xT"] = np.ascontiguousarray(x[b].T)
        in_maps.append(m)
    res = run_bass_kernel_spmd(prog.nc, in_maps, core_ids=list(range(Bn)))
    out = np.stack([np.ascontiguousarray(r["outT"].T) for r in res.results], axis=0)
    return out.astype(np.float32)
```

```python
import math
import os
import numpy as np
import concourse.bass as bass
import concourse.mybir as mybir
from concourse.bass_utils import run_bass_kernel_spmd

F32 = mybir.dt.float32
BF16 = mybir.dt.bfloat16
AF = mybir.ActivationFunctionType
ALU = mybir.AluOpType

D = 1024
T = 2048
DEPTH = 4
TG = 256
NG = T // TG
D_FF = 2816
EPS = 1e-6
NEG = -30000.0
NBA = 40
NBF = 22
WA_E = 2048
WSLOT = 3072
NSLOT = 5
NDSLOT = 6
O_QA, O_KA, O_VA, O_RA, O_ALR, O_QB, O_KB, O_VB, O_G = 0, 512, 1024, 2048, 3072, 3088, 3600, 4112, 4624

CV_NMIX = 0
CV_NFFN = DEPTH * 8
CV_GLA = 2 * DEPTH * 8
CV_NFIN = 3 * DEPTH * 8
CV_CW = 3 * DEPTH * 8 + 8
CV_N = CV_CW + DEPTH * 2 * 22 * 4
CM_B31 = 0
CM_NEG = 64
CM_N = 64
CMAT_N = 5 * 128


class Buf:
    __slots__ = ("name", "w", "r")

    def __init__(self, name):
        self.name = name
        self.w = {}
        self.r = {}


class Sched:
    ENGS = ("pe", "act", "dve", "pool", "sp")

    def __init__(self, nc, plan):
        self.nc = nc
        self.plan = plan
        self.real = plan is not None
        self.idx = {e: 0 for e in self.ENGS}
        self.incs = {e: 0 for e in self.ENGS}
        self.val = {}
        self.waited = {e: {} for e in self.ENGS}
        self.need = set()
        self.dcount = {}
        self.dsem = {}
        self.n_wait = 0
        if self.real:
            self.eng = {"pe": nc.tensor, "act": nc.scalar, "dve": nc.vector, "pool": nc.gpsimd, "sp": nc.sync}
            self.esem = {e: nc.alloc_semaphore("sem_" + e) for e in self.ENGS}

    def _dsem(self, name):
        if name not in self.dsem:
            self.dsem[name] = self.nc.alloc_semaphore("dsem_" + name) if self.real else None
            self.dcount[name] = 0
        return self.dsem[name]

    def _wait(self, eng, t):
        if t[0] == "e":
            _, f, i = t
            if f == eng:
                if eng == "pe" or self.idx[eng] - i > 6:
                    return
            if self.waited[eng].get(f, -1) >= i:
                return
            self.waited[eng][f] = i
            self.need.add((f, i))
            if self.real:
                self.eng[eng].wait_ge(self.esem[f], self.val[(f, i)])
                self.n_wait += 1
        else:
            _, name, n = t
            key = ("d", name)
            if self.waited[eng].get(key, 0) >= n:
                return
            self.waited[eng][key] = n
            if self.real:
                self.eng[eng].wait_ge(self.dsem[name], 16 * n)
                self.n_wait += 1

    def _deps(self, eng, reads, writes):
        for b in reads:
            for t in b.w.values():
                self._wait(eng, t)
        for b in writes:
            for t in b.w.values():
                self._wait(eng, t)
            for t in b.r.values():
                self._wait(eng, t)

    def op(self, eng, fn, reads=(), writes=()):
        self._deps(eng, reads, writes)
        i = self.idx[eng]
        if self.real:
            ins = fn()
            if (eng, i) in self.plan:
                self.incs[eng] += 1
                ins.then_inc(self.esem[eng], 1)
                self.val[(eng, i)] = self.incs[eng]
        self.idx[eng] = i + 1
        t = ("e", eng, i)
        for b in writes:
            b.w[eng] = t
        for b in reads:
            b.r[eng] = t
        return t

    def dma(self, q, fn, sem, reads=(), writes=()):
        self._deps(q, reads, writes)
        self._dsem(sem)
        self.dcount[sem] += 1
        n = self.dcount[sem]
        if self.real:
            fn().then_inc(self.dsem[sem], 16)
        self.idx[q] += 1
        t = ("d", sem, n)
        for b in writes:
            b.w["d:" + sem] = t
        for b in reads:
            b.r["d:" + sem] = t
        return t

    def wait_all_dma(self, eng, sem):
        self._wait(eng, ("d", sem, self.dcount[sem]))


class StopEmit(Exception):
    pass


class Bank:
    def __init__(self, t, i):
        self.t = t
        self.buf = Buf("bank%d" % i)
        self.fresh = True


class Rot:
    def __init__(self, items):
        self.items = items
        self.i = 0

    def next(self):
        it = self.items[self.i % len(self.items)]
        self.i += 1
        return it


class Prog:
    def __init__(self, n_layers=DEPTH, n_groups=NG):
        self.L = n_layers
        self.NGR = n_groups
        nc = bass.Bass("TRN2", target_bir_lowering=False)
        self.nc = nc
        L = n_layers
        dt = nc.dram_tensor
        self.d_x = dt("xT", [D, T], F32, kind="ExternalInput").ap()
        self.d_wa = dt("wa", [L * NBA, 128, WA_E], F32, kind="ExternalInput").ap()
        self.d_wf = dt("wf", [L * NBF, 128, WSLOT], F32, kind="ExternalInput").ap()
        self.d_walr = dt("walr", [L, 128, 128], F32, kind="ExternalInput").ap()
        self.d_wlr = dt("wlr", [17, DEPTH * 512], F32, kind="ExternalInput").ap()
        self.d_cvec = dt("cvec", [128, CV_N], F32, kind="ExternalInput").ap()
        self.d_cbias = dt("cbias", [128, 8 * 512], F32, kind="ExternalInput").ap()
        self.d_cmisc = dt("cmisc", [128, CM_N], F32, kind="ExternalInput").ap()
        self.d_cmat = dt("cmat", [128, CMAT_N], F32, kind="ExternalInput").ap()
        self.d_out = dt("outT", [D, T], F32, kind="ExternalOutput").ap()
        self.d_wa16 = dt("wa16", [L * NBA, 128, WA_E], BF16).ap()
        self.d_wf16 = dt("wf16", [L * NBF, 128, WSLOT], BF16).ap()
        self.d_walr16 = dt("walr16", [L, 128, 128], BF16).ap()

        A = nc.alloc_sbuf_tensor
        self.X = A("X", [128, 8, T], F32)
        self.KT = A("KT", [128, 4, T], BF16)
        self.Vaug = A("Vaug", [128, 16, 8, 65], BF16)
        self.Sf = A("Sf", [128, 4, 256], F32)
        self.Sbf = A("Sbf", [128, 4, 256], BF16)
        self.wslot = [A("wslot%d" % i, [128, WA_E], BF16) for i in range(NSLOT)]
        self.dslot = [A("dslot%d" % i, [128, 1024], BF16) for i in range(NDSLOT)]
        self.alrw = [A("alrw%d" % i, [128, 128], BF16) for i in range(2)]
        self.h = A("h", [128, 8, TG], BF16)
        self.rstd = A("rstd", [128, TG], F32)
        self.qdec = A("qdec", [128, 4, TG], BF16)
        self.kdec = A("kdec", [128, 4, TG], BF16)
        self.kte = A("kte", [128, 2, 512], BF16)
        self.vtm = A("vtm", [128, 2, 1024], BF16)
        self.sra = A("sra", [128, 8, TG], BF16)
        self.QTz = A("QTz", [128, 8, TG], BF16)
        self.alrT = A("alrT", [17, TG], BF16)
        self.lhi = A("lhi", [128, 2, 512], BF16)
        self.llo = A("llo", [128, 2, 512], BF16)
        self.obtm = self.lhi
        self.mixed = self.sra
        self.obT = self.llo[:, :, :].rearrange("p a (b t) -> p (a b) t", t=TG)
        self.oaT = A("oaT", [128, 8, TG], BF16)
        self.accO = A("accO", [128, 2, 8, 65], F32)
        self.gsb = A("gsb", [128, 128], F32)
        self.sel = A("sel", [128, 128], F32)
        self.selw = A("selw", [128, 128], F32)
        self.top8 = A("top8", [128, 16, 8], F32)
        self.smalls = A("smalls", [128, 64], F32)
        self.ksb = A("ksb", [128, 4, 8], BF16)
        self.carry = A("carry", [128, 2, 22, 2], F32)
        self.bigf = [A("bigf%d" % i, [128, 512], F32) for i in range(3)]
        self.smf = [A("smf%d" % i, [128, 2, 258], F32) for i in range(3)]
        self.bfp = [A("bfp%d" % i, [128, 512], BF16) for i in range(4)]
        self.g_at = A("g_at", [128, 4, 128], BF16)
        self.g_oa = A("g_oa", [128, 4, 256], BF16)
        self.cvec = A("cvec_s", [128, CV_N], F32)
        self.cbias = A("cbias_s", [128, 8, 512], BF16)
        self.cmisc = A("cmisc_s", [128, CM_N], F32)
        self.cmat = A("cmat_s", [128, CMAT_N], BF16)
        self.wlr = A("wlr_s", [17, 512], BF16)
        self.expb31 = A("expb31", [128, 64], F32)
        self.banks_t = [nc.alloc_psum_tensor("bank%d" % i, [128, 512], F32) for i in range(8)]
        self.sbuf_left = nc.sbuf_bytes_remaining

    def emit(self, S):
        self.S = S
        nc = self.nc
        self.banks = [Bank(t, i) for i, t in enumerate(self.banks_t)]
        self.P = Rot(self.banks)
        self.bigfR = Rot([(t, [Buf("bigf%da" % i), Buf("bigf%db" % i)]) for i, t in enumerate(self.bigf)])
        self.smfR = Rot([(t, Buf("smf%d" % i), Buf("smfh%d" % i)) for i, t in enumerate(self.smf)])
        bfl = [(t, Buf("bfp%d" % i)) for i, t in enumerate(self.bfp)]
        self.bfpR = Rot(bfl[0:4])
        self.b_gat = [Buf("gat%d" % i) for i in range(4)]
        self.b_goa = [Buf("goa%d" % i) for i in range(4)]
        B = Buf
        self.bX = [B("X%d" % g) for g in range(NG)]
        self.bKT = [B("KT%d" % g) for g in range(NG)]
        self.bV = [B("V%d" % g) for g in range(NG)]
        self.bS = [B("S%d" % i) for i in range(4)]
        self.bSb = [B("Sb%d" % i) for i in range(4)]
        self.bW = [B("wslot%d" % i) for i in range(NSLOT)]
        self.bD = [B("dslot%d" % i) for i in range(NDSLOT)]
        self.bAlrw = [B("alrw0"), B("alrw1")]
        self.b_h = [Buf("h%d" % k) for k in range(8)]
        for n in ("rstd", "qdec", "kdec", "kte", "vtm", "sra", "QTz", "alrT", "lhi", "llo", "eTM", "oaT",
                  "obtm", "obT", "mixed", "accO", "gsb", "sel", "selw", "top8", "dec", "ss", "ss2", "rc", "ksumf",
                  "ksb", "carry", "cvec", "cbias", "cmisc", "cmat", "wlr", "expb31"):
            setattr(self, "b_" + n, B(n))
        self.b_obtm = self.b_lhi
        self.b_mixed = self.b_sra
        self.b_obT = self.b_llo
        self.wlist = []
        for l in range(self.L):
            for g in range(self.NGR):
                self.wlist.append(("alr", l))
                for i in range(NBA):
                    self.wlist.append(("a", l * NBA + i))
                for i in range(NBF):
                    self.wlist.append(("f", l * NBF + i))
                    self.wlist.append(("d", l * NBF + i))
        self.w_issued = 0
        self.w_used = 0
        self.alr_n = 0
        self.big_n = 0
        self.d_used = 0
        self.d_n = 0

        self.stop = int(os.environ.get("KSTOP", "99"))
        self.phases = []
        self.init_phase()
        try:
            for l in range(self.L):
                self.layer_init(l)
                for g in range(self.NGR):
                    self.mixer_group(l, g)
                    self.ffn_group(l, g)
                    if l == self.L - 1:
                        self.final_group(g)
        except StopEmit:
            self.final_group(0)
        for sem in ["w%d" % i for i in range(NSLOT)] + ["d%d" % i for i in range(NDSLOT)] + ["alr0", "alr1", "c_wlr", "c_bias", "c_mat", "c_vec", "c_misc"] + ["cv%d" % i for i in range(DEPTH)] + ["cv0_alr", "cv0_a0", "cv0_a1", "cv0_a2", "cv0_a3", "cv0_f0", "cv0_f1"]:
            if sem in S.dcount:
                S.wait_all_dma("sp", sem)
        for sem in ("out0", "out1"):
            if sem in S.dcount:
                S.wait_all_dma("sp", sem)

    def w_issue(self):
        S = self.S
        while self.w_issued < len(self.wlist):
            kind, idx = self.wlist[self.w_issued]
            if kind == "alr":
                j = self.alr_n % 2
                tl, bf, src = self.alrw[j], self.bAlrw[j], self.d_walr16[idx]
                S.dma("sp", lambda: self.nc.sync.dma_start(out=tl[:], in_=src), "alr%d" % j, reads=[self.cvt[("alr", idx)]], writes=[bf])
                self.alr_n += 1
            elif kind == "d":
                if self.d_n >= self.d_used + NDSLOT:
                    return
                j = self.d_n % NDSLOT
                tl, bf, src = self.dslot[j], self.bD[j], self.d_wf16[idx][:, 2048:3072]
                S.dma("sp", lambda: self.nc.sync.dma_start(out=tl[:], in_=src), "d%d" % j, reads=[self.cvt[("f", idx)]], writes=[bf])
                self.d_n += 1
            else:
                if self.big_n >= self.w_used + NSLOT:
                    return
                j = self.big_n % NSLOT
                tl, bf = self.wslot[j], self.bW[j]
                if kind == "a":
                    src = self.d_wa16[idx]
                    S.dma("sp", lambda: self.nc.sync.dma_start(out=tl[:], in_=src), "w%d" % j,
                          reads=[self.cvt[("a", idx)]], writes=[bf])
                else:
                    src = self.d_wf16[idx][:, 0:2048]
                    S.dma("sp", lambda: self.nc.sync.dma_start(out=tl[:], in_=src), "w%d" % j,
                          reads=[self.cvt[("f", idx)]], writes=[bf])
                self.big_n += 1
            self.w_issued += 1

    def w_get(self, off=0):
        assert self.big_n > self.w_used + off, "weight block not issued (ring too small for this access pattern)"
        j = (self.w_used + off) % NSLOT
        return self.wslot[j], self.bW[j]

    def w_done(self):
        self.w_used += 1
        self.w_issue()

    def d_get(self, off=0):
        assert self.d_n > self.d_used + off, "down block not issued"
        j = (self.d_used + off) % NDSLOT
        return self.dslot[j], self.bD[j]

    def d_done(self):
        self.d_used += 1
        self.w_issue()

    def mm(self, bank, out, lhsT, rhs, first, last, reads):
        st = bool(first and bank.fresh)
        if first:
            bank.fresh = False
        self.S.op("pe", lambda: self.nc.tensor.matmul(out, lhsT, rhs, start=st, stop=bool(last), skip_group_check=True),
                  reads=reads, writes=[bank.buf])

    def chk(self, p):
        if self.S.real:
            self.phases.append((p, self.S.idx["pe"]))
        if p >= self.stop:
            raise StopEmit()

    def bank(self):
        b = self.P.next()
        b.fresh = True
        return b

    def init_phase(self):
        S, nc = self.S, self.nc
        act, dve, sp, pool = nc.scalar, nc.vector, nc.sync, nc.gpsimd
        S.dma("sp", lambda: sp.dma_start(out=self.cvec[:], in_=self.d_cvec), "c_vec", writes=[self.b_cvec])
        S.dma("sp", lambda: sp.dma_start(out=self.cmisc[:], in_=self.d_cmisc), "c_misc", writes=[self.b_cmisc])
        S.dma("pool", lambda: pool.dma_start(out=self.cmat[:], in_=self.d_cmat), "c_mat", writes=[self.b_cmat])
        S.dma("pool", lambda: pool.dma_start(out=self.cbias[:], in_=self.d_cbias.rearrange("p (h c) -> p h c", c=512)),
              "c_bias", writes=[self.b_cbias])
        xv = self.d_x.rearrange("(k p) t -> p k t", p=128)
        for g in range(self.NGR):
            S.dma("sp", lambda: sp.dma_start(out=self.X[:, :, g * TG:(g + 1) * TG], in_=xv[:, :, g * TG:(g + 1) * TG]),
                  "x%d" % g, writes=[self.bX[g]])
        self.cvt = {}
        b0 = Buf("cvt0_alr")
        S.dma("pool", lambda: pool.dma_start(out=self.d_walr16[0], in_=self.d_walr[0]), "cv0_alr", writes=[b0])
        self.cvt[("alr", 0)] = b0
        for c in range(4):
            bc = Buf("cvt0_a%d" % c)
            S.dma("pool", lambda: pool.dma_start(out=self.d_wa16[c * 10:(c + 1) * 10], in_=self.d_wa[c * 10:(c + 1) * 10]),
                  "cv0_a%d" % c, writes=[bc])
            for i in range(c * 10, (c + 1) * 10):
                self.cvt[("a", i)] = bc
        for c in range(2):
            bc = Buf("cvt0_f%d" % c)
            S.dma("pool", lambda: pool.dma_start(out=self.d_wf16[c * 11:(c + 1) * 11], in_=self.d_wf[c * 11:(c + 1) * 11]),
                  "cv0_f%d" % c, writes=[bc])
            for i in range(c * 11, (c + 1) * 11):
                self.cvt[("f", i)] = bc
        self.w_issue()
        S.op("dve", lambda: dve.memset(self.Vaug[:], 1.0), writes=self.bV)
        S.op("dve", lambda: dve.memset(self.alrT[:], 1.0), writes=[self.b_alrT])
        S.op("dve", lambda: dve.memset(self.QTz[:], 0.0), writes=[self.b_QTz])
        S.op("dve", lambda: dve.memset(self.ksb[:], 0.0), writes=[self.b_ksb])
        S.op("act", lambda: act.activation(out=self.expb31[:], in_=self.cmisc[:, CM_B31:CM_B31 + 64], func=AF.Exp),
             reads=[self.b_cmisc], writes=[self.b_expb31])
        self.ident = self.cmat[:, 0:128]
        self.ones_m = self.cmat[:, 128:256]
        self.maskU = self.cmat[:, 256:384]
        self.Mle = self.cmat[:, 384:512]
        self.Mgt = self.cmat[:, 512:640]

    def layer_init(self, l):
        S, dve = self.S, self.nc.vector
        S.dma("pool", lambda: self.nc.gpsimd.dma_start(out=self.wlr[:], in_=self.d_wlr[:, l * 512:(l + 1) * 512]), "c_wlr",
              writes=[self.b_wlr])
        S.op("dve", lambda: dve.memset(self.Sf[:], 0.0), writes=self.bS)
        S.op("dve", lambda: dve.memset(self.Sbf[:], 0.0), writes=self.bSb)
        S.op("dve", lambda: dve.memset(self.carry[:], 0.0), writes=[self.b_carry])

    def rmsnorm(self, g, col0, inplace=False):
        S, nc = self.S, self.nc
        act, dve = nc.scalar, nc.vector
        t0 = g * TG
        Xg = self.X[:, :, t0:t0 + TG]
        S.op("act", lambda: act.activation(out=self.h[:], in_=Xg, func=AF.Square), reads=[self.bX[g]], writes=self.b_h)
        bk = self.bank()
        for k in range(8):
            self.mm(bk, bk.t[:, 0:TG], self.ones_m, self.h[:, k, :], k == 0, k == 7, [self.b_h[k], self.b_cmat])
        S.op("act", lambda: act.activation(out=self.rstd[:], in_=bk.t[:, 0:TG], func=AF.Ln, bias=EPS),
             writes=[bk.buf, self.b_rstd])
        S.op("act", lambda: act.activation(out=self.rstd[:], in_=self.rstd[:], func=AF.Exp, scale=-0.5),
             reads=[self.b_rstd], writes=[self.b_rstd])
        for k in range(8):
            if inplace:
                S.op("dve", lambda: dve.scalar_tensor_tensor(out=self.X[:, k, t0:t0 + TG], in0=self.X[:, k, t0:t0 + TG],
                                                             scalar=self.cvec[:, col0 + k:col0 + k + 1], in1=self.rstd[:],
                                                             op0=ALU.mult, op1=ALU.mult),
                     reads=[self.b_rstd, self.b_cvec], writes=[self.bX[g]])
            else:
                S.op("dve", lambda: dve.scalar_tensor_tensor(out=self.h[:, k, :], in0=self.X[:, k, t0:t0 + TG],
                                                             scalar=self.cvec[:, col0 + k:col0 + k + 1], in1=self.rstd[:],
                                                             op0=ALU.mult, op1=ALU.mult),
                     reads=[self.bX[g], self.b_rstd, self.b_cvec], writes=[self.b_h[k]])

    def convert_layer(self, l):
        S, pool = self.S, self.nc.gpsimd
        bc = Buf("cvt%d" % l)
        gate = [self.bX[0]]
        S.dma("pool", lambda: pool.dma_start(out=self.d_walr16[l], in_=self.d_walr[l]), "cv%d" % l, reads=gate, writes=[bc])
        S.dma("pool", lambda: pool.dma_start(out=self.d_wa16[l * NBA:(l + 1) * NBA], in_=self.d_wa[l * NBA:(l + 1) * NBA]),
              "cv%d" % l, reads=gate, writes=[bc])
        S.dma("pool", lambda: pool.dma_start(out=self.d_wf16[l * NBF:(l + 1) * NBF], in_=self.d_wf[l * NBF:(l + 1) * NBF]),
              "cv%d" % l, reads=gate, writes=[bc])
        self.cvt[("alr", l)] = bc
        for i in range(l * NBA, (l + 1) * NBA):
            self.cvt[("a", i)] = bc
        for i in range(l * NBF, (l + 1) * NBF):
            self.cvt[("f", i)] = bc

    def mixer_group(self, l, g):
        S, nc = self.S, self.nc
        act, dve = nc.scalar, nc.vector
        t0 = g * TG
        if g == min(1, self.NGR - 1) and l + 1 < self.L:
            self.convert_layer(l + 1)
        h, hB = self.h, self.b_h
        self.rmsnorm(g, CV_NMIX + l * 8)
        self.chk(0)

        ja = (l * self.NGR + g) % 2
        alrw, alrwB = self.alrw[ja], self.bAlrw[ja]
        bk = self.bank()
        for k in range(8):
            self.mm(bk, bk.t[0:16, 0:TG], alrw[:, k * 16:(k + 1) * 16], h[:, k, :], k == 0, k == 7, [alrwB, hB[k]])
        S.op("act", lambda: act.activation(out=self.alrT[0:16, :], in_=bk.t[0:16, 0:TG], func=AF.Copy),
             writes=[bk.buf, self.b_alrT])
        eTMs = []
        for a in range(2):
            bk = self.bank()
            self.mm(bk, bk.t[:, :], self.alrT[:, a * 128:(a + 1) * 128], self.wlr[:, :], True, True,
                    [self.b_alrT, self.b_wlr])
            e, eB = self.bigfR.next()
            S.op("act", lambda: act.activation(out=e[:], in_=bk.t[:, :], func=AF.Exp, scale=-1.0), writes=[bk.buf] + eB)
            S.op("act", lambda: act.activation(out=e[:], in_=e[:], func=AF.Ln, bias=1.0), reads=eB, writes=eB)
            S.op("dve", lambda: dve.tensor_copy(out=self.lhi[:, a, :], in_=e[:]), reads=eB, writes=[self.b_lhi])
            S.op("dve", lambda: dve.tensor_tensor(out=self.llo[:, a, :], in0=e[:], in1=self.lhi[:, a, :], op=ALU.subtract),
                 reads=eB + [self.b_lhi], writes=[self.b_llo])
            bk2 = self.bank()
            self.mm(bk2, bk2.t[:, :], self.Mgt, self.lhi[:, a, :], True, False, [self.b_cmat, self.b_lhi])
            self.mm(bk2, bk2.t[:, :], self.Mgt, self.llo[:, a, :], False, True, [self.b_cmat, self.b_llo])
            et, etB = self.bigfR.next()
            S.op("act", lambda: act.activation(out=et[:], in_=bk2.t[:, :], func=AF.Exp), writes=[bk2.buf] + etB)
            eTMs.append((et, etB))

        self.chk(1)
        def tm_pair(nblk):
            bks = [self.bank(), self.bank()]
            for q in range(nblk):
                w, wB = self.w_get()
                for a in range(2):
                    for k in range(8):
                        self.mm(bks[a], bks[a].t[:, q * 256:(q + 1) * 256], h[:, k, a * 128:(a + 1) * 128], w[:, k * 256:(k + 1) * 256],
                                k == 0, k == 7, [wB, hB[k]])
                self.w_done()
            return bks

        bks = tm_pair(2)
        for a in range(2):
            bk = bks[a]
            et, etB = eTMs[a]
            S.op("dve", lambda: dve.tensor_tensor(out=self.kte[:, a, :], in0=bk.t[:, :], in1=et[:], op=ALU.mult),
                 reads=etB, writes=[bk.buf, self.b_kte])
        for hp in range(2):
            wq, wqB = self.w_get(0)
            wk, wkB = self.w_get(1)
            for hh in (2 * hp, 2 * hp + 1):
                c = hh % 2
                bkb = self.bank()
                for a in range(2):
                    o = bkb.t[:, a * 128:(a + 1) * 128]
                    self.mm(bkb, o, self.lhi[:, a, hh * 128:(hh + 1) * 128], self.Mle, True, False, [self.b_lhi, self.b_cmat])
                    self.mm(bkb, o, self.llo[:, a, hh * 128:(hh + 1) * 128], self.Mle, False, True, [self.b_llo, self.b_cmat])
                ebt, ebBs = self.bigfR.next()
                eb = ebt[:, 0:TG]
                enb = ebt[:, TG:2 * TG]
                S.op("act", lambda: act.activation(out=eb, in_=bkb.t[:, 0:TG], func=AF.Exp), writes=[bkb.buf] + ebBs)
                S.op("act", lambda: act.activation(out=enb, in_=bkb.t[:, 0:TG], func=AF.Exp, scale=-1.0), writes=[bkb.buf] + ebBs)
                for a in range(2):
                    S.op("dve", lambda: dve.tensor_copy(out=self.smalls[:, hh * 2 + a:hh * 2 + a + 1],
                                                        in_=ebt[:, a * 128 + 127:a * 128 + 128]),
                         reads=ebBs, writes=[self.b_dec])
                bq = self.bank()
                for k in range(8):
                    self.mm(bq, bq.t[:, 0:TG], wq[:, k * 256 + c * 128:k * 256 + (c + 1) * 128], h[:, k, :], k == 0, k == 7, [wqB, hB[k]])
                for k in range(8):
                    self.mm(bq, bq.t[:, TG:2 * TG], wk[:, k * 256 + c * 128:k * 256 + (c + 1) * 128], h[:, k, :], k == 0, k == 7, [wkB, hB[k]])
                S.op("dve", lambda: dve.scalar_tensor_tensor(out=self.qdec[:, hh, :], in0=bq.t[:, 0:TG], scalar=128.0 ** -0.5, in1=eb,
                                                             op0=ALU.mult, op1=ALU.mult),
                     reads=ebBs, writes=[bq.buf, self.b_qdec])
                S.op("dve", lambda: dve.tensor_tensor(out=self.kdec[:, hh, :], in0=bq.t[:, TG:2 * TG], in1=enb, op=ALU.mult),
                     reads=ebBs, writes=[bq.buf, self.b_kdec])
            self.w_done()
            self.w_done()

        self.chk(2)

        for half in range(2):
            bks = tm_pair(2)
            for a in range(2):
                bk = bks[a]
                S.op("act", lambda: act.activation(out=self.vtm[:, a, half * 512:(half + 1) * 512], in_=bk.t[:, :], func=AF.Copy),
                     writes=[bk.buf, self.b_vtm])
        self.chk(3)
        for pr2 in range(2):
            w, wB = self.w_get()
            bk = self.bank()
            for j in range(2):
                for k in range(8):
                    self.mm(bk, bk.t[:, j * TG:(j + 1) * TG], w[:, k * 256 + j * 128:k * 256 + (j + 1) * 128], h[:, k, :],
                            k == 0, k == 7, [wB, hB[k]])
            for j in range(2):
                p = 2 * pr2 + j
                S.op("act", lambda: act.activation(out=self.QTz[0:64, 2 * p, :], in_=bk.t[0:64, j * TG:(j + 1) * TG],
                                                   func=AF.Copy, scale=0.125), writes=[bk.buf, self.b_QTz])
                S.op("act", lambda: act.activation(out=self.QTz[64:128, 2 * p + 1, :], in_=bk.t[64:128, j * TG:(j + 1) * TG],
                                                   func=AF.Copy, scale=0.125), writes=[bk.buf, self.b_QTz])
            self.w_done()
        S.op("dve", lambda: dve.memset(self.smalls[:, 32:64], 0.0), writes=[self.b_ksumf])
        for pr2 in range(2):
            w, wB = self.w_get()
            bk = self.bank()
            for j in range(2):
                for k in range(8):
                    self.mm(bk, bk.t[:, j * TG:(j + 1) * TG], w[:, k * 256 + j * 128:k * 256 + (j + 1) * 128], h[:, k, :],
                            k == 0, k == 7, [wB, hB[k]])
            for j in range(2):
                p = 2 * pr2 + j
                S.op("act", lambda: act.activation(out=self.KT[:, p, t0:t0 + TG], in_=bk.t[:, j * TG:(j + 1) * TG], func=AF.Copy,
                                                   accum_out=self.smalls[:, 32 + p:33 + p]),
                     writes=[bk.buf, self.bKT[g], self.b_ksumf])
            self.w_done()
        S.op("dve", lambda: dve.tensor_copy(out=self.ksb[:, :, g], in_=self.smalls[:, 32:36]),
             reads=[self.b_ksumf], writes=[self.b_ksb])
        bks = tm_pair(2)
        for a in range(2):
            bk = bks[a]
            S.op("act", lambda: act.activation(out=self.Vaug[:, 2 * g + a, :, 0:64],
                                               in_=bk.t[:, :].rearrange("p (h d) -> p h d", d=64), func=AF.Copy),
                 writes=[bk.buf, self.bV[g]])
        self.chk(4)
        for i in range(4):
            w, wB = self.w_get()
            bk = self.bank()
            for j in range(2):
                for k in range(8):
                    self.mm(bk, bk.t[:, j * TG:(j + 1) * TG], w[:, k * 256 + j * 128:k * 256 + (j + 1) * 128], h[:, k, :],
                            k == 0, k == 7, [wB, hB[k]])
            S.op("act", lambda: act.activation(out=self.sra[:, 2 * i:2 * i + 2, :],
                                               in_=bk.t[:, :].rearrange("p (j t) -> p j t", t=TG), func=AF.Silu),
                 writes=[bk.buf, self.b_sra])
            self.w_done()

        self.chk(5)
        self.gla_moba_group(l, g)
        self.chk(7)

        for dtile in range(8):
            wA, wAB = self.w_get(0)
            wG, wGB = self.w_get(1)
            b1 = self.bank()
            for c in range(8):
                self.mm(b1, b1.t[:, 0:TG], wA[:, c * 128:(c + 1) * 128], self.oaT[:, c, :], c == 0, c == 7, [wAB, self.b_oaT])
            for c in range(4):
                self.mm(b1, b1.t[:, TG:2 * TG], wA[:, 1024 + c * 128:1024 + (c + 1) * 128], self.obT[:, c, :], c == 0, c == 3,
                        [wAB, self.b_obT])
            b2 = self.bank()
            for k in range(8):
                self.mm(b2, b2.t[:, 0:TG], wG[:, k * 128:(k + 1) * 128], h[:, k, :], k == 0, k == 7, [wGB, hB[k]])
            for k in range(8):
                self.mm(b2, b2.t[:, TG:2 * TG], wG[:, 1024 + k * 128:1024 + (k + 1) * 128], h[:, k, :], k == 0, k == 7, [wGB, hB[k]])
            sg, sgB = self.bigfR.next()
            S.op("act", lambda: act.activation(out=sg[:], in_=b2.t[:, :], func=AF.Sigmoid), writes=[b2.buf] + sgB)
            S.op("dve", lambda: dve.tensor_tensor(out=sg[:], in0=b1.t[:, :], in1=sg[:], op=ALU.mult), writes=[b1.buf] + sgB)
            S.op("dve", lambda: dve.tensor_tensor(out=self.mixed[:, dtile, :], in0=sg[:, 0:TG], in1=sg[:, TG:2 * TG], op=ALU.add),
                 reads=sgB, writes=[self.b_mixed])
            self.w_done()
            self.w_done()
        self.chk(8)
        for i in range(4):
            w, wB = self.w_get()
            bk = self.bank()
            for j in range(2):
                for k in range(8):
                    self.mm(bk, bk.t[:, j * TG:(j + 1) * TG], w[:, k * 256 + j * 128:k * 256 + (j + 1) * 128], self.mixed[:, k, :],
                            k == 0, k == 7, [wB, self.b_mixed])
            d0 = 2 * i
            S.op("dve", lambda: dve.tensor_tensor(out=self.X[:, d0:d0 + 2, t0:t0 + TG], in0=self.X[:, d0:d0 + 2, t0:t0 + TG],
                                                  in1=bk.t[:, :].rearrange("p (j t) -> p j t", t=TG), op=ALU.add),
                 writes=[bk.buf, self.bX[g]])
            self.w_done()

    def gla_group(self, l, g, mid_hook=None):
        S, nc = self.S, self.nc
        act, dve, pe = nc.scalar, nc.vector, nc.tensor
        S.op("dve", lambda: dve.memset(self.smalls[:, 8:16], 0.0), writes=[self.b_ss])
        H4 = range(4)
        for a in range(2):
            sl = slice(a * 128, (a + 1) * 128)
            vs = [self.vtm[:, a, hh * 256:(hh + 1) * 256] for hh in H4]
            bA = []
            for hh in H4:
                bk = self.bank()
                bA.append(bk)
                self.mm(bk, bk.t[:, 0:128], self.kdec[:, hh, sl], self.qdec[:, hh, sl], True, True, [self.b_kdec, self.b_qdec])
            for hh in H4:
                S.op("dve", lambda: dve.tensor_tensor(out=self.g_at[:, hh, :], in0=bA[hh].t[:, 0:128], in1=self.maskU, op=ALU.mult),
                     reads=[self.b_cmat], writes=[bA[hh].buf, self.b_gat[hh]])
            bO = []
            for hh in H4:
                bk = self.bank()
                bO.append(bk)
                self.mm(bk, bk.t[:, 0:256], self.qdec[:, hh, sl], self.Sbf[:, hh, :], True, False, [self.b_qdec, self.bSb[hh]])
                self.mm(bk, bk.t[:, 0:256], self.g_at[:, hh, :], vs[hh], False, True, [self.b_gat[hh], self.b_vtm])
                self.mm(bk, bk.t[:, 256:512], self.kte[:, a, hh * 128:(hh + 1) * 128], vs[hh], True, True, [self.b_kte, self.b_vtm])
            for hh in H4:
                col = hh * 2 + a
                S.op("dve", lambda: dve.scalar_tensor_tensor(out=self.Sf[:, hh, :], in0=self.Sf[:, hh, :],
                                                             scalar=self.smalls[:, col:col + 1], in1=bO[hh].t[:, 256:512],
                                                             op0=ALU.mult, op1=ALU.add),
                     reads=[self.b_dec], writes=[bO[hh].buf, self.bS[hh]])
            if a == 0 and mid_hook is not None:
                mid_hook(0)
            for hh in H4:
                col = hh * 2 + a
                junk, junkBs = self.bigfR.next()
                S.op("act", lambda: act.activation(out=junk[:, 0:256], in_=bO[hh].t[:, 0:256], func=AF.Square, scale=1.0 / 16.0,
                                                   accum_out=self.smalls[:, 8 + col:9 + col]),
                     writes=[bO[hh].buf, self.b_ss] + junkBs)
            for hh in H4:
                S.op("act", lambda: act.activation(out=self.Sbf[:, hh, :], in_=self.Sf[:, hh, :], func=AF.Copy),
                     reads=[self.bS[hh]], writes=[self.bSb[hh]])
            for hh in H4:
                col = hh * 2 + a
                S.op("act", lambda: act.activation(out=self.smalls[:, 16 + col:17 + col], in_=self.smalls[:, 8 + col:9 + col],
                                                   func=AF.Ln, bias=EPS), reads=[self.b_ss], writes=[self.b_ss2])
            for hh in H4:
                col = hh * 2 + a
                S.op("act", lambda: act.activation(out=self.smalls[:, 16 + col:17 + col], in_=self.smalls[:, 16 + col:17 + col],
                                                   func=AF.Exp, scale=-0.5), reads=[self.b_ss2], writes=[self.b_ss2])
            for hh in H4:
                col = hh * 2 + a
                S.op("dve", lambda: dve.tensor_scalar(out=self.g_oa[:, hh, :], in0=bO[hh].t[:, 0:256],
                                                      scalar1=self.smalls[:, 16 + col:17 + col], scalar2=None, op0=ALU.mult),
                     reads=[self.b_ss2], writes=[bO[hh].buf, self.b_goa[hh]])
            if a == 0 and mid_hook is not None:
                mid_hook(1)
            bT = []
            for hh in H4:
                bk = self.bank()
                bT.append(bk)
                bTv = bk.t[:, :].bitcast(BF16)
                for c in range(2):
                    S.op("pe", lambda: pe.transpose(bTv[:, c * 128:(c + 1) * 128], self.g_oa[:, hh, c * 128:(c + 1) * 128], self.ident),
                         reads=[self.b_goa[hh], self.b_cmat], writes=[bk.buf])
            for hh in H4:
                bTv = bT[hh].t[:, :].bitcast(BF16)
                for c in range(2):
                    ch = 2 * hh + c
                    gcol = CV_GLA + l * 8 + ch
                    S.op("dve", lambda: dve.scalar_tensor_tensor(out=self.oaT[:, ch, sl], in0=bTv[:, c * 128:(c + 1) * 128],
                                                                 scalar=self.cvec[:, gcol:gcol + 1], in1=self.sra[:, ch, sl],
                                                                 op0=ALU.mult, op1=ALU.mult),
                         reads=[self.b_cvec, self.b_sra], writes=[bT[hh].buf, self.b_oaT])

    def moba_gate1(self, l, g):
        S, nc = self.S, self.nc
        dve = nc.vector
        if g >= 1:
            bG = self.bank()
            for a in range(2):
                for hd in range(8):
                    o = bG.t[:, a * 64 + hd * 8:a * 64 + hd * 8 + 8]
                    self.mm(bG, o, self.QTz[:, hd, a * 128:(a + 1) * 128], self.ksb[:, hd // 2, :], True, True,
                            [self.b_QTz, self.b_ksb])
            S.op("dve", lambda: dve.tensor_copy(out=self.gsb[:, :], in_=bG.t[:, 0:128]), writes=[bG.buf, self.b_gsb])
            if g < 8:
                S.op("dve", lambda: dve.memset(self.gsb[:, :].rearrange("p (i n) -> p i n", n=8)[:, :, g:8], -1e30),
                     writes=[self.b_gsb])

    def moba_gate2(self, l, g, part):
        S, nc = self.S, self.nc
        dve = nc.vector
        if g >= 1 and part == 0:
            for i in range(16):
                S.op("dve", lambda: dve.max(out=self.top8[:, i, :], in_=self.gsb[:, i * 8:(i + 1) * 8]),
                     reads=[self.b_gsb], writes=[self.b_top8])
        if g >= 1 and part == 1:
            for i in range(16):
                S.op("dve", lambda: dve.tensor_scalar(out=self.sel[:, i * 8:(i + 1) * 8], in0=self.gsb[:, i * 8:(i + 1) * 8],
                                                      scalar1=self.top8[:, i, 2:3], scalar2=None, op0=ALU.is_ge),
                     reads=[self.b_gsb, self.b_top8], writes=[self.b_sel])
            for a in range(2):
                S.op("dve", lambda: dve.tensor_tensor(out=self.selw[:, a * 64:(a + 1) * 64], in0=self.sel[:, a * 64:(a + 1) * 64],
                                                      in1=self.expb31[:], op=ALU.mult),
                     reads=[self.b_sel, self.b_expb31], writes=[self.b_selw])

    def gla_moba_group(self, l, g):
        S, nc = self.S, self.nc
        act, dve, pe = nc.scalar, nc.vector, nc.tensor
        self.moba_gate1(l, g)
        self.gla_group(l, g, mid_hook=lambda part: self.moba_gate2(l, g, part))
        if self.S.real:
            self.phases.append((6, self.S.idx["pe"]))
        items = []
        for hd in range(8):
            items.append((hd, g))
            for n in range(g - 1, -1, -1):
                items.append((hd, n))
        n_it = len(items)
        LA = 3
        pend = []
        for i in range(n_it + LA):
            if i < n_it:
                pend.append(self.moba_stage1(g, *items[i]))
            if i >= LA:
                j = i - LA
                self.moba_stage2(g, items[j][0], items[j][1], pend[j])
        for a in range(2):
            bT = self.bank()
            bTv = bT.t[:, :].bitcast(BF16)
            for c in range(4):
                S.op("pe", lambda: pe.transpose(bTv[:, c * 128:(c + 1) * 128], self.obtm[:, a, c * 128:(c + 1) * 128], self.ident),
                     reads=[self.b_obtm, self.b_cmat], writes=[bT.buf])
            S.op("act", lambda: act.activation(out=self.obT[:, :, a * 128:(a + 1) * 128],
                                               in_=bTv[:, 0:512].rearrange("p (c t) -> p c t", t=128), func=AF.Copy),
                 writes=[bT.buf, self.b_obT])

    def moba_stage1(self, g, hd, n):
        S, act = self.S, self.nc.scalar
        pr = hd // 2
        bS = self.bank()
        Q = self.QTz[:, hd, :]
        rd = [self.b_QTz, self.bKT[n]]
        rdb = [self.b_cmat, self.b_cbias]
        BT = self.cbias
        if n == g:
            j0, j1 = 2 * g, 2 * g + 1
            self.mm(bS, bS.t[:, 0:256], self.KT[:, pr, j0 * 128:(j0 + 1) * 128], Q[:, 0:256], True, False, rd)
            self.mm(bS, bS.t[:, 0:256], self.ident, BT[:, hd, 0:256], False, True, rdb)
            self.mm(bS, bS.t[:, 256:384], self.KT[:, pr, j1 * 128:(j1 + 1) * 128], Q[:, 128:256], True, False, rd)
            self.mm(bS, bS.t[:, 256:384], self.ident, BT[:, hd, 0:128], False, True, rdb)
            width = 384
        else:
            j0, j1 = 2 * n, 2 * n + 1
            near = (n == g - 1)
            self.mm(bS, bS.t[:, 0:256], self.KT[:, pr, j0 * 128:(j0 + 1) * 128], Q[:, 0:256], True, not near, rd)
            if near:
                self.mm(bS, bS.t[:, 0:256], self.ident, BT[:, hd, 256:512], False, True, rdb)
            self.mm(bS, bS.t[:, 256:512], self.KT[:, pr, j1 * 128:(j1 + 1) * 128], Q[:, 0:256], True, not near, rd)
            if near:
                self.mm(bS, bS.t[:, 256:512], self.ident, BT[:, hd, 128:384], False, True, rdb)
            width = 512
        pt, ptB = self.bfpR.next()
        S.op("act", lambda: act.activation(out=pt[:, 0:width], in_=bS.t[:, 0:width], func=AF.Exp), writes=[bS.buf, ptB])
        return (pt, ptB)

    def moba_stage2(self, g, hd, n, st):
        S, dve = self.S, self.nc.vector
        pt, ptB = st
        bO = self.bank()
        V = self.Vaug
        if n == g:
            j0, j1 = 2 * g, 2 * g + 1
            rd = [ptB, self.bV[g]]
            self.mm(bO, bO.t[:, 0:65], pt[:, 0:128], V[:, j0, hd, :], True, True, rd)
            self.mm(bO, bO.t[:, 65:130], pt[:, 128:256], V[:, j0, hd, :], True, False, rd)
            self.mm(bO, bO.t[:, 65:130], pt[:, 256:384], V[:, j1, hd, :], False, True, rd)
            S.op("dve", lambda: dve.tensor_copy(out=self.accO[:, :, hd, :], in_=bO.t[:, 0:130].rearrange("p (a c) -> p a c", c=65)),
                 writes=[bO.buf, self.b_accO])
        else:
            j0, j1 = 2 * n, 2 * n + 1
            rd = [ptB, self.bV[n]]
            for a in range(2):
                o = bO.t[:, a * 65:(a + 1) * 65]
                self.mm(bO, o, pt[:, a * 128:(a + 1) * 128], V[:, j0, hd, :], True, False, rd)
                self.mm(bO, o, pt[:, 256 + a * 128:256 + (a + 1) * 128], V[:, j1, hd, :], False, True, rd)
            wt, wtB = (self.sel, self.b_sel) if n == g - 1 else (self.selw, self.b_selw)
            for a in range(2):
                cidx = a * 64 + hd * 8 + n
                S.op("dve", lambda: dve.scalar_tensor_tensor(out=self.accO[:, a, hd, :], in0=bO.t[:, a * 65:(a + 1) * 65],
                                                             scalar=wt[:, cidx:cidx + 1], in1=self.accO[:, a, hd, :],
                                                             op0=ALU.mult, op1=ALU.add),
                     reads=[wtB], writes=[bO.buf, self.b_accO])
        if n == 0:
            S.op("dve", lambda: dve.reciprocal(out=self.smalls[:, 24:26], in_=self.accO[:, :, hd, 64]),
                 reads=[self.b_accO], writes=[self.b_rc])
            for a in range(2):
                S.op("dve", lambda: dve.tensor_scalar(out=self.obtm[:, a, hd * 64:(hd + 1) * 64], in0=self.accO[:, a, hd, 0:64],
                                                      scalar1=self.smalls[:, 24 + a:25 + a], scalar2=None, op0=ALU.mult),
                     reads=[self.b_accO, self.b_rc], writes=[self.b_obtm])

    def ffn_group(self, l, g):
        S, nc = self.S, self.nc
        act, dve = nc.scalar, nc.vector
        t0 = g * TG
        h, hB = self.h, self.b_h
        self.chk(9)
        self.rmsnorm(g, CV_NFFN + l * 8)
        accb = self.banks[4:8]
        for b in accb:
            b.fresh = True
        rot = Rot(self.banks[0:4])
        u0 = self.w_used
        slots = {}

        pool = nc.gpsimd
        st = {}

        def s1(ct):
            w, wB = self.w_get(0)
            bk = rot.next()
            bk.fresh = True
            for k in range(8):
                self.mm(bk, bk.t[:, 0:TG], w[:, k * 128:(k + 1) * 128], h[:, k, :], k == 0, k == 7, [wB, hB[k]])
            for k in range(8):
                self.mm(bk, bk.t[:, TG:2 * TG], w[:, 1024 + k * 128:1024 + (k + 1) * 128], h[:, k, :],
                        k == 0, k == 7, [wB, hB[k]])
            ue, ueB, ueH = self.smfR.next()
            S.op("dve", lambda: dve.tensor_copy(out=ue[:, :, 0:2], in_=self.carry[:, :, ct, :]), reads=[self.b_carry], writes=[ueH])
            S.op("act", lambda: act.activation(out=ue[:, :, 2:258], in_=bk.t[:, :].rearrange("p (a t) -> p a t", t=TG), func=AF.Copy),
                 writes=[bk.buf, ueB])
            st[ct] = dict(ue=ue, ueB=ueB, ueH=ueH)
            self.w_done()

        def s2(ct):
            ue, ueB, ueH = st[ct]["ue"], st[ct]["ueB"], st[ct]["ueH"]
            S.op("dve", lambda: dve.tensor_copy(out=self.carry[:, :, ct, :], in_=ue[:, :, 256:258]), reads=[ueB], writes=[self.b_carry])
            cc, ccBs = self.bigfR.next()
            ccB, ccB2 = ccBs
            st[ct].update(cc=cc, ccB=ccB, ccB2=ccB2)
            for ab in range(2):
                cbuf = ccB if ab == 0 else ccB2
                cb = CV_CW + ((l * 2 + ab) * 22 + ct) * 4
                co = cc[:, ab * TG:(ab + 1) * TG]
                wr = [cbuf]
                S.op("pool", lambda: pool.tensor_scalar(out=co, in0=ue[:, ab, 2:258], scalar1=self.cvec[:, cb + 2:cb + 3],
                                                        scalar2=self.cvec[:, cb + 3:cb + 4], op0=ALU.mult, op1=ALU.add),
                     reads=[ueB, self.b_cvec], writes=wr)
            for ab in range(2):
                cbuf = ccB if ab == 0 else ccB2
                cb = CV_CW + ((l * 2 + ab) * 22 + ct) * 4
                co = cc[:, ab * TG:(ab + 1) * TG]
                wr = [cbuf]
                S.op("dve", lambda: dve.scalar_tensor_tensor(out=co, in0=ue[:, ab, 1:257], scalar=self.cvec[:, cb + 1:cb + 2], in1=co,
                                                             op0=ALU.mult, op1=ALU.add),
                     reads=[ueB, ueH, self.b_cvec], writes=wr)
                S.op("dve", lambda: dve.scalar_tensor_tensor(out=co, in0=ue[:, ab, 0:256], scalar=self.cvec[:, cb:cb + 1], in1=co,
                                                             op0=ALU.mult, op1=ALU.add),
                     reads=[ueB, ueH, self.b_cvec], writes=wr)

        def s3(ct):
            cc, ccB = st[ct]["cc"], st[ct]["ccB"]
            S.op("act", lambda: act.activation(out=cc[:, 0:TG], in_=cc[:, 0:TG], func=AF.Silu), reads=[ccB], writes=[ccB])

        def s4(ct):
            cc, ccB = st[ct]["cc"], st[ct]["ccB"]
            at, atB = self.bfpR.next()
            S.op("dve", lambda: dve.tensor_tensor(out=at[:, 0:TG], in0=cc[:, 0:TG], in1=cc[:, TG:2 * TG], op=ALU.mult),
                 reads=[ccB, st[ct]["ccB2"]], writes=[atB])
            st[ct].update(at=at, atB=atB)

        def s5(ct):
            w, wB = self.d_get(0)
            at, atB = st[ct]["at"], st[ct]["atB"]
            for dtile in range(8):
                ab_ = accb[dtile // 2]
                o = ab_.t[:, (dtile % 2) * TG:(dtile % 2 + 1) * TG]
                self.mm(ab_, o, w[:, dtile * 128:(dtile + 1) * 128], at[:, 0:TG],
                        ct == 0, ct == 21, [wB, atB])
            self.d_done()
            del st[ct]

        for i in range(22 + 3):
            if i < 22:
                s1(i)
            if 0 <= i - 1 < 22:
                s3(i - 1)
            if i < 22:
                s2(i)
            if 0 <= i - 1 < 22:
                s4(i - 1)
            if 0 <= i - 3 < 22:
                s5(i - 3)
        self.chk(10)
        for i in range(4):
            S.op("dve", lambda: dve.tensor_tensor(out=self.X[:, 2 * i:2 * i + 2, t0:t0 + TG], in0=self.X[:, 2 * i:2 * i + 2, t0:t0 + TG],
                                                  in1=accb[i].t[:, :].rearrange("p (j t) -> p j t", t=TG), op=ALU.add),
                 writes=[accb[i].buf, self.bX[g]])

    def final_group(self, g):
        S, sp = self.S, self.nc.sync
        t0 = g * TG
        self.rmsnorm(g, CV_NFIN, inplace=True)
        ov = self.d_out.rearrange("(k p) t -> p k t", p=128)
        S.dma("sp", lambda: sp.dma_start(out=ov[:, :, t0:t0 + TG], in_=self.X[:, :, t0:t0 + TG]), "out%d" % (g % 2),
              reads=[self.bX[g]])


def build_program(n_layers=DEPTH, n_groups=NG):
    prog = Prog(n_layers, n_groups)
    s1 = Sched(prog.nc, None)
    prog.emit(s1)
    s2 = Sched(prog.nc, s1.need)
    prog.emit(s2)
    prog.stats = dict(idx=dict(s2.idx), incs=dict(s2.incs), waits=s2.n_wait, sbuf_left=prog.sbuf_left)
    return prog


def _t5_bucket(n):
    n = np.maximum(n, 0)
    nf = np.maximum(n, 1).astype(np.float32)
    large = 16 + (np.log(nf / np.float32(16.0)) / np.float32(math.log(128 / 16)) * np.float32(16)).astype(np.int32)
    large = np.minimum(large, 31)
    return np.where(n < 16, n, large)


def _blk(w, c0, nc_):
    K = w.shape[0] // 128
    return np.ascontiguousarray(w[:, c0:c0 + nc_].reshape(K, 128, nc_).transpose(1, 0, 2)).reshape(128, K * nc_)


def prep_shared(inp, n_layers=DEPTH):
    f32 = np.float32
    L = n_layers
    w_in = np.asarray(inp["w_in"], f32)
    wa = np.zeros((L * NBA, 128, WA_E), f32)
    wf = np.zeros((L * NBF, 128, WSLOT), f32)
    walr = np.zeros((L, 128, 128), f32)
    for l in range(L):
        wi = w_in[l]
        blocks = []
        blocks += [_blk(wi, O_KA + q * 256, 256) for q in range(2)]
        for hp in range(2):
            blocks += [_blk(wi, O_QA + hp * 256, 256), _blk(wi, O_KA + hp * 256, 256)]
        blocks += [_blk(wi, O_VA + q * 256, 256) for q in range(4)]
        blocks += [_blk(wi, O_QB + q * 256, 256) for q in range(2)]
        blocks += [_blk(wi, O_KB + q * 256, 256) for q in range(2)]
        blocks += [_blk(wi, O_VB + q * 256, 256) for q in range(2)]
        blocks += [_blk(wi, O_RA + q * 256, 256) for q in range(4)]
        wbg = np.asarray(inp["w_branch_gla"][l], f32)
        wbm = np.asarray(inp["w_branch_moba"][l], f32)
        for dtile in range(8):
            c0 = dtile * 128
            m = np.zeros((128, WA_E), f32)
            m[:, 0:1024] = _blk(wbg, c0, 128)
            m[:, 1024:1536] = _blk(wbm, c0, 128)
            blocks.append(m)
            m = np.zeros((128, WA_E), f32)
            m[:, 0:1024] = _blk(wi, O_G + c0, 128)
            m[:, 1024:2048] = _blk(wi, O_G + 1024 + c0, 128)
            blocks.append(m)
        wo = np.asarray(inp["w_out"][l], f32)
        blocks += [_blk(wo, q * 256, 256) for q in range(4)]
        assert len(blocks) == NBA
        for i, bb in enumerate(blocks):
            wa[l * NBA + i] = bb
        walr[l] = _blk(wi, O_ALR, 16)
        wu = np.asarray(inp["w_up"][l], f32)
        wd = np.asarray(inp["w_down"][l], f32)
        for ct in range(NBF):
            wf[l * NBF + ct, :, 0:1024] = _blk(wu, ct * 128, 128)
            wf[l * NBF + ct, :, 1024:2048] = _blk(wu, D_FF + ct * 128, 128)
            wf[l * NBF + ct, :, 2048:3072] = wd[ct * 128:(ct + 1) * 128, :]
    wlr = np.zeros((17, DEPTH * 512), f32)
    for l in range(L):
        wlr[0:16, l * 512:(l + 1) * 512] = np.asarray(inp["w_lr_up"][l], f32)
        wlr[16, l * 512:(l + 1) * 512] = np.asarray(inp["b_forget"][l], f32)
    cvec = np.zeros((128, CV_N), f32)
    for l in range(L):
        cvec[:, CV_NMIX + l * 8:CV_NMIX + (l + 1) * 8] = np.asarray(inp["norm_mix"][l], f32).reshape(8, 128).T
        cvec[:, CV_NFFN + l * 8:CV_NFFN + (l + 1) * 8] = np.asarray(inp["norm_ffn"][l], f32).reshape(8, 128).T
        cvec[:, CV_GLA + l * 8:CV_GLA + (l + 1) * 8] = np.asarray(inp["gla_out_norm"][l], f32).reshape(8, 128).T
        cw = np.asarray(inp["conv_w"][l], f32)
        cb = np.asarray(inp["conv_b"][l], f32)
        for ab in range(2):
            full = np.concatenate([cw[:, ab * D_FF:(ab + 1) * D_FF], cb[None, ab * D_FF:(ab + 1) * D_FF]], axis=0)
            arr = full.reshape(4, 22, 128).transpose(2, 1, 0)
            c0 = CV_CW + (l * 2 + ab) * 22 * 4
            cvec[:, c0:c0 + 88] = arr.reshape(128, 88)
    cvec[:, CV_NFIN:CV_NFIN + 8] = np.asarray(inp["norm_final"], f32).reshape(8, 128).T
    rb = np.asarray(inp["rel_bias"], f32)
    kk = np.arange(128)[:, None]
    qq = np.arange(128)[None, :]
    cbias = np.zeros((128, 8, 512), f32)
    bd = _t5_bucket(qq - kk)
    bs = _t5_bucket(128 + qq - kk)
    for hd in range(8):
        diag = rb[bd, hd]
        cbias[:, hd, 0:128] = np.where(qq >= kk, diag, np.float32(NEG))
        cbias[:, hd, 128:256] = rb[bs, hd]
        cbias[:, hd, 256:512] = rb[31, hd]
    cmisc = np.zeros((128, CM_N), f32)
    cmisc[:, CM_B31:CM_B31 + 64] = np.repeat(rb[31, :], 8)[None, :]
    cmat = np.zeros((128, CMAT_N), f32)
    s = np.arange(128)[:, None]
    t = np.arange(128)[None, :]
    cmat[:, 0:128] = np.eye(128, dtype=f32)
    cmat[:, 128:256] = 1.0 / 1024.0
    cmat[:, 256:384] = (s <= t).astype(f32)
    cmat[:, 384:512] = np.where(s <= t, -1.0 / 16.0, 0.0)
    cmat[:, 512:640] = np.where(s > t, -1.0 / 16.0, 0.0)
    return dict(wa=wa, wf=wf, walr=walr, wlr=wlr, cvec=cvec, cbias=cbias.reshape(128, 8 * 512), cmisc=cmisc, cmat=cmat)


_PROG_CACHE = {}


def kernel(x, rel_bias, norm_mix, w_in, w_lr_up, b_forget, gla_out_norm, w_branch_gla, w_branch_moba, w_out,
           norm_ffn, w_up, conv_w, conv_b, w_down, norm_final):
    inp = dict(rel_bias=rel_bias, norm_mix=norm_mix, w_in=w_in, w_lr_up=w_lr_up, b_forget=b_forget,
               gla_out_norm=gla_out_norm, w_branch_gla=w_branch_gla, w_branch_moba=w_branch_moba, w_out=w_out,
               norm_ffn=norm_ffn, w_up=w_up, conv_w=conv_w, conv_b=conv_b, w_down=w_down, norm_final=norm_final)
    x = np.asarray(x, np.float32)
    Bn = x.shape[0]
    shared = prep_shared(inp)
    prog = build_program()
    in_maps = []
    for b in range(Bn):
        m = dict(shared)
        m["xT"] = np.ascontiguousarray(x[b].T)
        in_maps.append(m)
    res = run_bass_kernel_spmd(prog.nc, in_maps, core_ids=list(range(Bn)))
    out = np.stack([np.ascontiguousarray(r["outT"].T) for r in res.results], axis=0)
    return out.astype(np.float32)
```
